# Optimizing a Trainium2 kernel written in Bass

```python
import jax, jax.numpy as jnp
from jax import lax
import numpy as np

D_MODEL = 1024
BATCH = 8
SEQ = 2048
DEPTH = 4

GRID_W = 64
CTX_LEN = 256
N_MIXERS = 3
N_WIN = (DEPTH + 2) // 3
N_GLB = (DEPTH + 1) // 3
N_GDN = DEPTH // 3

HEAD_DIM = 64
N_HEADS = D_MODEL // HEAD_DIM
N_KV_HEADS = 4
GROUP = N_HEADS // N_KV_HEADS
QKV_DIM = (N_HEADS + 2 * N_KV_HEADS) * HEAD_DIM
WINDOW = 128
Q_BLOCK = 128
ROPE_THETA = 10000.0
AXIS_DIM = HEAD_DIM // 2

GDN_HEADS = D_MODEL // 128
GDN_DK = 128
GDN_DV = 128
GDN_KW = GDN_HEADS * GDN_DK
GDN_VW = GDN_HEADS * GDN_DV
GDN_IN_DIM = 2 * GDN_KW + 2 * GDN_VW + 4 * GDN_HEADS
CONV_K = 5
CHUNK = 64

N_EXPERTS = 32
TOP_K = 4
D_EXPERT = D_MODEL
SWIGLU_LIMIT = 7.0
SWIGLU_ALPHA = 1.702
EXPERT_BLOCK = 128

NORM_EPS = 1e-6
NEG_INF = -1e30

kernel_name = 'hybrid_interleaved_flow_backbone'


def rms_norm(x, g):
    xf = x.astype(jnp.float32)
    y = xf * lax.rsqrt(jnp.mean(xf * xf, axis=-1, keepdims=True) + NORM_EPS)
    return (y * g.astype(jnp.float32)).astype(x.dtype)


def l2_norm(x):
    return x * lax.rsqrt(jnp.sum(x * x, axis=-1, keepdims=True) + NORM_EPS)


def adaln(cond, w_mod, b_mod):
    m = jax.nn.silu(cond) @ w_mod + b_mod
    return jnp.split(m[:, None, :], 6, axis=-1)


def axial_tables(length, dtype):
    rows = length // GRID_W
    row = jnp.broadcast_to(jnp.arange(rows)[:, None], (rows, GRID_W)).reshape(-1)
    col = jnp.broadcast_to(jnp.arange(GRID_W)[None, :], (rows, GRID_W)).reshape(-1)
    pos = jnp.stack([row, col], axis=-1).astype(jnp.float32)
    inv = ROPE_THETA ** (-jnp.arange(0, AXIS_DIM, 2, dtype=jnp.float32) / AXIS_DIM)
    ang = pos[:, None, :, None] * inv
    return jnp.cos(ang).astype(dtype), jnp.sin(ang).astype(dtype)


def axial_rope(x, cos, sin):
    b, l, h, _ = x.shape
    xa = x.reshape(b, l, h, 2, AXIS_DIM)
    x1, x2 = xa[..., :AXIS_DIM // 2], xa[..., AXIS_DIM // 2:]
    out = jnp.concatenate([x1 * cos - x2 * sin, x2 * cos + x1 * sin], axis=-1)
    return out.reshape(b, l, h, HEAD_DIM)


def split_qkv(p):
    b, l, _ = p.shape
    nq, nk = N_HEADS * HEAD_DIM, N_KV_HEADS * HEAD_DIM
    q = p[..., :nq].reshape(b, l, N_HEADS, HEAD_DIM)
    k = p[..., nq:nq + nk].reshape(b, l, N_KV_HEADS, HEAD_DIM)
    v = p[..., nq + nk:].reshape(b, l, N_KV_HEADS, HEAD_DIM)
    return q, k, v


def attend(q, k, v, mask, sink):
    b, lq = q.shape[:2]
    qg = q.reshape(b, lq, N_KV_HEADS, GROUP, HEAD_DIM)
    s = jnp.einsum('bqkgd,bskd->bkgqs', qg, k).astype(jnp.float32) * (HEAD_DIM ** -0.5)
    if mask is not None:
        s = jnp.where(mask, s, NEG_INF)
    m = jnp.max(s, axis=-1, keepdims=True)
    if sink is not None:
        sk = sink.astype(jnp.float32).reshape(1, N_KV_HEADS, GROUP, 1, 1)
        m = jnp.maximum(m, sk)
        p = jnp.exp(s - m)
        den = jnp.sum(p, axis=-1, keepdims=True) + jnp.exp(sk - m)
    else:
        p = jnp.exp(s - m)
        den = jnp.sum(p, axis=-1, keepdims=True)
    o = jnp.einsum('bkgqs,bskd->bqkgd', (p / den).astype(v.dtype), v)
    return o.reshape(b, lq, N_HEADS * HEAD_DIM)


def window_gqa(h_lat, h_ctx, w_qkv, b_qkv, sink, w_o, b_o, cos, sin, need_ctx):
    b, l, _ = h_lat.shape
    q, k, v = split_qkv(h_lat @ w_qkv + b_qkv)
    q, k = axial_rope(q, cos, sin), axial_rope(k, cos, sin)
    qc, kc, vc = split_qkv(h_ctx @ w_qkv + b_qkv)
    pad = ((0, 0), (WINDOW, WINDOW), (0, 0), (0, 0))
    kp, vp = jnp.pad(k, pad), jnp.pad(v, pad)
    nb = l // Q_BLOCK
    span = Q_BLOCK + 2 * WINDOW
    qb = q.reshape(b, nb, Q_BLOCK, N_HEADS, HEAD_DIM).swapaxes(0, 1)
    ctx_ok = jnp.ones((Q_BLOCK, kc.shape[1]), bool)

    def block(args):
        qblk, bi = args
        start = bi * Q_BLOCK
        kblk = lax.dynamic_slice_in_dim(kp, start, span, axis=1)
        vblk = lax.dynamic_slice_in_dim(vp, start, span, axis=1)
        qpos = start + jnp.arange(Q_BLOCK)
        kpos = start - WINDOW + jnp.arange(span)
        band = (jnp.abs(qpos[:, None] - kpos[None, :]) <= WINDOW) & (kpos >= 0)[None, :] & (kpos < l)[None, :]
        mask = jnp.concatenate([band, ctx_ok], axis=1)
        return attend(qblk, jnp.concatenate([kblk, kc], axis=1), jnp.concatenate([vblk, vc], axis=1), mask, sink)

    o = lax.map(block, (qb, jnp.arange(nb)))
    y_lat = o.swapaxes(0, 1).reshape(b, l, N_HEADS * HEAD_DIM) @ w_o + b_o
    y_ctx = attend(qc, kc, vc, None, sink) @ w_o + b_o if need_ctx else None
    return y_lat, y_ctx


def global_qknorm_gqa(h_lat, h_ctx, w_qkv, g_q, g_k, w_o, cos, sin, need_ctx):
    b, l, _ = h_lat.shape
    q, k, v = split_qkv(h_lat @ w_qkv)
    q = axial_rope(rms_norm(q, g_q), cos, sin)
    k = axial_rope(rms_norm(k, g_k), cos, sin)
    qc, kc, vc = split_qkv(h_ctx @ w_qkv)
    qc, kc = rms_norm(qc, g_q), rms_norm(kc, g_k)
    k_all = jnp.concatenate([k, kc], axis=1)
    v_all = jnp.concatenate([v, vc], axis=1)
    nb = l // Q_BLOCK
    qb = q.reshape(b, nb, Q_BLOCK, N_HEADS, HEAD_DIM).swapaxes(0, 1)
    o = lax.map(lambda qblk: attend(qblk, k_all, v_all, None, None), qb)
    y_lat = o.swapaxes(0, 1).reshape(b, l, N_HEADS * HEAD_DIM) @ w_o
    y_ctx = attend(qc, kc, vc, None, None) @ w_o if need_ctx else None
    return y_lat, y_ctx


def short_conv(x, w):
    return lax.conv_general_dilated(x, w[:, None, :].astype(x.dtype), window_strides=(1,),
                                    padding=[(CONV_K // 2, CONV_K // 2)],
                                    dimension_numbers=('NWC', 'WIO', 'NWC'),
                                    feature_group_count=x.shape[-1])


def gdn_project(h, w_in, conv_w, a_log, dt_bias):
    b, l, _ = h.shape
    p = h @ w_in
    qkv = jax.nn.silu(short_conv(p[..., :2 * GDN_KW + GDN_VW], conv_w)).astype(jnp.float32)
    q = l2_norm(qkv[..., :GDN_KW].reshape(b, l, GDN_HEADS, GDN_DK)) * (GDN_DK ** -0.5)
    k = l2_norm(qkv[..., GDN_KW:2 * GDN_KW].reshape(b, l, GDN_HEADS, GDN_DK))
    v = qkv[..., 2 * GDN_KW:].reshape(b, l, GDN_HEADS, GDN_DV)
    o = 2 * GDN_KW + GDN_VW
    z = p[..., o:o + GDN_VW].reshape(b, l, GDN_HEADS, GDN_DV)
    o += GDN_VW
    beta = jax.nn.sigmoid(p[..., o:o + 2 * GDN_HEADS].astype(jnp.float32)).reshape(b, l, 2, GDN_HEADS)
    a = p[..., o + 2 * GDN_HEADS:].astype(jnp.float32).reshape(b, l, 2, GDN_HEADS)
    g = -jnp.exp(a_log.astype(jnp.float32)) * jax.nn.softplus(a + dt_bias.astype(jnp.float32))
    return q, k, v, z, beta, g


def chunk_gated_delta(q, k, v, g, beta, s0):
    b, t, h, _ = q.shape
    dv = v.shape[-1]
    n = t // CHUNK

    def chunks(a):
        a = a.reshape(b, n, CHUNK, h, *a.shape[3:])
        return jnp.moveaxis(a, 3, 2).swapaxes(0, 1)

    qc, kc, vc, gc, bc = chunks(q), chunks(k), chunks(v), chunks(g), chunks(beta)
    gcum = jnp.cumsum(gc, axis=-1)
    lower = jnp.tril(jnp.ones((CHUNK, CHUNK), bool))
    strict = jnp.tril(jnp.ones((CHUNK, CHUNK), bool), -1)
    diff = gcum[..., :, None] - gcum[..., None, :]
    decay = jnp.where(lower, jnp.exp(jnp.where(lower, diff, 0.0)), 0.0)
    kb = kc * bc[..., None]
    a_mat = jnp.where(strict, jnp.einsum('nbhid,nbhjd->nbhij', kb, kc) * decay, 0.0)
    eye = jnp.eye(CHUNK, dtype=jnp.float32)
    t_inv = lax.linalg.triangular_solve(eye + a_mat, jnp.broadcast_to(eye, a_mat.shape),
                                        left_side=True, lower=True, unit_diagonal=True)
    u = t_inv @ (vc * bc[..., None])
    w = t_inv @ (kb * jnp.exp(gcum)[..., None])
    intra = jnp.einsum('nbhid,nbhjd->nbhij', qc, kc) * decay

    def step(s, xs):
        q_i, k_i, u_i, w_i, intra_i, g_i = xs
        v_new = u_i - w_i @ s
        o = (q_i * jnp.exp(g_i)[..., None]) @ s + intra_i @ v_new
        g_last = g_i[..., -1:]
        s = s * jnp.exp(g_last)[..., None] + jnp.einsum('bhcd,bhce->bhde', k_i * jnp.exp(g_last - g_i)[..., None], v_new)
        return s, o

    s, o = lax.scan(step, s0, (qc, kc, u, w, intra, gcum))
    o = jnp.moveaxis(o.swapaxes(0, 1), 2, 3).reshape(b, t, h, dv)
    return o, s


def gated_deltanet(h_lat, h_ctx, w_in, conv_w, a_log, dt_bias, g_out, w_o, need_ctx):
    ql, kl, vl, zl, bl, gl = gdn_project(h_lat, w_in, conv_w, a_log, dt_bias)
    qc, kc, vc, zc, bc, gc = gdn_project(h_ctx, w_in, conv_w, a_log, dt_bias)
    b = h_lat.shape[0]
    s0 = jnp.zeros((b, GDN_HEADS, GDN_DK, GDN_DV), jnp.float32)
    flip = lambda a: jnp.flip(a, axis=1)
    oc_f, sc_f = chunk_gated_delta(qc, kc, vc, gc[:, :, 0], bc[:, :, 0], s0)
    ol_f, _ = chunk_gated_delta(ql, kl, vl, gl[:, :, 0], bl[:, :, 0], sc_f)
    oc_b, sc_b = chunk_gated_delta(flip(qc), flip(kc), flip(vc), flip(gc[:, :, 1]), flip(bc[:, :, 1]), s0)
    ol_b, _ = chunk_gated_delta(flip(ql), flip(kl), flip(vl), flip(gl[:, :, 1]), flip(bl[:, :, 1]), sc_b)

    def readout(o, z):
        o = (rms_norm(o, g_out) * jax.nn.silu(z.astype(jnp.float32))).astype(h_lat.dtype)
        return o.reshape(b, o.shape[1], GDN_VW) @ w_o

    y_lat = readout(ol_f + flip(ol_b), zl)
    y_ctx = readout(oc_f + flip(oc_b), zc) if need_ctx else None
    return y_lat, y_ctx


def moe_ffn(h, w_router, b_router, w_up, b_up, w_down, b_down):
    n, d = h.shape
    logits = (h @ w_router + b_router).astype(jnp.float32)
    top_val, top_idx = lax.top_k(logits, TOP_K)
    gates = jax.nn.softmax(top_val, axis=-1).astype(h.dtype)
    flat_e = top_idx.reshape(-1)
    flat_tok = jnp.repeat(jnp.arange(n, dtype=jnp.int32), TOP_K)
    order = jnp.argsort(flat_e)
    e_sorted = flat_e[order]
    counts = jnp.bincount(flat_e, length=N_EXPERTS)
    padded = (counts + EXPERT_BLOCK - 1) // EXPERT_BLOCK * EXPERT_BLOCK
    start = jnp.cumsum(counts) - counts
    pend = jnp.cumsum(padded)
    pstart = pend - padded
    dest = pstart[e_sorted] + jnp.arange(n * TOP_K) - start[e_sorted]
    n_blocks = -(-(n * TOP_K) // EXPERT_BLOCK) + N_EXPERTS
    rows = n_blocks * EXPERT_BLOCK
    tok_buf = jnp.full((rows,), n, jnp.int32).at[dest].set(flat_tok[order])
    gate_buf = jnp.zeros((rows,), h.dtype).at[dest].set(gates.reshape(-1)[order])
    blk_expert = jnp.minimum(jnp.searchsorted(pend, jnp.arange(n_blocks) * EXPERT_BLOCK, side='right'), N_EXPERTS - 1)
    xb = jnp.concatenate([h, jnp.zeros((1, d), h.dtype)], axis=0)[tok_buf].reshape(n_blocks, EXPERT_BLOCK, d)

    def expert_block(args):
        xe, e = args
        u = xe @ w_up[e] + b_up[e]
        glu = jnp.minimum(u[..., :D_EXPERT], SWIGLU_LIMIT)
        lin = jnp.clip(u[..., D_EXPERT:], -SWIGLU_LIMIT, SWIGLU_LIMIT)
        act = glu * jax.nn.sigmoid(SWIGLU_ALPHA * glu) * (lin + 1.0)
        return act @ w_down[e] + b_down[e]

    yb = lax.map(expert_block, (xb, blk_expert)).reshape(rows, d) * gate_buf[:, None]
    return jnp.zeros((n + 1, d), h.dtype).at[tok_buf].add(yb)[:n]


def setup_inputs(seed: int = 0) -> dict:
    key = jax.random.key(seed)
    ks = jax.random.split(key, 32)
    f32 = jnp.float32
    nrm = lambda k, shape, scale: jax.random.normal(k, shape, f32) * scale
    d = D_MODEL
    dt = jnp.exp(jax.random.uniform(ks[20], (N_GDN, 2, GDN_HEADS), f32, np.log(1e-3), np.log(1e-1)))
    return {
        'x': nrm(ks[0], (BATCH, SEQ, d), 1.0),
        'c': nrm(ks[1], (BATCH, d), 1.0),
        'ctx': nrm(ks[2], (BATCH, CTX_LEN, d), 1.0),
        'c_ctx': nrm(ks[3], (d,), 1.0),
        'w_mod': nrm(ks[4], (DEPTH, d, 6 * d), 0.5 * d ** -0.5),
        'b_mod': nrm(ks[5], (DEPTH, 6 * d), 0.02),
        'g_mix': 1.0 + nrm(ks[6], (DEPTH, d), 0.02),
        'g_ffn': 1.0 + nrm(ks[7], (DEPTH, d), 0.02),
        'win_w_qkv': nrm(ks[8], (N_WIN, d, QKV_DIM), d ** -0.5),
        'win_b_qkv': nrm(ks[9], (N_WIN, QKV_DIM), 0.02),
        'win_sink': nrm(ks[10], (N_WIN, N_HEADS), 1.0),
        'win_w_o': nrm(ks[11], (N_WIN, N_HEADS * HEAD_DIM, d), (N_HEADS * HEAD_DIM) ** -0.5),
        'win_b_o': nrm(ks[12], (N_WIN, d), 0.02),
        'glb_w_qkv': nrm(ks[13], (N_GLB, d, QKV_DIM), d ** -0.5),
        'glb_g_q': 1.0 + nrm(ks[14], (N_GLB, HEAD_DIM), 0.02),
        'glb_g_k': 1.0 + nrm(ks[15], (N_GLB, HEAD_DIM), 0.02),
        'glb_w_o': nrm(ks[16], (N_GLB, N_HEADS * HEAD_DIM, d), (N_HEADS * HEAD_DIM) ** -0.5),
        'gdn_w_in': nrm(ks[17], (N_GDN, d, GDN_IN_DIM), d ** -0.5),
        'gdn_conv_w': nrm(ks[18], (N_GDN, CONV_K, 2 * GDN_KW + GDN_VW), CONV_K ** -0.5),
        'gdn_a_log': jnp.log(jax.random.uniform(ks[19], (N_GDN, 2, GDN_HEADS), f32, 1.0, 16.0)),
        'gdn_dt_bias': jnp.log(jnp.expm1(dt)),
        'gdn_g_out': 1.0 + nrm(ks[21], (N_GDN, GDN_DV), 0.02),
        'gdn_w_o': nrm(ks[22], (N_GDN, GDN_VW, d), GDN_VW ** -0.5),
        'moe_w_router': nrm(ks[23], (DEPTH, d, N_EXPERTS), d ** -0.5),
        'moe_b_router': nrm(ks[24], (DEPTH, N_EXPERTS), 0.01),
        'moe_w_up': nrm(ks[25], (DEPTH, N_EXPERTS, d, 2 * D_EXPERT), d ** -0.5),
        'moe_b_up': nrm(ks[26], (DEPTH, N_EXPERTS, 2 * D_EXPERT), 0.02),
        'moe_w_down': nrm(ks[27], (DEPTH, N_EXPERTS, D_EXPERT, d), D_EXPERT ** -0.5),
        'moe_b_down': nrm(ks[28], (DEPTH, N_EXPERTS, d), 0.02),
        'g_final': 1.0 + nrm(ks[29], (d,), 0.02),
    }


def reference(x, c, ctx, c_ctx, w_mod, b_mod, g_mix, g_ffn, win_w_qkv, win_b_qkv, win_sink, win_w_o, win_b_o,
              glb_w_qkv, glb_g_q, glb_g_k, glb_w_o, gdn_w_in, gdn_conv_w, gdn_a_log, gdn_dt_bias, gdn_g_out, gdn_w_o,
              moe_w_router, moe_b_router, moe_w_up, moe_b_up, moe_w_down, moe_b_down, g_final):
    b, l, d = x.shape
    cos, sin = axial_tables(l, x.dtype)
    x_lat, x_ctx = x, ctx
    for i in range(DEPTH):
        kind, j = i % N_MIXERS, i // N_MIXERS
        need_ctx = i < DEPTH - 1
        sh1, sc1, gt1, sh2, sc2, gt2 = adaln(c, w_mod[i], b_mod[i])
        csh1, csc1, cgt1, csh2, csc2, cgt2 = adaln(c_ctx[None, :], w_mod[i], b_mod[i])
        h_lat = rms_norm(x_lat, g_mix[i]) * (1.0 + sc1) + sh1
        h_ctx = rms_norm(x_ctx, g_mix[i]) * (1.0 + csc1) + csh1
        if kind == 0:
            y_lat, y_ctx = window_gqa(h_lat, h_ctx, win_w_qkv[j], win_b_qkv[j], win_sink[j], win_w_o[j], win_b_o[j],
                                      cos, sin, need_ctx)
        elif kind == 1:
            y_lat, y_ctx = global_qknorm_gqa(h_lat, h_ctx, glb_w_qkv[j], glb_g_q[j], glb_g_k[j], glb_w_o[j],
                                             cos, sin, need_ctx)
        else:
            y_lat, y_ctx = gated_deltanet(h_lat, h_ctx, gdn_w_in[j], gdn_conv_w[j], gdn_a_log[j], gdn_dt_bias[j],
                                          gdn_g_out[j], gdn_w_o[j], need_ctx)
        x_lat = x_lat + gt1 * y_lat
        h_lat = rms_norm(x_lat, g_ffn[i]) * (1.0 + sc2) + sh2
        if need_ctx:
            x_ctx = x_ctx + cgt1 * y_ctx
            h_ctx = rms_norm(x_ctx, g_ffn[i]) * (1.0 + csc2) + csh2
            n_ctx = h_ctx.shape[0] * h_ctx.shape[1]
            tokens = jnp.concatenate([h_ctx.reshape(-1, d), h_lat.reshape(-1, d)], axis=0)
            y = moe_ffn(tokens, moe_w_router[i], moe_b_router[i], moe_w_up[i], moe_b_up[i], moe_w_down[i], moe_b_down[i])
            x_ctx = x_ctx + cgt2 * y[:n_ctx].reshape(x_ctx.shape)
            x_lat = x_lat + gt2 * y[n_ctx:].reshape(x_lat.shape)
        else:
            y = moe_ffn(h_lat.reshape(-1, d), moe_w_router[i], moe_b_router[i], moe_w_up[i], moe_b_up[i],
                        moe_w_down[i], moe_b_down[i])
            x_lat = x_lat + gt2 * y.reshape(x_lat.shape)
    return rms_norm(x_lat, g_final)
```

```python
import numpy as np
import concourse.bass as bass
import concourse.mybir as mybir
from concourse.bass_utils import run_bass_kernel_spmd
from contextlib import ExitStack

F32 = mybir.dt.float32
BF16 = mybir.dt.bfloat16
ALU = mybir.AluOpType
AF = mybir.ActivationFunctionType
AX = mybir.AxisListType

D = 1024
NCTX = 256
NLAT = 2048
NTOK = NCTX + NLAT
NT = NTOK // 128
DEPTH = 4
NE = 32


class Dep:
    __slots__ = ("name", "w", "r")

    def __init__(self, name=""):
        self.name = name
        self.w = None
        self.r = []


class Tile:
    def __init__(self, t, dep=None):
        self.t = t
        self.dep = dep or Dep()

    def __getitem__(self, k):
        return self.t[k]


class Ring:
    def __init__(self, tiles):
        self.tiles = tiles
        self.i = 0

    def next(self):
        t = self.tiles[self.i % len(self.tiles)]
        self.i += 1
        return t


class Prog:
    ENG = ("pe", "act", "dve", "pool", "sp")

    def __init__(self, nc, n_dma_sems=24):
        self.nc = nc
        self.es = ExitStack()
        self.sem = {}
        self.cnt = {}
        self.ops = {e: [] for e in self.ENG}
        self.waited = {e: {} for e in self.ENG}
        for e in self.ENG:
            self.sem[e] = self.es.enter_context(nc.semaphore("s_" + e))
            self.cnt[e] = 0
        self.dq = {}
        for q in ("sp", "pool", "act"):
            n = n_dma_sems if q != "act" else 8
            sems = [self.es.enter_context(nc.semaphore(f"d_{q}{i}")) for i in range(n)]
            self.dq[q] = {"sems": sems, "tgt": [0] * n, "i": 0}
        self.semobj = {}
        for e in self.ENG:
            self.semobj[("e", e)] = self.sem[e]
        for q, d in self.dq.items():
            for i, s in enumerate(d["sems"]):
                self.semobj[("d", q, i)] = s
        self.stk = [ExitStack()]
        self.n_t = 0
        self.ps = None

    def sb(self, shape, dtype, name=None):
        self.n_t += 1
        name = f"{name or 't'}_{self.n_t}"
        t = self.stk[-1].enter_context(self.nc.sbuf_tensor(name, list(shape), dtype))
        return Tile(t, Dep(name))

    def ring(self, n, shape, dtype, name=None):
        return Ring([self.sb(shape, dtype, name) for _ in range(n)])

    def mark(self):
        self.stk.append(ExitStack())

    def release(self):
        self.barrier()
        self.stk.pop().close()

    def psum(self, shape, dtype=F32, name=None):
        self.n_t += 1
        name = name or f"ps{self.n_t}"
        t = self.nc.alloc_psum_tensor(name, list(shape), dtype)
        return Tile(t, Dep(name))

    @staticmethod
    def _d(x):
        return x.dep if isinstance(x, Tile) else x

    def _collect(self, reads, writes):
        need = {}
        for r in reads:
            d = self._d(r)
            if d.w is not None:
                k, v = d.w
                if need.get(k, 0) < v:
                    need[k] = v
        for w in writes:
            d = self._d(w)
            if d.w is not None:
                k, v = d.w
                if need.get(k, 0) < v:
                    need[k] = v
            for (k, v) in d.r:
                if need.get(k, 0) < v:
                    need[k] = v
        return need

    def _waits(self, eng, need):
        ws = []
        wd = self.waited[eng]
        for k, v in need.items():
            if eng == "pe" and k == ("e", "pe"):
                continue
            if wd.get(k, 0) < v:
                wd[k] = v
                ws.append((k, v))
        return ws

    def _commit(self, tok, reads, writes):
        for r in reads:
            d = self._d(r)
            d.r.append(tok)
            if len(d.r) > 48:
                m = {}
                for k, v in d.r:
                    if m.get(k, 0) < v:
                        m[k] = v
                d.r = list(m.items())
        for w in writes:
            d = self._d(w)
            d.w = tok
            d.r = []

    def op(self, eng, fn, reads=(), writes=()):
        need = self._collect(reads, writes)
        ws = self._waits(eng, need)
        self.cnt[eng] += 1
        tok = (("e", eng), self.cnt[eng])
        self.ops[eng].append((ws, fn, (("e", eng), 1)))
        self._commit(tok, reads, writes)
        return tok

    def dma(self, q, out, in_, reads=(), writes=(), **kw):
        need = self._collect(reads, writes)
        d = self.dq[q]
        i = d["i"] % len(d["sems"])
        d["i"] += 1
        key = ("d", q, i)
        if d["tgt"][i] > 0:
            need[key] = max(need.get(key, 0), d["tgt"][i])
        ws = self._waits(q, need)
        d["tgt"][i] += 16
        tok = (key, d["tgt"][i])
        self.ops[q].append((ws, (lambda e: e.dma_start(out=out, in_=in_, **kw)), (key, 16)))
        self._commit(tok, reads, writes)
        return tok

    def barrier(self):
        need = {}
        for e in self.ENG:
            if self.cnt[e] > 0:
                need[("e", e)] = self.cnt[e]
        for q, d in self.dq.items():
            for i, t in enumerate(d["tgt"]):
                if t > 0:
                    need[("d", q, i)] = t
        for e in self.ENG:
            ws = []
            wd = self.waited[e]
            for k, v in need.items():
                if k == ("e", e):
                    continue
                if wd.get(k, 0) < v:
                    wd[k] = v
                    ws.append((k, v))
            if ws:
                self.ops[e].append((ws, None, None))

    def act(self, out, in_, func, R, W, **kw):
        return self.op("act", lambda e: e.activation(out=out, in_=in_, func=func, **kw), R, W)

    def ts(self, out, in0, s1, s2, op0, op1, R, W, eng="dve"):
        if op1 is None:
            return self.op(eng, lambda e: e.tensor_scalar(out=out, in0=in0, scalar1=s1, scalar2=None, op0=op0), R, W)
        return self.op(eng, lambda e: e.tensor_scalar(out=out, in0=in0, scalar1=s1, scalar2=s2, op0=op0, op1=op1), R, W)

    def tt(self, out, in0, in1, op, R, W, eng="dve"):
        return self.op(eng, lambda e: e.tensor_tensor(out=out, in0=in0, in1=in1, op=op), R, W)

    def stt(self, out, in0, scalar, in1, op0, op1, R, W, eng="dve"):
        return self.op(eng, lambda e: e.scalar_tensor_tensor(out=out, in0=in0, scalar=scalar, in1=in1, op0=op0, op1=op1), R, W)

    def cp(self, out, in_, R, W, eng="dve"):
        if eng == "act":
            return self.op("act", lambda e: e.copy(out=out, in_=in_), R, W)
        return self.op(eng, lambda e: e.tensor_copy(out=out, in_=in_), R, W)

    def memset(self, ap, val, W, eng="dve"):
        return self.op(eng, lambda e: e.memset(ap, val), (), W)

    def mm(self, out, lhsT, rhs, start, stop, R, W):
        return self.op("pe", lambda e: e.matmul(out, lhsT=lhsT, rhs=rhs, start=start, stop=stop), R, W)

    def tr(self, out, in_, ident, R, W):
        return self.op("pe", lambda e: e.transpose(out=out, in_=in_, identity=ident), R, W)

    def recip(self, out, in_, R, W):
        return self.op("dve", lambda e: e.reciprocal(out=out, in_=in_), R, W)

    def next_ps(self):
        return self.ps.next()

    def emit(self):
        self.barrier()
        nc = self.nc
        with nc.Block() as block:
            def body(eng):
                def run(e):
                    for ws, fn, inc in self.ops[eng]:
                        for k, v in ws:
                            e.wait_ge(self.semobj[k], v)
                        if fn is not None:
                            ins = fn(e)
                            ins.then_inc(self.semobj[inc[0]], inc[1])
                return run
            block.tensor(body("pe"))
            block.scalar(body("act"))
            block.vector(body("dve"))
            block.gpsimd(body("pool"))
            block.sync(body("sp"))
        while self.stk:
            self.stk.pop().close()
        self.es.close()


INPUT_SHAPES = {
    "x": [NLAT, D], "c": [D], "ctx": [NCTX, D], "c_ctx": [D],
    "w_mod": [4, D, 6 * D], "b_mod": [4, 6 * D], "g_mix": [4, D], "g_ffn": [4, D],
    "win_w_qkv": [2, D, 1536], "win_b_qkv": [2, 1536], "win_sink": [2, 16], "win_w_o": [2, D, D], "win_b_o": [2, D],
    "glb_w_qkv": [1, D, 1536], "glb_g_q": [1, 64], "glb_g_k": [1, 64], "glb_w_o": [1, D, D],
    "gdn_w_in": [1, D, 4128], "gdn_conv_w": [1, 5, 3072], "gdn_a_log": [1, 2, 8], "gdn_dt_bias": [1, 2, 8],
    "gdn_g_out": [1, 128], "gdn_w_o": [1, D, D],
    "moe_w_router": [4, D, NE], "moe_b_router": [4, NE], "moe_w_up": [4, NE, D, 2 * D], "moe_b_up": [4, NE, 2 * D],
    "moe_w_down": [4, NE, D, D], "moe_b_down": [4, NE, D], "g_final": [D],
}


def host_consts():
    c = {}
    c["ident"] = np.eye(128, dtype=np.float32)
    a = np.arange(128)
    lo = (a[None, :] <= a[:, None]).astype(np.float32)
    hi = (a[:, None] <= a[None, :]).astype(np.float32)
    c["mask_lo"] = np.tile(lo, (1, 4))
    c["mask_hi"] = np.tile(hi, (1, 4))
    t = np.arange(NLAT)
    pos = np.stack([t // 64, t % 64], 0).astype(np.float32)
    inv = (10000.0 ** (-np.arange(0, 32, 2, dtype=np.float32) / 32)).astype(np.float32)
    cosT = np.zeros((64, NLAT), np.float32)
    sinT = np.zeros((64, NLAT), np.float32)
    PT = np.zeros((64, 64), np.float32)
    for d in range(64):
        ax, r = d // 32, d % 32
        half, f = r // 16, r % 16
        ang = (pos[ax] * inv[f]).astype(np.float32)
        cosT[d] = np.cos(ang)
        sinT[d] = np.sin(ang)
        if half == 0:
            PT[d + 16, d] = -1.0
        else:
            PT[d - 16, d] = 1.0
    c["cosT"] = cosT
    c["sinT"] = sinT
    c["PT"] = PT
    idx = np.arange(128)
    ch = idx // 64
    same = ch[:, None] == ch[None, :]
    le = idx[:, None] <= idx[None, :]
    ge = idx[:, None] >= idx[None, :]
    lt = idx[:, None] < idx[None, :]
    gt = idx[:, None] > idx[None, :]
    f32 = np.float32
    c["gd_L0"] = (same & le).astype(f32)
    c["gd_L1"] = (same & ge).astype(f32)
    for d, last in ((0, (63, 127)), (1, (0, 64))):
        lastv = np.array([last[ci] for ci in ch])
        c[f"gd_SelC{d}"] = (idx[:, None] == lastv[None, :]).astype(f32)
        c[f"gd_SelA{d}"] = np.repeat((idx == last[0]).astype(f32)[:, None], 128, 1)
        c[f"gd_SelB{d}"] = np.repeat((idx == last[1]).astype(f32)[:, None], 128, 1)
    c["gd_Ms0"] = (same & gt).astype(f32)
    c["gd_Ms1"] = (same & lt).astype(f32)
    c["gd_MiT0"] = (same & le).astype(f32)
    c["gd_MiT1"] = (same & ge).astype(f32)
    c["gd_rowm"] = np.stack([(idx < 64), (idx >= 64)], 1).astype(f32)
    return c


class K:
    pass


class LazyInputs(dict):
    def __init__(self, nc):
        super().__init__()
        self.nc = nc

    def __missing__(self, n):
        ap = self.nc.dram_tensor(n, INPUT_SHAPES[n], F32, kind="ExternalInput").ap()
        self[n] = ap
        return ap


def load_T(p, k, dst_ap, dst_tile, src_rows_ap, R, src_dep=None, C=128):
    tmp = k.ltmp.next()
    p.dma("sp", tmp[0:R, 0:C], src_rows_ap, reads=[src_dep] if src_dep else (), writes=[tmp])
    ps = p.next_ps()
    p.tr(ps[0:C, 0:R], tmp[0:R, 0:C], k.ident[0:R, 0:R], [tmp, k.ident], [ps])
    p.cp(dst_ap, ps[0:C, 0:R], [ps], [dst_tile])


def bcast_rows(p, k, ps_ap, ps_tile, row_ap, row_tile, start, stop):
    p.mm(ps_ap, k.ones_row[0:1, :], row_ap, start, stop, [k.ones_row, row_tile], [ps_tile])


def stage_init(p, k):
    p.dma("sp", k.X[0:NCTX, :], k.inp["ctx"], reads=(), writes=[k.Xd])
    p.dma("sp", k.X[NCTX:NTOK, :], k.inp["x"], reads=(), writes=[k.Xd])


def stage_mod(p, k, layers):
    p.mark()
    inp = k.inp
    craw = p.sb([128, 16], F32, "craw")
    load_T(p, k, craw[:, 0:8], craw, inp["c"].rearrange("(k q) -> k q", q=128), 8)
    load_T(p, k, craw[:, 8:16], craw, inp["c_ctx"].rearrange("(k q) -> k q", q=128), 8)
    csil = p.sb([128, 16], F32, "csil")
    p.act(csil[:], craw[:], AF.Silu, [craw], [csil])
    ones_bf = p.sb([128, 128], F32, "ones128")
    p.memset(ones_bf[:], 1.0, [ones_bf])
    lhs = p.sb([128, 16, 128], BF16, "modlhs")
    for j in range(16):
        p.ts(lhs[:, j, :], ones_bf[:], csil[:, j:j + 1], None, ALU.mult, None, [ones_bf, csil], [lhs])
    wring = p.ring(2, [128, 8, 512], BF16, "wmod")
    brow = p.ring(2, [1, 512], F32, "bmodrow")
    grow = p.ring(2, [1, 1024], F32, "grow")
    oring = p.ring(3, [128, 512], F32, "modo")
    gb = [p.sb([128, 1024], F32, "gb0"), p.sb([128, 1024], F32, "gb1")]
    for l in layers:
        for gi, gname in enumerate(("g_mix", "g_ffn")):
            gr = grow.next()
            p.dma("sp", gr[0:1, :], inp[gname][l:l + 1, :], writes=[gr])
            for nh in range(2):
                ps = p.next_ps()
                bcast_rows(p, k, ps[:, :], ps, gr[0:1, nh * 512:(nh + 1) * 512], gr, True, True)
                p.cp(gb[gi][:, nh * 512:(nh + 1) * 512], ps[:, :], [ps], [gb[gi]], eng="act")
        for j in range(6):
            for nh in range(2):
                n0 = j * 1024 + nh * 512
                wt = wring.next()
                p.dma("pool", wt[:, :, :], inp["w_mod"][l].rearrange("(k q) n -> q k n", q=128)[:, :, n0:n0 + 512], writes=[wt])
                br = brow.next()
                p.dma("sp", br[0:1, :], inp["b_mod"][l:l + 1, n0:n0 + 512], writes=[br])
                for s in range(2):
                    ps = p.next_ps()
                    for kk in range(8):
                        p.mm(ps[:, :], lhs[:, s * 8 + kk, :], wt[:, kk, :], kk == 0, False, [lhs, wt], [ps])
                    bcast_rows(p, k, ps[:, :], ps, br[0:1, :], br, False, True)
                    o = oring.next()
                    if j in (1, 4):
                        g = gb[0] if j == 1 else gb[1]
                        p.stt(o[:], ps[:, :], 1.0, g[:, nh * 512:(nh + 1) * 512], ALU.add, ALU.mult, [ps, g], [o])
                    else:
                        p.cp(o[:], ps[:, :], [ps], [o], eng="act")
                    p.dma("sp", k.MODS[l, s, j, :, nh * 512:(nh + 1) * 512], o[:], reads=[o], writes=[k.MODSd])
    p.release()


def rstd_of(p, k, ss, n, eps, R, W):
    p.ts(ss, ss, 1.0 / n, eps, ALU.mult, ALU.add, R, W)
    p.recip(ss, ss, R, W)
    p.act(ss, ss, AF.Sqrt, R, W)


def stage_norm(p, k, l, which, tiles, hT, router=None, p32=None):
    p.mark()
    jsh, jA = (0, 1) if which == 0 else (3, 4)
    modt = {}
    for s in (0, 1):
        a = p.sb([128, 1024], F32, "modA")
        b = p.sb([128, 1024], F32, "modS")
        p.dma("sp", a[:], k.MODS[l, s, jA], reads=[k.MODSd], writes=[a])
        p.dma("sp", b[:], k.MODS[l, s, jsh], reads=[k.MODSd], writes=[b])
        modt[s] = (a, b)
    xr = p.ring(2, [128, 1024], F32, "xn")
    hr = p.ring(2, [128, 1024], F32, "hn")
    sqr = p.sb([128, 1024], F32, "sq")
    ssr = p.ring(2, [128, 1], F32, "ss")
    need32 = router is not None or p32 is not None
    if need32:
        h32r = p.ring(2, [128, 8, 128], F32, "h32")
    if router is not None:
        wr = p.sb([128, 8, NE], F32, "wr")
        for kk in range(8):
            p.dma("sp", wr[:, kk, :], k.inp["moe_w_router"][l, kk * 128:(kk + 1) * 128, :], writes=[wr])
        brr = p.sb([1, NE], F32, "brr")
        p.dma("sp", brr[0:1, :], k.inp["moe_b_router"][l:l + 1, :], writes=[brr])
        lgr = p.ring(2, [128, NE], F32, "lg")
        m8r = p.ring(2, [128, 8], F32, "m8")
        er = p.ring(2, [128, NE], F32, "eg")
        smr = p.ring(2, [128, 1], F32, "sm")
    for tt in tiles:
        s = 1 if tt < 2 else 0
        A, S = modt[s]
        x = xr.next()
        p.dma("sp", x[:], k.X[tt * 128:(tt + 1) * 128, :], reads=[k.Xd], writes=[x])
        ss = ssr.next()
        p.act(sqr[:], x[:], AF.Square, [x], [sqr, ss], accum_out=ss[:])
        rstd_of(p, k, ss[:], D, 1e-6, [ss], [ss])
        h = hr.next()
        p.stt(h[:], x[:], ss[:, 0:1], A[:], ALU.mult, ALU.mult, [x, ss, A], [h])
        p.tt(h[:], h[:], S[:], ALU.add, [h, S], [h])
        if need32:
            h32 = h32r.next()
        for half in range(2):
            ps = p.next_ps()
            for q in range(4):
                kk = half * 4 + q
                p.tr(ps[:, q * 128:(q + 1) * 128], h[:, kk * 128:(kk + 1) * 128], k.ident[:], [h, k.ident], [ps])
            for q in range(4):
                if need32:
                    p.cp(h32[:, half * 4 + q, :], ps[:, q * 128:(q + 1) * 128], [ps], [h32], eng="act")
                    p.cp(hT[:, half * 4 + q, tt * 128:(tt + 1) * 128], h32[:, half * 4 + q, :], [h32], [hT])
                else:
                    p.cp(hT[:, half * 4 + q, tt * 128:(tt + 1) * 128], ps[:, q * 128:(q + 1) * 128], [ps], [hT], eng="act")
        if p32 is not None:
            ps = p.next_ps()
            n = p32["n"]
            for kk in range(8):
                p.mm(ps[:, 0:n], h32[:, kk, :], p32["w"][:, kk, :], kk == 0, kk == 7, [h32, p32["w"]], [ps])
            p32["cb"](tt, ps)
        if router is not None:
            G = router["G"]
            ps = p.next_ps()
            for kk in range(8):
                p.mm(ps[:, 0:NE], h32[:, kk, :], wr[:, kk, :], kk == 0, False, [h32, wr], [ps])
            bcast_rows(p, k, ps[:, 0:NE], ps, brr[0:1, :], brr, False, True)
            lg = lgr.next()
            p.cp(lg[:], ps[:, 0:NE], [ps], [lg])
            m8 = m8r.next()
            p.op("dve", lambda e, m8=m8, lg=lg: e.max(out=m8[:], in_=lg[:]), [lg], [m8])
            nm = smr.next()
            p.ts(nm[:], m8[:, 0:1], -1.0, None, ALU.mult, None, [m8], [nm])
            eg = er.next()
            p.act(eg[:], lg[:], AF.Exp, [lg, nm], [eg], bias=nm[:, 0:1], scale=1.0)
            sm = smr.next()
            p.stt(eg[:], lg[:], m8[:, 3:4], eg[:], ALU.is_ge, ALU.mult, [lg, m8, eg], [eg])
            p.op("dve", lambda e, sm=sm, eg=eg: e.reduce_sum(out=sm[:], in_=eg[:], axis=AX.X), [eg], [sm])
            p.recip(sm[:], sm[:], [sm], [sm])
            p.ts(G[:, tt, :], eg[:], sm[:, 0:1], None, ALU.mult, None, [eg, sm], [G])
    p.release()


def stage_moe(p, k, l, tiles, hT, G):
    p.mark()
    inp = k.inp
    t0 = tiles[0]
    ntile = len(tiles)
    acc = p.sb([128, ntile, 1024], F32, "acc")
    bupT = p.sb([128, NE * 16], F32, "bupT")
    bsrc = inp["moe_b_up"][l].rearrange("e (m q) -> (e m) q", q=128)
    for i in range(4):
        load_T(p, k, bupT[:, i * 128:(i + 1) * 128], bupT, bsrc[i * 128:(i + 1) * 128, :], 128)
    bd = p.sb([NE, 1024], F32, "bd")
    p.dma("sp", bd[:], inp["moe_b_down"][l], writes=[bd])
    gtr = p.ring(2, [NE, 128], F32, "GT")
    for ti, tt in enumerate(tiles):
        ps = p.next_ps()
        p.tr(ps[0:NE, 0:128], G[:, tt, :], k.ident[:], [G, k.ident], [ps])
        gt = gtr.next()
        p.cp(gt[:], ps[0:NE, 0:128], [ps], [gt])
        for nh in range(2):
            ps2 = p.next_ps()
            p.mm(ps2[:, :], gt[:, :], bd[:, nh * 512:(nh + 1) * 512], True, True, [gt, bd], [ps2])
            p.cp(acc[:, ti, nh * 512:(nh + 1) * 512], ps2[:, :], [ps2], [acc], eng="act")
    blocks = []
    i = 0
    while i < ntile:
        n = min(4, ntile - i)
        blocks.append((i, n))
        i += n
    wur = p.ring(2, [128, 8, 2, 512], BF16, "wu")
    wdr = p.ring(2, [128, 4, 1024], BF16, "wd")
    aTr = [p.ring(2, [128, 512], BF16, f"aT{m}") for m in range(4)]
    t1r = p.ring(2, [128, 512], F32, "t1")
    sgr = p.ring(2, [128, 512], F32, "sg")
    t2r = p.ring(2, [128, 512], F32, "t2")
    accd = [[Dep(f"acc{ti}_{nh}") for nh in range(2)] for ti in range(ntile)]
    for ti in range(ntile):
        for nh in range(2):
            accd[ti][nh].w = acc.dep.w
    for e in range(NE):
        for half in range(2):
            wu = wur.next()
            wd = wdr.next()
            src = inp["moe_w_up"][l, e].rearrange("r (g h c) -> r g h c", g=2, h=2)
            for kk in range(8):
                p.dma("pool", wu[:, kk, :, :], src[kk * 128:(kk + 1) * 128, :, half, :], writes=[wu])
            for kk in range(4):
                r0 = half * 512 + kk * 128
                p.dma("pool", wd[:, kk, :], inp["moe_w_down"][l, e, r0:r0 + 128, :], writes=[wd])
            for (b0, bn) in blocks:
                tok0 = (t0 + b0) * 128
                ntok = bn * 128
                aT = []
                for m in range(4):
                    pa = p.next_ps()
                    for kk in range(8):
                        p.mm(pa[:, 0:ntok], wu[:, kk, 0, m * 128:(m + 1) * 128], hT[:, kk, tok0:tok0 + ntok], kk == 0, kk == 7, [wu, hT], [pa])
                    pb = p.next_ps()
                    for kk in range(8):
                        p.mm(pb[:, 0:ntok], wu[:, kk, 1, m * 128:(m + 1) * 128], hT[:, kk, tok0:tok0 + ntok], kk == 0, kk == 7, [wu, hT], [pb])
                    ca = e * 16 + half * 4 + m
                    cb = ca + 8
                    t1 = t1r.next()
                    p.ts(t1[:, 0:ntok], pa[:, 0:ntok], bupT[:, ca:ca + 1], 7.0, ALU.add, ALU.min, [pa, bupT], [t1])
                    sg = sgr.next()
                    p.act(sg[:, 0:ntok], t1[:, 0:ntok], AF.Sigmoid, [t1], [sg], scale=1.702)
                    t2 = t2r.next()
                    p.act(t2[:, 0:ntok], pb[:, 0:ntok], AF.Identity, [pb, bupT], [t2], bias=bupT[:, cb:cb + 1], scale=1.0)
                    p.ts(t2[:, 0:ntok], t2[:, 0:ntok], -7.0, 7.0, ALU.max, ALU.min, [t2], [t2])
                    p.tt(t1[:, 0:ntok], t1[:, 0:ntok], sg[:, 0:ntok], ALU.mult, [t1, sg], [t1])
                    a = aTr[m].next()
                    p.stt(a[:, 0:ntok], t2[:, 0:ntok], 1.0, t1[:, 0:ntok], ALU.add, ALU.mult, [t2, t1], [a])
                    aT.append(a)
                for j in range(bn):
                    ti = b0 + j
                    for nh in range(2):
                        po = p.next_ps()
                        for kk in range(4):
                            p.mm(po[:, :], aT[kk][:, j * 128:(j + 1) * 128], wd[:, kk, nh * 512:(nh + 1) * 512], kk == 0, kk == 3, [aT[kk], wd], [po])
                        av = acc[:, ti, nh * 512:(nh + 1) * 512]
                        p.stt(av, po[:, :], G[:, t0 + ti, e:e + 1], av, ALU.mult, ALU.add, [po, G, accd[ti][nh]], [accd[ti][nh]])
    gts = {}
    for s in (0, 1):
        g = p.sb([128, 1024], F32, "gt2")
        p.dma("sp", g[:], k.MODS[l, s, 5], reads=[k.MODSd], writes=[g])
        gts[s] = g
    xr = p.ring(2, [128, 1024], F32, "xres")
    for ti, tt in enumerate(tiles):
        s = 1 if tt < 2 else 0
        x = xr.next()
        p.dma("sp", x[:], k.X[tt * 128:(tt + 1) * 128, :], reads=[k.Xd], writes=[x])
        p.tt(acc[:, ti, :], acc[:, ti, :], gts[s][:], ALU.mult, [accd[ti][0], accd[ti][1], gts[s]], [accd[ti][0], accd[ti][1]])
        p.tt(x[:], x[:], acc[:, ti, :], ALU.add, [x, accd[ti][0], accd[ti][1]], [x])
        p.dma("sp", k.X[tt * 128:(tt + 1) * 128, :], x[:], reads=[x], writes=[k.Xd])
    p.release()


def stage_attn(p, k, l, kind, j, need_ctx):
    inp = k.inp
    wname = "win" if kind == 0 else "glb"
    wqkv = inp[wname + "_w_qkv"][j]
    p.mark()
    qT = p.sb([64, 16, NTOK], BF16, "qT")
    qTd = [[Dep(f"qT{t}_{g}") for g in range(4)] for t in range(NT)]
    kT = p.sb([64, 4, NTOK], BF16, "kT")
    V = p.sb([128, NT, 256], BF16, "V")
    cosT = p.sb([64, NLAT], F32, "cosT")
    sinT = p.sb([64, NLAT], F32, "sinT")
    PT = p.sb([64, 64], F32, "PT")
    p.dma("sp", cosT[:], k.cin["cosT"], writes=[cosT])
    p.dma("sp", sinT[:], k.cin["sinT"], writes=[sinT])
    p.dma("sp", PT[:], k.cin["PT"], writes=[PT])
    ones_bf = p.sb([128, 64], BF16, "ones_bf")
    p.memset(ones_bf[:], 1.0, [ones_bf])
    p.mark()
    hT = p.sb([128, 8, NTOK], BF16, "hT")
    stage_norm(p, k, l, 0, list(range(NT)), hT)
    wq = p.sb([128, 8, 1024], BF16, "wq")
    wk = p.sb([128, 8, 256], BF16, "wk")
    wv = p.sb([128, 8, 256], BF16, "wv")
    for kk in range(8):
        p.dma("pool", wq[:, kk, :], wqkv[kk * 128:(kk + 1) * 128, 0:1024], writes=[wq])
        p.dma("pool", wk[:, kk, :], wqkv[kk * 128:(kk + 1) * 128, 1024:1280], writes=[wk])
        p.dma("pool", wv[:, kk, :], wqkv[kk * 128:(kk + 1) * 128, 1280:1536], writes=[wv])
    if kind == 0:
        bT = p.sb([64, 24], F32, "bT")
        load_T(p, k, bT[:, :], bT, inp["win_b_qkv"][j].rearrange("(h d) -> h d", d=64), 24, C=64)
        bvrow = p.sb([1, 256], F32, "bvrow")
        p.dma("sp", bvrow[0:1, :], inp["win_b_qkv"][j:j + 1, 1280:1536], writes=[bvrow])
    else:
        gqk = p.sb([64, 2], F32, "gqk")
        load_T(p, k, gqk[:, 0:1], gqk, inp["glb_g_q"][j:j + 1, :], 1, C=64)
        load_T(p, k, gqk[:, 1:2], gqk, inp["glb_g_k"][j:j + 1, :], 1, C=64)
        avg64 = p.sb([64, 64], F32, "avg64")
        p.memset(avg64[:], 1.0 / 64, [avg64])
    q32r = p.ring(2, [64, 512], F32, "q32")
    sqr = p.ring(2, [64, 512], F32, "qsq")
    rsr = p.ring(2, [64, 512], F32, "qrs")
    tr_ = p.ring(2, [64, 512], F32, "qt")
    blocks = [(0, 256), (256, 512), (768, 512), (1280, 512), (1792, 512)]
    for hh in range(20):
        isq = hh < 16
        for (t0, n) in blocks:
            ps = p.next_ps()
            for kk in range(8):
                lw = wq[:, kk, hh * 64:(hh + 1) * 64] if isq else wk[:, kk, (hh - 16) * 64:(hh - 15) * 64]
                p.mm(ps[0:64, 0:n], lw, hT[:, kk, t0:t0 + n], kk == 0, kk == 7, [wq if isq else wk, hT], [ps])
            q32 = q32r.next()
            if kind == 0:
                p.act(q32[:, 0:n], ps[0:64, 0:n], AF.Identity, [ps, bT], [q32], bias=bT[:, hh:hh + 1], scale=1.0)
            else:
                p.cp(q32[:, 0:n], ps[0:64, 0:n], [ps], [q32], eng="act")
                sq = sqr.next()
                p.act(sq[:, 0:n], q32[:, 0:n], AF.Square, [q32], [sq])
                psn = p.next_ps()
                p.mm(psn[0:64, 0:n], avg64[:, :], sq[:, 0:n], True, True, [avg64, sq], [psn])
                rs = rsr.next()
                p.ts(rs[:, 0:n], psn[0:64, 0:n], 1e-6, None, ALU.add, None, [psn], [rs])
                p.recip(rs[:, 0:n], rs[:, 0:n], [rs], [rs])
                p.act(rs[:, 0:n], rs[:, 0:n], AF.Sqrt, [rs], [rs])
                gcol = gqk[:, 0:1] if isq else gqk[:, 1:2]
                p.stt(q32[:, 0:n], q32[:, 0:n], gcol, rs[:, 0:n], ALU.mult, ALU.mult, [q32, gqk, rs], [q32])
            dst = qT[:, hh, t0:t0 + n] if isq else kT[:, hh - 16, t0:t0 + n]
            dW = [qTd[t][hh // 4] for t in range(t0 // 128, (t0 + n) // 128)] if isq else [kT]
            if t0 == 0:
                p.cp(dst, q32[:, 0:n], [q32], dW)
            else:
                l0 = t0 - NCTX
                ps2 = p.next_ps()
                p.mm(ps2[0:64, 0:n], PT[:, :], q32[:, 0:n], True, True, [PT, q32], [ps2])
                t = tr_.next()
                p.tt(t[:, 0:n], ps2[0:64, 0:n], sinT[:, l0:l0 + n], ALU.mult, [ps2, sinT], [t])
                p.tt(q32[:, 0:n], q32[:, 0:n], cosT[:, l0:l0 + n], ALU.mult, [q32, cosT], [q32])
                p.tt(dst, q32[:, 0:n], t[:, 0:n], ALU.add, [q32, t], dW)
    for tt in range(NT):
        ps = p.next_ps()
        for kk in range(8):
            p.mm(ps[:, 0:256], hT[:, kk, tt * 128:(tt + 1) * 128], wv[:, kk, :], kk == 0, (kk == 7 and kind == 1), [hT, wv], [ps])
        if kind == 0:
            bcast_rows(p, k, ps[:, 0:256], ps, bvrow[0:1, :], bvrow, False, True)
        p.cp(V[:, tt, :], ps[:, 0:256], [ps], [V], eng="act")
    p.release()
    p.mark()
    if kind == 0:
        mlo = p.sb([128, 512], BF16, "mlo")
        mhi = p.sb([128, 512], BF16, "mhi")
        p.dma("pool", mlo[:], k.cin["mask_lo"], writes=[mlo])
        p.dma("pool", mhi[:], k.cin["mask_hi"], writes=[mhi])
        srow = p.sb([1, 16], F32, "srow")
        p.dma("sp", srow[0:1, :], inp["win_sink"][j:j + 1, :], writes=[srow])
        p.act(srow[0:1, :], srow[0:1, :], AF.Exp, [srow], [srow])
        sinkexp = p.sb([64, 16], F32, "sinkexp")
        ps = p.next_ps()
        p.mm(ps[0:64, 0:16], k.ones_row[0:1, 0:64], srow[0:1, :], True, True, [k.ones_row, srow], [ps])
        p.cp(sinkexp[:], ps[0:64, 0:16], [ps], [sinkexp])
    wo = p.sb([64, 16, 1024], BF16, "wo")
    wsrc = inp[wname + "_w_o"][j].rearrange("(h d) n -> d h n", d=64)
    for h in range(16):
        p.dma("pool", wo[:, h, :], wsrc[:, h, :], writes=[wo])
    ptr = p.ring(4, [128, 512], BF16, "Pt")
    rdr = p.ring(2, [64, 512], F32, "rd")
    qtiles = list(range(2, NT)) + ([0, 1] if need_ctx else [])
    its = []
    acc_i = 0
    for qt in qtiles:
        if qt < 2:
            keys = [0, 1]
        elif kind == 1:
            keys = list(range(NT))
        else:
            keys = [0, 1] + [kt for kt in (qt - 1, qt, qt + 1) if 2 <= kt < NT]
        for g in range(4):
            acc = (k.psx[(acc_i % 2) * 2], k.psx[(acc_i % 2) * 2 + 1])
            acc_i += 1
            for ki, kt in enumerate(keys):
                its.append((qt, g, ki, kt, len(keys), acc))

    def stage1(it):
        qt, g, ki, kt, nk, acc = it
        rhs = qT[:, 4 * g:4 * g + 4, qt * 128:(qt + 1) * 128]
        psS = p.next_ps()
        p.mm(psS[:, :].rearrange("p (a b) -> p a b", a=4), kT[:, g, kt * 128:(kt + 1) * 128], rhs, True, True, [kT, qTd[qt][g]], [psS])
        pt = ptr.next()
        p.act(pt[:], psS[:, :], AF.Exp, [psS], [pt], scale=0.125)
        if kind == 0 and qt >= 2 and kt >= 2 and kt == qt - 1:
            p.tt(pt[:], pt[:], mlo[:], ALU.mult, [pt, mlo], [pt])
        elif kind == 0 and qt >= 2 and kt >= 2 and kt == qt + 1:
            p.tt(pt[:], pt[:], mhi[:], ALU.mult, [pt, mhi], [pt])
        return pt

    def stage2(it, pt):
        qt, g, ki, kt, nk, (psO, psD) = it
        first, last = ki == 0, ki == nk - 1
        p.mm(psO[0:64, :], V[:, kt, g * 64:(g + 1) * 64], pt[:], first, last, [V, pt], [psO])
        p.mm(psD[0:64, :], ones_bf[:, :], pt[:], first, last, [ones_bf, pt], [psD])
        if not last:
            return
        rhs = qT[:, 4 * g:4 * g + 4, qt * 128:(qt + 1) * 128]
        rd = rdr.next()
        if kind == 0:
            for a in range(4):
                h = 4 * g + a
                p.ts(rd[:, a * 128:(a + 1) * 128], psD[0:64, a * 128:(a + 1) * 128], sinkexp[:, h:h + 1], None, ALU.add, None, [psD, sinkexp], [rd])
            p.recip(rd[:], rd[:], [rd], [rd])
        else:
            p.recip(rd[:], psD[0:64, :], [psD], [rd])
        p.tt(rhs, psO[0:64, :].rearrange("p (a b) -> p a b", a=4), rd[:, :].rearrange("p (a b) -> p a b", a=4), ALU.mult, [psO, rd], [qTd[qt][g]])

    prev = None
    for it in its:
        pt = stage1(it)
        if prev is not None:
            stage2(*prev)
        prev = (it, pt)
    stage2(*prev)
    gts = {}
    for s_ in ((0, 1) if need_ctx else (0,)):
        g_ = p.sb([128, 1024], F32, "gt1")
        p.dma("sp", g_[:], k.MODS[l, s_, 2], reads=[k.MODSd], writes=[g_])
        gts[s_] = g_
    if kind == 0:
        borow = p.sb([1, 1024], F32, "borow")
        p.dma("sp", borow[0:1, :], inp["win_b_o"][j:j + 1, :], writes=[borow])
    xr = p.ring(2, [128, 1024], F32, "xo")
    yr = p.ring(2, [128, 512], F32, "yo")
    for qt in qtiles:
        s_ = 1 if qt < 2 else 0
        x = xr.next()
        p.dma("sp", x[:], k.X[qt * 128:(qt + 1) * 128, :], reads=[k.Xd], writes=[x])
        for nh in range(2):
            ps = p.next_ps()
            for h in range(16):
                p.mm(ps[:, :], qT[:, h, qt * 128:(qt + 1) * 128], wo[:, h, nh * 512:(nh + 1) * 512], h == 0, (h == 15 and kind == 1), [qTd[qt][h // 4], wo], [ps])
            if kind == 0:
                bcast_rows(p, k, ps[:, :], ps, borow[0:1, nh * 512:(nh + 1) * 512], borow, False, True)
            y = yr.next()
            p.tt(y[:], ps[:, :], gts[s_][:, nh * 512:(nh + 1) * 512], ALU.mult, [ps, gts[s_]], [y])
            p.tt(x[:, nh * 512:(nh + 1) * 512], x[:, nh * 512:(nh + 1) * 512], y[:], ALU.add, [x, y], [x])
        p.dma("sp", k.X[qt * 128:(qt + 1) * 128, :], x[:], reads=[x], writes=[k.Xd])
    p.release()
    p.release()


def stage_gdn(p, k, l, j, need_ctx):
    inp = k.inp
    nc = k.nc
    w_in = inp["gdn_w_in"][j]
    if not hasattr(k, "gd"):
        k.gd = {n: (nc.dram_tensor("gd_" + n, shp, F32).ap(), Dep("gd_" + n)) for n, shp in (
            ("QT", [8, 128, NTOK]), ("KT", [8, 128, NTOK]), ("Kt", [NTOK, 8, 128]), ("Vt", [NTOK, 8, 128]),
            ("Z", [NTOK, 1024]), ("O0", [NTOK, 1024]), ("O1", [NTOK, 1024]))}
    gd = k.gd
    p.mark()
    BG = p.sb([128, NT, 32], F32, "BG")
    p.mark()
    hT = p.sb([128, 8, NTOK], BF16, "hT")
    wbg = p.sb([128, 8, 32], F32, "wbg")
    for kk in range(8):
        p.dma("sp", wbg[:, kk, :], w_in[kk * 128:(kk + 1) * 128, 4096:4128], writes=[wbg])
    r2 = p.sb([1, 32], F32, "r2")
    p.dma("sp", r2[0:1, 0:16], inp["gdn_dt_bias"][j].rearrange("a b -> (a b)").rearrange("(o n) -> o n", o=1), writes=[r2])
    p.dma("sp", r2[0:1, 16:32], inp["gdn_a_log"][j].rearrange("a b -> (a b)").rearrange("(o n) -> o n", o=1), writes=[r2])
    p.act(r2[0:1, 16:32], r2[0:1, 16:32], AF.Exp, [r2], [r2])
    p.ts(r2[0:1, 16:32], r2[0:1, 16:32], -1.0, None, ALU.mult, None, [r2], [r2])
    dtb = p.sb([128, 32], F32, "dtb")
    ps = p.next_ps()
    bcast_rows(p, k, ps[:, 0:32], ps, r2[0:1, :], r2, True, True)
    p.cp(dtb[:], ps[:, 0:32], [ps], [dtb])
    tmpr = p.ring(2, [128, 32], F32, "bgtmp")

    def cb(tt, ps):
        t = tmpr.next()
        p.cp(t[:], ps[:, 0:32], [ps], [t], eng="act")
        p.act(BG[:, tt, 0:16], t[:, 0:16], AF.Sigmoid, [t], [BG])
        p.tt(t[:, 16:32], t[:, 16:32], dtb[:, 0:16], ALU.add, [t, dtb], [t])
        p.act(t[:, 16:32], t[:, 16:32], AF.Exp, [t], [t])
        p.act(t[:, 16:32], t[:, 16:32], AF.Ln, [t], [t], bias=1.0, scale=1.0)
        p.tt(BG[:, tt, 16:32], t[:, 16:32], dtb[:, 16:32], ALU.mult, [t, dtb], [BG])

    stage_norm(p, k, l, 0, list(range(NT)), hT, p32={"w": wbg, "n": 32, "cb": cb})
    cw = p.sb([128, 24 * 5], F32, "cw")
    for c in range(24):
        load_T(p, k, cw[:, c * 5:(c + 1) * 5], cw, inp["gdn_conv_w"][j][:, c * 128:(c + 1) * 128], 5)
    ones128 = p.sb([128, 128], F32, "ones128")
    p.memset(ones128[:], 1.0, [ones128])
    pcr = p.ring(2, [128, 260], F32, "pc")
    plr = p.ring(2, [128, 2052], F32, "pl")
    for t_ in pcr.tiles + plr.tiles:
        p.memset(t_[:], 0.0, [t_])
    ycr = p.ring(2, [128, 256], F32, "yc")
    ylr = p.ring(2, [128, 2048], F32, "yl")
    wcr = p.ring(2, [128, 8, 128], BF16, "wc")
    sqr = p.ring(2, [128, 512], F32, "gsq")
    rnr = p.ring(2, [128, 512], F32, "grn")
    tkr = p.ring(3, [128, 128], F32, "tok")
    for c in range(24):
        wc = wcr.next()
        for kk in range(8):
            p.dma("pool", wc[:, kk, :], w_in[kk * 128:(kk + 1) * 128, c * 128:(c + 1) * 128], writes=[wc])
        for (t0, n, buf, y) in ((0, 256, pcr.next(), ycr.next()), (256, 2048, plr.next(), ylr.next())):
            for blk in range(0, n, 512):
                nb = min(512, n - blk)
                ps = p.next_ps()
                for kk in range(8):
                    p.mm(ps[:, 0:nb], wc[:, kk, :], hT[:, kk, t0 + blk:t0 + blk + nb], kk == 0, kk == 7, [wc, hT], [ps])
                p.cp(buf[:, 2 + blk:2 + blk + nb], ps[:, 0:nb], [ps], [buf], eng="act")
            p.ts(y[:, 0:n], buf[:, 0:n], cw[:, c * 5:c * 5 + 1], None, ALU.mult, None, [buf, cw], [y])
            for jj in range(1, 5):
                p.stt(y[:, 0:n], buf[:, jj:jj + n], cw[:, c * 5 + jj:c * 5 + jj + 1], y[:, 0:n], ALU.mult, ALU.add, [buf, cw, y], [y])
            p.act(y[:, 0:n], y[:, 0:n], AF.Silu, [y], [y])
            if c < 16:
                scale = (128.0 ** -0.5) if c < 8 else 1.0
                for blk in range(0, n, 512):
                    nb = min(512, n - blk)
                    sq = sqr.next()
                    p.act(sq[:, 0:nb], y[:, blk:blk + nb], AF.Square, [y], [sq])
                    ps = p.next_ps()
                    p.mm(ps[:, 0:nb], ones128[:, :], sq[:, 0:nb], True, True, [ones128, sq], [ps])
                    rn = rnr.next()
                    p.ts(rn[:, 0:nb], ps[:, 0:nb], 1e-6, None, ALU.add, None, [ps], [rn])
                    p.recip(rn[:, 0:nb], rn[:, 0:nb], [rn], [rn])
                    p.act(rn[:, 0:nb], rn[:, 0:nb], AF.Sqrt, [rn], [rn])
                    p.stt(y[:, blk:blk + nb], y[:, blk:blk + nb], scale, rn[:, 0:nb], ALU.mult, ALU.mult, [y, rn], [y])
                dst = gd["QT"] if c < 8 else gd["KT"]
                p.dma("sp", dst[0][c % 8, :, t0:t0 + n], y[:, 0:n], reads=[y], writes=[dst[1]])
            if c >= 8:
                dst = gd["Kt"] if c < 16 else gd["Vt"]
                for ti in range(n // 128):
                    ps = p.next_ps()
                    p.tr(ps[:, 0:128], y[:, ti * 128:(ti + 1) * 128], k.ident[:], [y, k.ident], [ps])
                    tk = tkr.next()
                    p.cp(tk[:], ps[:, 0:128], [ps], [tk], eng="act")
                    r0 = t0 + ti * 128
                    p.dma("sp", dst[0][r0:r0 + 128, c % 8, :], tk[:], reads=[tk], writes=[dst[1]])
    wz = p.sb([128, 8, 1024], BF16, "wz")
    for kk in range(8):
        p.dma("pool", wz[:, kk, :], w_in[kk * 128:(kk + 1) * 128, 3072:4096], writes=[wz])
    ztr = p.ring(2, [128, 1024], F32, "zt")
    for tt in range(NT):
        zt = ztr.next()
        for nh in range(2):
            ps = p.next_ps()
            for kk in range(8):
                p.mm(ps[:, :], hT[:, kk, tt * 128:(tt + 1) * 128], wz[:, kk, nh * 512:(nh + 1) * 512], kk == 0, kk == 7, [hT, wz], [ps])
            p.act(zt[:, nh * 512:(nh + 1) * 512], ps[:, :], AF.Silu, [ps], [zt])
        p.dma("sp", gd["Z"][0][tt * 128:(tt + 1) * 128, :], zt[:], reads=[zt], writes=[gd["Z"][1]])
    p.release()
    p.mark()
    cn = {}
    for n_ in ("L0", "L1", "SelC0", "SelC1", "SelA0", "SelA1", "SelB0", "SelB1", "Ms0", "Ms1", "MiT0", "MiT1"):
        t_ = p.sb([128, 128], F32, "c" + n_)
        p.dma("sp", t_[:], k.cin["gd_" + n_], writes=[t_])
        cn[n_] = t_
    rowm = p.sb([128, 2], F32, "rowm")
    p.dma("sp", rowm[:], k.cin["gd_rowm"], writes=[rowm])
    ones128 = p.sb([128, 128], F32, "ones128b")
    p.memset(ones128[:], 1.0, [ones128])
    ident = k.ident
    S = [[p.sb([128, 128], F32, f"S{d}{h}") for h in range(8)] for d in range(2)]
    for d in range(2):
        for h in range(8):
            p.memset(S[d][h][:], 0.0, [S[d][h]], eng="pool")
    H = range(8)
    VH = [(d, h) for h in range(8) for d in range(2)]

    def mk(name):
        return {vh: p.sb([128, 128], F32, f"{name}{vh[0]}{vh[1]}") for vh in VH}
    kTh, qTh, kh, vh_ = mk("kTh"), mk("qTh"), mk("kh"), mk("vh")
    dg, E1, E2, Bm, iT = mk("dg"), mk("E1"), mk("E2"), mk("Bm"), mk("iT")
    Pa, Pb, PTa, PTb, RT = mk("Pa"), mk("Pb"), mk("PTa"), mk("PTb"), mk("RT")
    qs1, qs2, osb = mk("qs1"), mk("qs2"), mk("osb")
    vb, kbg, kdF, kdS, u, wT, v1, v2 = Bm, Pb, PTa, PTb, dg, E1, E2, Pa
    scr = p.ring(4, [128, 64], F32, "gsc")
    order = [list(range(NT)), [1, 0] + list(range(NT - 1, 1, -1))]
    rows = [slice(0, 64), slice(64, 128)]
    for s_ in range(NT):
        tts = [order[0][s_], order[1][s_]]
        fis = [0, 1]
        ses = [1, 0]
        scs = [scr.next(), scr.next()]
        for d in range(2):
            tt, sc, fi, se = tts[d], scs[d], fis[d], ses[d]
            ps = p.next_ps()
            p.mm(ps[:, 0:8], cn[f"L{d}"][:, :], BG[:, tt, 16 + d * 8:24 + d * 8], True, True, [cn[f"L{d}"], BG], [ps])
            p.cp(sc[:, 0:8], ps[:, 0:8], [ps], [sc])
            ps = p.next_ps()
            for ci, nm_ in enumerate(("SelC", "SelA", "SelB")):
                p.mm(ps[:, ci * 8:(ci + 1) * 8], cn[f"{nm_}{d}"][:, :], sc[:, 0:8], True, True, [cn[f"{nm_}{d}"], sc], [ps])
            p.cp(sc[:, 8:32], ps[:, 0:24], [ps], [sc])
            p.act(sc[:, 32:40], sc[:, 0:8], AF.Exp, [sc], [sc])
            p.tt(sc[:, 40:48], sc[:, 8:16], sc[:, 0:8], ALU.subtract, [sc], [sc])
            p.act(sc[:, 40:48], sc[:, 40:48], AF.Exp, [sc], [sc])
            p.act(sc[:, 16:32], sc[:, 16:32], AF.Exp, [sc], [sc])
            p.ts(sc[:, 48:56], sc[:, 40:48], rowm[:, se:se + 1], None, ALU.mult, None, [sc, rowm], [sc])
            p.ts(sc[:, 40:48], sc[:, 40:48], rowm[:, fi:fi + 1], None, ALU.mult, None, [sc, rowm], [sc])
            p.ts(sc[:, 56:64], BG[:, tt, d * 8:d * 8 + 8], -1.0, None, ALU.mult, None, [BG], [sc])
            p.tt(sc[:, 8:16], BG[:, tt, d * 8:d * 8 + 8], sc[:, 32:40], ALU.mult, [BG, sc], [sc])
        for (d, h) in VH:
            vh = (d, h)
            r0 = tts[d] * 128
            p.dma("sp", kTh[vh][:], gd["KT"][0][h, :, r0:r0 + 128], reads=[gd["KT"][1]], writes=[kTh[vh]])
            p.dma("sp", qTh[vh][:], gd["QT"][0][h, :, r0:r0 + 128], reads=[gd["QT"][1]], writes=[qTh[vh]])
            p.dma("sp", kh[vh][:], gd["Kt"][0][r0:r0 + 128, h, :], reads=[gd["Kt"][1]], writes=[kh[vh]])
            p.dma("sp", vh_[vh][:], gd["Vt"][0][r0:r0 + 128, h, :], reads=[gd["Vt"][1]], writes=[vh_[vh]])
        for (d, h) in VH:
            vh = (d, h)
            sc = scs[d]
            gcs = sc[:, h:h + 1]
            p.ts(dg[vh][:], ident[:], gcs, None, ALU.mult, None, [ident, sc], [dg[vh]], eng="pool")
            psKK = p.next_ps()
            p.mm(psKK[:, 0:128], kTh[vh][:, :], kTh[vh][:, :], True, True, [kTh[vh]], [psKK])
            psKQ = p.next_ps()
            p.mm(psKQ[:, 0:128], kTh[vh][:, :], qTh[vh][:, :], True, True, [kTh[vh], qTh[vh]], [psKQ])
            psBc = p.next_ps()
            p.mm(psBc[:, 0:128], ones128[:, :], dg[vh][:, :], True, True, [ones128, dg[vh]], [psBc])
            p.ts(E1[vh][:], psBc[:, 0:128], gcs, 0.0, ALU.subtract, ALU.max, [psBc, sc], [E1[vh]])
            p.ts(E2[vh][:], psBc[:, 0:128], gcs, 0.0, ALU.subtract, ALU.min, [psBc, sc], [E2[vh]])
            p.act(E1[vh][:], E1[vh][:], AF.Exp, [E1[vh]], [E1[vh]], scale=-1.0)
            p.act(E2[vh][:], E2[vh][:], AF.Exp, [E2[vh]], [E2[vh]])
            p.tt(E1[vh][:], E1[vh][:], cn[f"Ms{d}"][:], ALU.mult, [E1[vh], cn[f"Ms{d}"]], [E1[vh]], eng="pool")
            p.tt(E2[vh][:], E2[vh][:], cn[f"MiT{d}"][:], ALU.mult, [E2[vh], cn[f"MiT{d}"]], [E2[vh]], eng="pool")
            p.stt(Bm[vh][:], psKK[:, 0:128], sc[:, 56 + h:57 + h], E1[vh][:], ALU.mult, ALU.mult, [psKK, sc, E1[vh]], [Bm[vh]])
            p.tt(iT[vh][:], psKQ[:, 0:128], E2[vh][:], ALU.mult, [psKQ, E2[vh]], [iT[vh]])
        for vh in VH:
            ps = p.next_ps()
            p.tr(ps[:, 0:128], Bm[vh][:, :], ident[:], [Bm[vh], ident], [ps])
            p.cp(PTa[vh][:], ps[:, 0:128], [ps], [PTa[vh]], eng="act")
            p.tt(RT[vh][:], PTa[vh][:], ident[:], ALU.add, [PTa[vh], ident], [RT[vh]], eng="pool")
        Pc, PTc, Pn, PTn = Bm, PTa, Pa, PTb
        for step in range(5):
            for vh in VH:
                ps1 = p.next_ps()
                p.mm(ps1[:, 0:128], PTc[vh][:, :], Pc[vh][:, :], True, True, [PTc[vh], Pc[vh]], [ps1])
                p.cp(Pn[vh][:], ps1[:, 0:128], [ps1], [Pn[vh]], eng="act")
                if step < 4:
                    ps2 = p.next_ps()
                    p.mm(ps2[:, 0:128], Pc[vh][:, :], PTc[vh][:, :], True, True, [Pc[vh], PTc[vh]], [ps2])
                    p.cp(PTn[vh][:], ps2[:, 0:128], [ps2], [PTn[vh]])
            for vh in VH:
                ps3 = p.next_ps()
                p.mm(ps3[:, 0:128], Pn[vh][:, :], RT[vh][:, :], True, True, [Pn[vh], RT[vh]], [ps3])
                p.tt(RT[vh][:], RT[vh][:], ps3[:, 0:128], ALU.add, [RT[vh], ps3], [RT[vh]])
            Pc, PTc = Pn, PTn
            Pn = Pb if Pn is Pa else Pa
            PTn = PTa if PTn is PTb else PTb
        for (d, h) in VH:
            vh = (d, h)
            sc, tt = scs[d], tts[d]
            p.ts(vb[vh][:], vh_[vh][:], BG[:, tt, d * 8 + h:d * 8 + h + 1], None, ALU.mult, None, [vh_[vh], BG], [vb[vh]], eng="pool")
            p.ts(kbg[vh][:], kh[vh][:], sc[:, 8 + h:9 + h], None, ALU.mult, None, [kh[vh], sc], [kbg[vh]], eng="pool")
            p.ts(kdF[vh][:], kh[vh][:], sc[:, 40 + h:41 + h], None, ALU.mult, None, [kh[vh], sc], [kdF[vh]], eng="pool")
            p.ts(kdS[vh][:], kh[vh][:], sc[:, 48 + h:49 + h], None, ALU.mult, None, [kh[vh], sc], [kdS[vh]], eng="pool")
            psu = p.next_ps()
            p.mm(psu[:, 0:128], RT[vh][:, :], vb[vh][:, :], True, True, [RT[vh], vb[vh]], [psu])
            p.cp(u[vh][:], psu[:, 0:128], [psu], [u[vh]], eng="act")
            psw = p.next_ps()
            p.mm(psw[:, 0:128], kbg[vh][:, :], RT[vh][:, :], True, True, [kbg[vh], RT[vh]], [psw])
            p.cp(wT[vh][:], psw[:, 0:128], [psw], [wT[vh]])
        for (d, h) in VH:
            vh = (d, h)
            Sd = S[d][h]
            ps = p.next_ps()
            p.mm(ps[:, 0:128], wT[vh][:, :], Sd[:, :], True, True, [wT[vh], Sd], [ps])
            p.tt(v1[vh][:], u[vh][:], ps[:, 0:128], ALU.subtract, [u[vh], ps], [v1[vh]])
            ps = p.next_ps()
            p.mm(ps[:, 0:128], qTh[vh][:, :], Sd[:, :], True, True, [qTh[vh], Sd], [ps])
            p.cp(qs1[vh][:], ps[:, 0:128], [ps], [qs1[vh]], eng="act")
        for (d, h) in VH:
            vh = (d, h)
            Sd, sc, fi = S[d][h], scs[d], fis[d]
            ps = p.next_ps()
            p.mm(ps[:, 0:128], kdF[vh][:, :], v1[vh][:, :], True, True, [kdF[vh], v1[vh]], [ps])
            p.stt(Sd[:], Sd[:], sc[:, 16 + fi * 8 + h:17 + fi * 8 + h], ps[:, 0:128], ALU.mult, ALU.add, [Sd, sc, ps], [Sd])
        for (d, h) in VH:
            vh = (d, h)
            Sd = S[d][h]
            ps = p.next_ps()
            p.mm(ps[:, 0:128], wT[vh][:, :], Sd[:, :], True, True, [wT[vh], Sd], [ps])
            p.tt(v2[vh][:], u[vh][:], ps[:, 0:128], ALU.subtract, [u[vh], ps], [v2[vh]])
            ps = p.next_ps()
            p.mm(ps[:, 0:128], qTh[vh][:, :], Sd[:, :], True, True, [qTh[vh], Sd], [ps])
            p.cp(qs2[vh][:], ps[:, 0:128], [ps], [qs2[vh]], eng="act")
        for (d, h) in VH:
            vh = (d, h)
            Sd, sc, se = S[d][h], scs[d], ses[d]
            p.cp(v1[vh][rows[se], :], v2[vh][rows[se], :], [v2[vh]], [v1[vh]], eng="pool")
            ps = p.next_ps()
            p.mm(ps[:, 0:128], kdS[vh][:, :], v2[vh][:, :], True, True, [kdS[vh], v2[vh]], [ps])
            p.stt(Sd[:], Sd[:], sc[:, 16 + se * 8 + h:17 + se * 8 + h], ps[:, 0:128], ALU.mult, ALU.add, [Sd, sc, ps], [Sd])
        for (d, h) in VH:
            vh = (d, h)
            sc, fi, se = scs[d], fis[d], ses[d]
            r0 = tts[d] * 128
            ps = p.next_ps()
            p.mm(ps[:, 0:128], iT[vh][:, :], v1[vh][:, :], True, True, [iT[vh], v1[vh]], [ps])
            p.cp(osb[vh][:], ps[:, 0:128], [ps], [osb[vh]], eng="act")
            for (rr, qs) in ((rows[fi], qs1), (rows[se], qs2)):
                p.stt(osb[vh][rr, :], qs[vh][rr, :], sc[rr, 32 + h:33 + h], osb[vh][rr, :], ALU.mult, ALU.add, [qs[vh], sc, osb[vh]], [osb[vh]])
            od = gd[f"O{d}"]
            p.dma("sp", od[0][r0:r0 + 128, h * 128:(h + 1) * 128], osb[vh][:], reads=[osb[vh]], writes=[od[1]])
    p.release()
    p.mark()
    grow = p.sb([1, 1024], F32, "gorow")
    for h in H:
        p.dma("sp", grow[0:1, h * 128:(h + 1) * 128], inp["gdn_g_out"][j:j + 1, :], writes=[grow])
    gob = p.sb([128, 1024], F32, "gob")
    for nh in range(2):
        ps = p.next_ps()
        bcast_rows(p, k, ps[:, :], ps, grow[0:1, nh * 512:(nh + 1) * 512], grow, True, True)
        p.cp(gob[:, nh * 512:(nh + 1) * 512], ps[:, :], [ps], [gob], eng="act")
    wo = p.sb([128, 8, 1024], BF16, "gwo")
    for kk in range(8):
        p.dma("pool", wo[:, kk, :], inp["gdn_w_o"][j, kk * 128:(kk + 1) * 128, :], writes=[wo])
    gts = {}
    for s_ in ((0, 1) if need_ctx else (0,)):
        g_ = p.sb([128, 1024], F32, "ggt1")
        p.dma("sp", g_[:], k.MODS[l, s_, 2], reads=[k.MODSd], writes=[g_])
        gts[s_] = g_
    o0r = p.ring(2, [128, 1024], F32, "o0")
    o1r = p.ring(2, [128, 1024], F32, "o1")
    zr = p.ring(2, [128, 1024], F32, "zz")
    xr = p.ring(2, [128, 1024], F32, "gx")
    sq = p.sb([128, 1024], F32, "gsq2")
    ssr = p.ring(2, [128, 8], F32, "gss")
    oTr = p.ring(2, [128, 8, 128], BF16, "goT")
    yr = p.ring(2, [128, 512], F32, "gy")
    tiles = list(range(NT)) if need_ctx else list(range(2, NT))
    for tt in tiles:
        s_ = 1 if tt < 2 else 0
        r0 = tt * 128
        o0, o1, zz, x = o0r.next(), o1r.next(), zr.next(), xr.next()
        p.dma("sp", o0[:], gd["O0"][0][r0:r0 + 128, :], reads=[gd["O0"][1]], writes=[o0])
        p.dma("sp", o1[:], gd["O1"][0][r0:r0 + 128, :], reads=[gd["O1"][1]], writes=[o1])
        p.dma("sp", zz[:], gd["Z"][0][r0:r0 + 128, :], reads=[gd["Z"][1]], writes=[zz])
        p.dma("sp", x[:], k.X[r0:r0 + 128, :], reads=[k.Xd], writes=[x])
        p.tt(o0[:], o0[:], o1[:], ALU.add, [o0, o1], [o0])
        p.act(sq[:], o0[:], AF.Square, [o0], [sq])
        ss = ssr.next()
        p.op("dve", lambda e, ss=ss: e.reduce_sum(out=ss[:], in_=sq[:, :].rearrange("p (h d) -> p h d", h=8), axis=AX.X), [sq], [ss])
        rstd_of(p, k, ss[:], 128, 1e-6, [ss], [ss])
        for h in H:
            p.ts(o0[:, h * 128:(h + 1) * 128], o0[:, h * 128:(h + 1) * 128], ss[:, h:h + 1], None, ALU.mult, None, [o0, ss], [o0])
        p.tt(o0[:], o0[:], gob[:], ALU.mult, [o0, gob], [o0], eng="pool")
        p.tt(o0[:], o0[:], zz[:], ALU.mult, [o0, zz], [o0])
        oT = oTr.next()
        for half in range(2):
            ps = p.next_ps()
            for q in range(4):
                kk = half * 4 + q
                p.tr(ps[:, q * 128:(q + 1) * 128], o0[:, kk * 128:(kk + 1) * 128], k.ident[:], [o0, k.ident], [ps])
            for q in range(4):
                p.cp(oT[:, half * 4 + q, :], ps[:, q * 128:(q + 1) * 128], [ps], [oT], eng="act")
        for nh in range(2):
            ps = p.next_ps()
            for kk in range(8):
                p.mm(ps[:, :], oT[:, kk, :], wo[:, kk, nh * 512:(nh + 1) * 512], kk == 0, kk == 7, [oT, wo], [ps])
            y = yr.next()
            p.tt(y[:], ps[:, :], gts[s_][:, nh * 512:(nh + 1) * 512], ALU.mult, [ps, gts[s_]], [y])
            p.tt(x[:, nh * 512:(nh + 1) * 512], x[:, nh * 512:(nh + 1) * 512], y[:], ALU.add, [x, y], [x])
        p.dma("sp", k.X[r0:r0 + 128, :], x[:], reads=[x], writes=[k.Xd])
    p.release()
    p.release()


def stage_final(p, k):
    p.mark()
    gr = p.sb([1, 1024], F32, "gfrow")
    p.dma("sp", gr[0:1, :], k.inp["g_final"].rearrange("(o n) -> o n", o=1), writes=[gr])
    gb = p.sb([128, 1024], F32, "gfb")
    for nh in range(2):
        ps = p.next_ps()
        bcast_rows(p, k, ps[:, :], ps, gr[0:1, nh * 512:(nh + 1) * 512], gr, True, True)
        p.cp(gb[:, nh * 512:(nh + 1) * 512], ps[:, :], [ps], [gb], eng="act")
    xr = p.ring(2, [128, 1024], F32, "xf")
    sq = p.sb([128, 1024], F32, "sqf")
    ssr = p.ring(2, [128, 1], F32, "ssf")
    for tt in range(2, NT):
        x = xr.next()
        p.dma("sp", x[:], k.X[tt * 128:(tt + 1) * 128, :], reads=[k.Xd], writes=[x])
        ss = ssr.next()
        p.act(sq[:], x[:], AF.Square, [x], [sq, ss], accum_out=ss[:])
        rstd_of(p, k, ss[:], D, 1e-6, [ss], [ss])
        p.stt(x[:], x[:], ss[:, 0:1], gb[:], ALU.mult, ALU.mult, [x, ss, gb], [x])
        p.dma("sp", k.out[(tt - 2) * 128:(tt - 1) * 128, :], x[:], reads=[x], writes=[k.outd])
    p.release()


def build(cfg=None):
    cfg = cfg or {"stages": "all"}
    nc = bass.Bass("TRN2", target_bir_lowering=False)
    k = K()
    k.nc = nc
    k.inp = LazyInputs(nc)
    consts = host_consts()
    k.cin = {n: nc.dram_tensor("k_" + n, list(v.shape), F32, kind="ExternalInput").ap() for n, v in consts.items()}
    k.out = nc.dram_tensor("out", [NLAT, D], F32, kind="ExternalOutput").ap()
    k.outd = Dep("out")
    k.X = nc.dram_tensor("Xres", [NTOK, D], F32).ap()
    k.Xd = Dep("X")
    k.MODS = nc.dram_tensor("MODS", [DEPTH, 2, 6, 128, 1024], F32).ap()
    k.MODSd = Dep("MODS")
    dbg = cfg.get("debug_out", {})
    k.dbg = {n: nc.dram_tensor("dbg_" + n, s, F32, kind="ExternalOutput").ap() for n, s in dbg.items()}
    k.dbgd = Dep("dbg")
    p = Prog(nc)
    k.pstiles = [p.psum([128, 512], F32, f"psb{i}") for i in range(8)]
    p.ps = Ring(k.pstiles)
    k.psx = k.pstiles[4:8]
    k.ident = p.sb([128, 128], F32, "ident")
    p.dma("sp", k.ident[:], k.cin["ident"], writes=[k.ident])
    k.ones_row = p.sb([1, 128], F32, "ones_row")
    p.memset(k.ones_row[:], 1.0, [k.ones_row])
    k.ltmp = p.ring(2, [128, 128], F32, "ltmp")
    stages = cfg["stages"]
    if stages == "all":
        stages = [("init",), ("mod", list(range(DEPTH)))]
        for l in range(DEPTH):
            stages += [("mixer", l), ("ffn", l)]
        stages += [("final",)]
    for st in stages:
        if st[0] == "init":
            stage_init(p, k)
        elif st[0] == "mod":
            stage_mod(p, k, st[1])
        elif st[0] == "ffn":
            l = st[1]
            tiles = list(range(NT)) if l < DEPTH - 1 else list(range(2, NT))
            p.mark()
            hT = p.sb([128, 8, NTOK], BF16, "hT")
            G = p.sb([128, NT, NE], F32, "G")
            stage_norm(p, k, l, 1, tiles, hT, router=({"G": G} if not cfg.get("no_router") else None))
            if "G" in k.dbg:
                for tt in tiles:
                    p.dma("sp", k.dbg["G"][tt * 128:(tt + 1) * 128, :], G[:, tt, :], reads=[G], writes=[k.dbgd])
            if not cfg.get("skip_moe"):
                stage_moe(p, k, l, tiles, hT, G)
            p.release()
        elif st[0] == "final":
            stage_final(p, k)
        elif st[0] == "dumpX":
            p.dma("sp", k.dbg["X"], k.X, reads=[k.Xd], writes=[k.dbgd])
        elif st[0] == "mixer":
            l = st[1]
            kind, j = l % 3, l // 3
            need_ctx = l < DEPTH - 1
            if kind in (0, 1):
                p.barrier()
                p.ps = Ring(k.pstiles[0:4])
                stage_attn(p, k, l, kind, j, need_ctx)
                p.ps = Ring(k.pstiles)
            else:
                stage_gdn(p, k, l, j, need_ctx)
    p.emit()
    k_used = list(k.inp.keys())
    return nc, consts, k_used


def from_mixers(p, k, l):
    raise NotImplementedError


_CACHE = {}


def kernel(**inputs):
    if "nc" not in _CACHE:
        _CACHE["nc"] = build()
    nc, consts, used = _CACHE["nc"]
    n = 8
    in_maps = []
    for b in range(n):
        m = {}
        for name in used:
            a = np.asarray(inputs[name], dtype=np.float32)
            if name in ("x", "c", "ctx"):
                a = a[b]
            m[name] = np.ascontiguousarray(a)
        for cn, cv in consts.items():
            m["k_" + cn] = cv
        in_maps.append(m)
    res = run_bass_kernel_spmd(nc, in_maps, core_ids=list(range(n)))
    return np.stack([r["out"] for r in res.results], axis=0).astype(np.float32)
```

```python
import numpy as np
import concourse.bass as bass
import concourse.mybir as mybir
from concourse.bass_utils import run_bass_kernel_spmd
from contextlib import ExitStack

F32 = mybir.dt.float32
BF16 = mybir.dt.bfloat16
ALU = mybir.AluOpType
AF = mybir.ActivationFunctionType
AX = mybir.AxisListType

D = 1024
NCTX = 256
NLAT = 2048
NTOK = NCTX + NLAT
NT = NTOK // 128
DEPTH = 4
NE = 32


class Dep:
    __slots__ = ("name", "w", "r")

    def __init__(self, name=""):
        self.name = name
        self.w = None
        self.r = []


class Tile:
    def __init__(self, t, dep=None):
        self.t = t
        self.dep = dep or Dep()

    def __getitem__(self, k):
        return self.t[k]


class Ring:
    def __init__(self, tiles):
        self.tiles = tiles
        self.i = 0

    def next(self):
        t = self.tiles[self.i % len(self.tiles)]
        self.i += 1
        return t


class Prog:
    ENG = ("pe", "act", "dve", "pool", "sp")

    def __init__(self, nc, n_dma_sems=24):
        self.nc = nc
        self.es = ExitStack()
        self.sem = {}
        self.cnt = {}
        self.ops = {e: [] for e in self.ENG}
        self.waited = {e: {} for e in self.ENG}
        for e in self.ENG:
            self.sem[e] = self.es.enter_context(nc.semaphore("s_" + e))
            self.cnt[e] = 0
        self.dq = {}
        for q in ("sp", "pool", "act"):
            n = n_dma_sems if q != "act" else 8
            sems = [self.es.enter_context(nc.semaphore(f"d_{q}{i}")) for i in range(n)]
            self.dq[q] = {"sems": sems, "tgt": [0] * n, "i": 0}
        self.semobj = {}
        for e in self.ENG:
            self.semobj[("e", e)] = self.sem[e]
        for q, d in self.dq.items():
            for i, s in enumerate(d["sems"]):
                self.semobj[("d", q, i)] = s
        self.stk = [ExitStack()]
        self.n_t = 0
        self.ps = None

    def sb(self, shape, dtype, name=None):
        self.n_t += 1
        name = f"{name or 't'}_{self.n_t}"
        t = self.stk[-1].enter_context(self.nc.sbuf_tensor(name, list(shape), dtype))
        return Tile(t, Dep(name))

    def ring(self, n, shape, dtype, name=None):
        return Ring([self.sb(shape, dtype, name) for _ in range(n)])

    def mark(self):
        self.stk.append(ExitStack())

    def release(self):
        self.barrier()
        self.stk.pop().close()

    def psum(self, shape, dtype=F32, name=None):
        self.n_t += 1
        name = name or f"ps{self.n_t}"
        t = self.nc.alloc_psum_tensor(name, list(shape), dtype)
        return Tile(t, Dep(name))

    @staticmethod
    def _d(x):
        return x.dep if isinstance(x, Tile) else x

    def _collect(self, reads, writes):
        need = {}
        for r in reads:
            d = self._d(r)
            if d.w is not None:
                k, v = d.w
                if need.get(k, 0) < v:
                    need[k] = v
        for w in writes:
            d = self._d(w)
            if d.w is not None:
                k, v = d.w
                if need.get(k, 0) < v:
                    need[k] = v
            for (k, v) in d.r:
                if need.get(k, 0) < v:
                    need[k] = v
        return need

    def _waits(self, eng, need):
        ws = []
        wd = self.waited[eng]
        for k, v in need.items():
            if eng == "pe" and k == ("e", "pe"):
                continue
            if wd.get(k, 0) < v:
                wd[k] = v
                ws.append((k, v))
        return ws

    def _commit(self, tok, reads, writes):
        for r in reads:
            d = self._d(r)
            d.r.append(tok)
            if len(d.r) > 48:
                m = {}
                for k, v in d.r:
                    if m.get(k, 0) < v:
                        m[k] = v
                d.r = list(m.items())
        for w in writes:
            d = self._d(w)
            d.w = tok
            d.r = []

    def op(self, eng, fn, reads=(), writes=()):
        need = self._collect(reads, writes)
        ws = self._waits(eng, need)
        self.cnt[eng] += 1
        tok = (("e", eng), self.cnt[eng])
        self.ops[eng].append((ws, fn, (("e", eng), 1)))
        self._commit(tok, reads, writes)
        return tok

    def dma(self, q, out, in_, reads=(), writes=(), **kw):
        need = self._collect(reads, writes)
        d = self.dq[q]
        i = d["i"] % len(d["sems"])
        d["i"] += 1
        key = ("d", q, i)
        if d["tgt"][i] > 0:
            need[key] = max(need.get(key, 0), d["tgt"][i])
        ws = self._waits(q, need)
        d["tgt"][i] += 16
        tok = (key, d["tgt"][i])
        self.ops[q].append((ws, (lambda e: e.dma_start(out=out, in_=in_, **kw)), (key, 16)))
        self._commit(tok, reads, writes)
        return tok

    def barrier(self):
        need = {}
        for e in self.ENG:
            if self.cnt[e] > 0:
                need[("e", e)] = self.cnt[e]
        for q, d in self.dq.items():
            for i, t in enumerate(d["tgt"]):
                if t > 0:
                    need[("d", q, i)] = t
        for e in self.ENG:
            ws = []
            wd = self.waited[e]
            for k, v in need.items():
                if k == ("e", e):
                    continue
                if wd.get(k, 0) < v:
                    wd[k] = v
                    ws.append((k, v))
            if ws:
                self.ops[e].append((ws, None, None))

    def act(self, out, in_, func, R, W, **kw):
        return self.op("act", lambda e: e.activation(out=out, in_=in_, func=func, **kw), R, W)

    def ts(self, out, in0, s1, s2, op0, op1, R, W, eng="dve"):
        if op1 is None:
            return self.op(eng, lambda e: e.tensor_scalar(out=out, in0=in0, scalar1=s1, scalar2=None, op0=op0), R, W)
        return self.op(eng, lambda e: e.tensor_scalar(out=out, in0=in0, scalar1=s1, scalar2=s2, op0=op0, op1=op1), R, W)

    def tt(self, out, in0, in1, op, R, W, eng="dve"):
        return self.op(eng, lambda e: e.tensor_tensor(out=out, in0=in0, in1=in1, op=op), R, W)

    def stt(self, out, in0, scalar, in1, op0, op1, R, W, eng="dve"):
        return self.op(eng, lambda e: e.scalar_tensor_tensor(out=out, in0=in0, scalar=scalar, in1=in1, op0=op0, op1=op1), R, W)

    def cp(self, out, in_, R, W, eng="dve"):
        if eng == "act":
            return self.op("act", lambda e: e.copy(out=out, in_=in_), R, W)
        return self.op(eng, lambda e: e.tensor_copy(out=out, in_=in_), R, W)

    def memset(self, ap, val, W, eng="dve"):
        return self.op(eng, lambda e: e.memset(ap, val), (), W)

    def mm(self, out, lhsT, rhs, start, stop, R, W):
        return self.op("pe", lambda e: e.matmul(out, lhsT=lhsT, rhs=rhs, start=start, stop=stop), R, W)

    def tr(self, out, in_, ident, R, W):
        return self.op("pe", lambda e: e.transpose(out=out, in_=in_, identity=ident), R, W)

    def recip(self, out, in_, R, W):
        return self.op("dve", lambda e: e.reciprocal(out=out, in_=in_), R, W)

    def next_ps(self):
        return self.ps.next()

    def emit(self):
        self.barrier()
        nc = self.nc
        with nc.Block() as block:
            def body(eng):
                def run(e):
                    for ws, fn, inc in self.ops[eng]:
                        for k, v in ws:
                            e.wait_ge(self.semobj[k], v)
                        if fn is not None:
                            ins = fn(e)
                            ins.then_inc(self.semobj[inc[0]], inc[1])
                return run
            block.tensor(body("pe"))
            block.scalar(body("act"))
            block.vector(body("dve"))
            block.gpsimd(body("pool"))
            block.sync(body("sp"))
        while self.stk:
            self.stk.pop().close()
        self.es.close()


INPUT_SHAPES = {
    "x": [NLAT, D], "c": [D], "ctx": [NCTX, D], "c_ctx": [D],
    "w_mod": [4, D, 6 * D], "b_mod": [4, 6 * D], "g_mix": [4, D], "g_ffn": [4, D],
    "win_w_qkv": [2, D, 1536], "win_b_qkv": [2, 1536], "win_sink": [2, 16], "win_w_o": [2, D, D], "win_b_o": [2, D],
    "glb_w_qkv": [1, D, 1536], "glb_g_q": [1, 64], "glb_g_k": [1, 64], "glb_w_o": [1, D, D],
    "gdn_w_in": [1, D, 4128], "gdn_conv_w": [1, 5, 3072], "gdn_a_log": [1, 2, 8], "gdn_dt_bias": [1, 2, 8],
    "gdn_g_out": [1, 128], "gdn_w_o": [1, D, D],
    "moe_w_router": [4, D, NE], "moe_b_router": [4, NE], "moe_w_up": [4, NE, D, 2 * D], "moe_b_up": [4, NE, 2 * D],
    "moe_w_down": [4, NE, D, D], "moe_b_down": [4, NE, D], "g_final": [D],
}


def host_consts():
    c = {}
    c["ident"] = np.eye(128, dtype=np.float32)
    a = np.arange(128)
    lo = (a[None, :] <= a[:, None]).astype(np.float32)
    hi = (a[:, None] <= a[None, :]).astype(np.float32)
    c["mask_lo"] = np.tile(lo, (1, 4))
    c["mask_hi"] = np.tile(hi, (1, 4))
    t = np.arange(NLAT)
    pos = np.stack([t // 64, t % 64], 0).astype(np.float32)
    inv = (10000.0 ** (-np.arange(0, 32, 2, dtype=np.float32) / 32)).astype(np.float32)
    cosT = np.zeros((64, NLAT), np.float32)
    sinT = np.zeros((64, NLAT), np.float32)
    PT = np.zeros((64, 64), np.float32)
    for d in range(64):
        ax, r = d // 32, d % 32
        half, f = r // 16, r % 16
        ang = (pos[ax] * inv[f]).astype(np.float32)
        cosT[d] = np.cos(ang)
        sinT[d] = np.sin(ang)
        if half == 0:
            PT[d + 16, d] = -1.0
        else:
            PT[d - 16, d] = 1.0
    c["cosT"] = cosT
    c["sinT"] = sinT
    c["PT"] = PT
    idx = np.arange(128)
    ch = idx // 64
    same = ch[:, None] == ch[None, :]
    le = idx[:, None] <= idx[None, :]
    ge = idx[:, None] >= idx[None, :]
    lt = idx[:, None] < idx[None, :]
    gt = idx[:, None] > idx[None, :]
    f32 = np.float32
    c["gd_L0"] = (same & le).astype(f32)
    c["gd_L1"] = (same & ge).astype(f32)
    for d, last in ((0, (63, 127)), (1, (0, 64))):
        lastv = np.array([last[ci] for ci in ch])
        c[f"gd_SelC{d}"] = (idx[:, None] == lastv[None, :]).astype(f32)
        c[f"gd_SelA{d}"] = np.repeat((idx == last[0]).astype(f32)[:, None], 128, 1)
        c[f"gd_SelB{d}"] = np.repeat((idx == last[1]).astype(f32)[:, None], 128, 1)
    c["gd_Ms0"] = (same & gt).astype(f32)
    c["gd_Ms1"] = (same & lt).astype(f32)
    c["gd_MiT0"] = (same & le).astype(f32)
    c["gd_MiT1"] = (same & ge).astype(f32)
    c["gd_rowm"] = np.stack([(idx < 64), (idx >= 64)], 1).astype(f32)
    for d in (0, 1):
        c[f"gd_Mb{d}"] = ((1.0 - c[f"gd_Ms{d}"]) * 1e4).astype(f32)
        c[f"gd_Mn{d}"] = ((c[f"gd_MiT{d}"] - 1.0) * 1e4).astype(f32)
    return c


class K:
    pass


class LazyInputs(dict):
    def __init__(self, nc):
        super().__init__()
        self.nc = nc

    def __missing__(self, n):
        ap = self.nc.dram_tensor(n, INPUT_SHAPES[n], F32, kind="ExternalInput").ap()
        self[n] = ap
        return ap


def load_T(p, k, dst_ap, dst_tile, src_rows_ap, R, src_dep=None, C=128):
    tmp = k.ltmp.next()
    p.dma("sp", tmp[0:R, 0:C], src_rows_ap, reads=[src_dep] if src_dep else (), writes=[tmp])
    ps = p.next_ps()
    p.tr(ps[0:C, 0:R], tmp[0:R, 0:C], k.ident[0:R, 0:R], [tmp, k.ident], [ps])
    p.cp(dst_ap, ps[0:C, 0:R], [ps], [dst_tile])


def bcast_rows(p, k, ps_ap, ps_tile, row_ap, row_tile, start, stop):
    p.mm(ps_ap, k.ones_row[0:1, :], row_ap, start, stop, [k.ones_row, row_tile], [ps_tile])


def stage_init(p, k):
    p.dma("sp", k.X[0:NCTX, :], k.inp["ctx"], reads=(), writes=[k.Xd])
    p.dma("sp", k.X[NCTX:NTOK, :], k.inp["x"], reads=(), writes=[k.Xd])


def stage_mod(p, k, layers):
    p.mark()
    inp = k.inp
    craw = p.sb([128, 16], F32, "craw")
    load_T(p, k, craw[:, 0:8], craw, inp["c"].rearrange("(k q) -> k q", q=128), 8)
    load_T(p, k, craw[:, 8:16], craw, inp["c_ctx"].rearrange("(k q) -> k q", q=128), 8)
    csil = p.sb([128, 16], F32, "csil")
    p.act(csil[:], craw[:], AF.Silu, [craw], [csil])
    ones_bf = p.sb([128, 128], F32, "ones128")
    p.memset(ones_bf[:], 1.0, [ones_bf])
    lhs = p.sb([128, 16, 128], BF16, "modlhs")
    for j in range(16):
        p.ts(lhs[:, j, :], ones_bf[:], csil[:, j:j + 1], None, ALU.mult, None, [ones_bf, csil], [lhs])
    wring = p.ring(2, [128, 8, 512], BF16, "wmod")
    brow = p.ring(2, [1, 512], F32, "bmodrow")
    grow = p.ring(2, [1, 1024], F32, "grow")
    oring = p.ring(3, [128, 512], F32, "modo")
    gb = [p.sb([128, 1024], F32, "gb0"), p.sb([128, 1024], F32, "gb1")]
    for l in layers:
        for gi, gname in enumerate(("g_mix", "g_ffn")):
            gr = grow.next()
            p.dma("sp", gr[0:1, :], inp[gname][l:l + 1, :], writes=[gr])
            for nh in range(2):
                ps = p.next_ps()
                bcast_rows(p, k, ps[:, :], ps, gr[0:1, nh * 512:(nh + 1) * 512], gr, True, True)
                p.cp(gb[gi][:, nh * 512:(nh + 1) * 512], ps[:, :], [ps], [gb[gi]], eng="act")
        for j in range(6):
            for nh in range(2):
                n0 = j * 1024 + nh * 512
                wt = wring.next()
                p.dma("pool", wt[:, :, :], inp["w_mod"][l].rearrange("(k q) n -> q k n", q=128)[:, :, n0:n0 + 512], writes=[wt])
                br = brow.next()
                p.dma("sp", br[0:1, :], inp["b_mod"][l:l + 1, n0:n0 + 512], writes=[br])
                for s in range(2):
                    ps = p.next_ps()
                    for kk in range(8):
                        p.mm(ps[:, :], lhs[:, s * 8 + kk, :], wt[:, kk, :], kk == 0, False, [lhs, wt], [ps])
                    bcast_rows(p, k, ps[:, :], ps, br[0:1, :], br, False, True)
                    o = oring.next()
                    if j in (1, 4):
                        g = gb[0] if j == 1 else gb[1]
                        p.stt(o[:], ps[:, :], 1.0, g[:, nh * 512:(nh + 1) * 512], ALU.add, ALU.mult, [ps, g], [o])
                    else:
                        p.cp(o[:], ps[:, :], [ps], [o], eng="act")
                    p.dma("sp", k.MODS[l, s, j, :, nh * 512:(nh + 1) * 512], o[:], reads=[o], writes=[k.MODSd])
    p.release()


def rstd_of(p, k, ss, n, eps, R, W):
    p.ts(ss, ss, 1.0 / n, eps, ALU.mult, ALU.add, R, W)
    p.recip(ss, ss, R, W)
    p.act(ss, ss, AF.Sqrt, R, W)


def stage_norm(p, k, l, which, tiles, hT, router=None, p32=None):
    p.mark()
    jsh, jA = (0, 1) if which == 0 else (3, 4)
    modt = {}
    for s in (0, 1):
        a = p.sb([128, 1024], F32, "modA")
        b = p.sb([128, 1024], F32, "modS")
        p.dma("sp", a[:], k.MODS[l, s, jA], reads=[k.MODSd], writes=[a])
        p.dma("sp", b[:], k.MODS[l, s, jsh], reads=[k.MODSd], writes=[b])
        modt[s] = (a, b)
    xr = p.ring(3, [128, 1024], F32, "xn")
    hr = p.ring(3, [128, 1024], F32, "hn")
    sqr = p.ring(2, [128, 1024], F32, "sq")
    ssr = p.ring(3, [128, 1], F32, "ss")
    need32 = router is not None or p32 is not None
    if need32:
        h32r = p.ring(3, [128, 8, 128], F32, "h32")
    if router is not None:
        wr = p.sb([128, 8, NE], F32, "wr")
        for kk in range(8):
            p.dma("sp", wr[:, kk, :], k.inp["moe_w_router"][l, kk * 128:(kk + 1) * 128, :], writes=[wr])
        brr = p.sb([1, NE], F32, "brr")
        p.dma("sp", brr[0:1, :], k.inp["moe_b_router"][l:l + 1, :], writes=[brr])
        lgr = p.ring(2, [128, NE], F32, "lg")
        m8r = p.ring(2, [128, 8], F32, "m8")
        er = p.ring(2, [128, NE], F32, "eg")
        smr = p.ring(4, [128, 1], F32, "sm")

    def s1(tt):
        s = 1 if tt < 2 else 0
        A, S = modt[s]
        x = xr.next()
        p.dma("sp", x[:], k.X[tt * 128:(tt + 1) * 128, :], reads=[k.Xd], writes=[x])
        ss = ssr.next()
        sq = sqr.next()
        p.act(sq[:], x[:], AF.Square, [x], [sq, ss], accum_out=ss[:])
        rstd_of(p, k, ss[:], D, 1e-6, [ss], [ss])
        h = hr.next()
        p.stt(h[:], x[:], ss[:, 0:1], A[:], ALU.mult, ALU.mult, [x, ss, A], [h])
        p.tt(h[:], h[:], S[:], ALU.add, [h, S], [h])
        return h

    def s2(tt, h):
        h32 = h32r.next() if need32 else None
        for half in range(2):
            ps = p.next_ps()
            for q in range(4):
                kk = half * 4 + q
                p.tr(ps[:, q * 128:(q + 1) * 128], h[:, kk * 128:(kk + 1) * 128], k.ident[:], [h, k.ident], [ps])
            for q in range(4):
                if need32:
                    p.cp(h32[:, half * 4 + q, :], ps[:, q * 128:(q + 1) * 128], [ps], [h32], eng="act")
                    p.cp(hT[:, half * 4 + q, tt * 128:(tt + 1) * 128], h32[:, half * 4 + q, :], [h32], [hT])
                else:
                    p.cp(hT[:, half * 4 + q, tt * 128:(tt + 1) * 128], ps[:, q * 128:(q + 1) * 128], [ps], [hT], eng="act")
        return h32

    def s3(tt, h32):
        if p32 is not None:
            ps = p.next_ps()
            n = p32["n"]
            for kk in range(8):
                p.mm(ps[:, 0:n], h32[:, kk, :], p32["w"][:, kk, :], kk == 0, kk == 7, [h32, p32["w"]], [ps])
            p32["cb"](tt, ps)
        if router is not None:
            G = router["G"]
            ps = p.next_ps()
            for kk in range(8):
                p.mm(ps[:, 0:NE], h32[:, kk, :], wr[:, kk, :], kk == 0, False, [h32, wr], [ps])
            bcast_rows(p, k, ps[:, 0:NE], ps, brr[0:1, :], brr, False, True)
            lg = lgr.next()
            p.cp(lg[:], ps[:, 0:NE], [ps], [lg])
            m8 = m8r.next()
            p.op("dve", lambda e, m8=m8, lg=lg: e.max(out=m8[:], in_=lg[:]), [lg], [m8])
            nm = smr.next()
            p.ts(nm[:], m8[:, 0:1], -1.0, None, ALU.mult, None, [m8], [nm])
            eg = er.next()
            p.act(eg[:], lg[:], AF.Exp, [lg, nm], [eg], bias=nm[:, 0:1], scale=1.0)
            sm = smr.next()
            p.stt(eg[:], lg[:], m8[:, 3:4], eg[:], ALU.is_ge, ALU.mult, [lg, m8, eg], [eg])
            p.op("dve", lambda e, sm=sm, eg=eg: e.reduce_sum(out=sm[:], in_=eg[:], axis=AX.X), [eg], [sm])
            p.recip(sm[:], sm[:], [sm], [sm])
            p.ts(G[:, tt, :], eg[:], sm[:, 0:1], None, ALU.mult, None, [eg, sm], [G])

    q1, q2 = [], []
    for tt in tiles:
        q1.append((tt, s1(tt)))
        if len(q1) > 1:
            t_, h_ = q1.pop(0)
            q2.append((t_, s2(t_, h_)))
        if need32 and len(q2) > 1:
            s3(*q2.pop(0))
    while q1:
        t_, h_ = q1.pop(0)
        q2.append((t_, s2(t_, h_)))
    if need32:
        while q2:
            s3(*q2.pop(0))
    p.release()


def stage_moe(p, k, l, tiles, hT, G):
    p.mark()
    inp = k.inp
    t0 = tiles[0]
    ntile = len(tiles)
    acc = p.sb([128, ntile, 1024], F32, "acc")
    bupT = p.sb([128, NE * 16], F32, "bupT")
    bsrc = inp["moe_b_up"][l].rearrange("e (m q) -> (e m) q", q=128)
    for i in range(4):
        load_T(p, k, bupT[:, i * 128:(i + 1) * 128], bupT, bsrc[i * 128:(i + 1) * 128, :], 128)
    p.mark()
    bd = p.sb([NE, 1024], F32, "bd")
    p.dma("sp", bd[:], inp["moe_b_down"][l], writes=[bd])
    gtr = p.ring(2, [NE, 128], F32, "GT")
    for ti, tt in enumerate(tiles):
        ps = p.next_ps()
        p.tr(ps[0:NE, 0:128], G[:, tt, :], k.ident[:], [G, k.ident], [ps])
        gt = gtr.next()
        p.cp(gt[:], ps[0:NE, 0:128], [ps], [gt])
        for nh in range(2):
            ps2 = p.next_ps()
            p.mm(ps2[:, :], gt[:, :], bd[:, nh * 512:(nh + 1) * 512], True, True, [gt, bd], [ps2])
            p.cp(acc[:, ti, nh * 512:(nh + 1) * 512], ps2[:, :], [ps2], [acc], eng="act")
    p.release()
    p.mark()
    blocks = []
    i = 0
    while i < ntile:
        n = min(4, ntile - i)
        blocks.append((i, n))
        i += n
    wur = p.ring(2, [128, 8, 2, 512], BF16, "wu")
    wdr = p.ring(2, [128, 4, 1024], BF16, "wd")
    aTr = [p.ring(3, [128, 512], BF16, f"aT{m}") for m in range(4)]
    t1r = p.ring(3, [128, 512], F32, "t1")
    sgr = p.ring(2, [128, 512], F32, "sg")
    t2r = p.ring(3, [128, 512], F32, "t2")
    accd = [[Dep(f"acc{ti}_{nh}") for nh in range(2)] for ti in range(ntile)]
    for ti in range(ntile):
        for nh in range(2):
            accd[ti][nh].w = acc.dep.w
    pending = []

    def make_down(aT, wd, e, b0, bn):
        def emit():
            for j in range(bn):
                ti = b0 + j
                for nh in range(2):
                    po = p.next_ps()
                    for kk in range(4):
                        p.mm(po[:, :], aT[kk][:, j * 128:(j + 1) * 128], wd[:, kk, nh * 512:(nh + 1) * 512], kk == 0, kk == 3, [aT[kk], wd], [po])
                    av = acc[:, ti, nh * 512:(nh + 1) * 512]
                    p.stt(av, po[:, :], G[:, t0 + ti, e:e + 1], av, ALU.mult, ALU.add, [po, G, accd[ti][nh]], [accd[ti][nh]])
        return emit

    for e in range(NE):
        for half in range(2):
            wu = wur.next()
            wd = wdr.next()
            src = inp["moe_w_up"][l, e].rearrange("r (g h c) -> r g h c", g=2, h=2)
            for kk in range(8):
                p.dma("pool", wu[:, kk, :, :], src[kk * 128:(kk + 1) * 128, :, half, :], writes=[wu])
            for kk in range(4):
                r0 = half * 512 + kk * 128
                p.dma("pool", wd[:, kk, :], inp["moe_w_down"][l, e, r0:r0 + 128, :], writes=[wd])
            for (b0, bn) in blocks:
                tok0 = (t0 + b0) * 128
                ntok = bn * 128
                aT = []
                for m in range(4):
                    pa = p.next_ps()
                    for kk in range(8):
                        p.mm(pa[:, 0:ntok], wu[:, kk, 0, m * 128:(m + 1) * 128], hT[:, kk, tok0:tok0 + ntok], kk == 0, kk == 7, [wu, hT], [pa])
                    pb = p.next_ps()
                    for kk in range(8):
                        p.mm(pb[:, 0:ntok], wu[:, kk, 1, m * 128:(m + 1) * 128], hT[:, kk, tok0:tok0 + ntok], kk == 0, kk == 7, [wu, hT], [pb])
                    ca = e * 16 + half * 4 + m
                    cb = ca + 8
                    t1 = t1r.next()
                    p.ts(t1[:, 0:ntok], pa[:, 0:ntok], bupT[:, ca:ca + 1], 7.0, ALU.add, ALU.min, [pa, bupT], [t1])
                    sg = sgr.next()
                    p.act(sg[:, 0:ntok], t1[:, 0:ntok], AF.Sigmoid, [t1], [sg], scale=1.702)
                    t2 = t2r.next()
                    p.act(t2[:, 0:ntok], pb[:, 0:ntok], AF.Identity, [pb, bupT], [t2], bias=bupT[:, cb:cb + 1], scale=1.0)
                    p.ts(t2[:, 0:ntok], t2[:, 0:ntok], -7.0, 7.0, ALU.max, ALU.min, [t2], [t2])
                    p.tt(t1[:, 0:ntok], t1[:, 0:ntok], sg[:, 0:ntok], ALU.mult, [t1, sg], [t1])
                    a = aTr[m].next()
                    p.stt(a[:, 0:ntok], t2[:, 0:ntok], 1.0, t1[:, 0:ntok], ALU.add, ALU.mult, [t2, t1], [a])
                    aT.append(a)
                if pending:
                    pending.pop(0)()
                pending.append(make_down(aT, wd, e, b0, bn))
    while pending:
        pending.pop(0)()
    p.release()
    gts = {}
    for s in (0, 1):
        g = p.sb([128, 1024], F32, "gt2")
        p.dma("sp", g[:], k.MODS[l, s, 5], reads=[k.MODSd], writes=[g])
        gts[s] = g
    xr = p.ring(2, [128, 1024], F32, "xres")
    for ti, tt in enumerate(tiles):
        s = 1 if tt < 2 else 0
        x = xr.next()
        p.dma("sp", x[:], k.X[tt * 128:(tt + 1) * 128, :], reads=[k.Xd], writes=[x])
        p.tt(acc[:, ti, :], acc[:, ti, :], gts[s][:], ALU.mult, [accd[ti][0], accd[ti][1], gts[s]], [accd[ti][0], accd[ti][1]])
        p.tt(x[:], x[:], acc[:, ti, :], ALU.add, [x, accd[ti][0], accd[ti][1]], [x])
        p.dma("sp", k.X[tt * 128:(tt + 1) * 128, :], x[:], reads=[x], writes=[k.Xd])
    p.release()


def stage_attn(p, k, l, kind, j, need_ctx):
    inp = k.inp
    wname = "win" if kind == 0 else "glb"
    wqkv = inp[wname + "_w_qkv"][j]
    p.mark()
    qT = p.sb([64, 16, NTOK], BF16, "qT")
    qTd = [[Dep(f"qT{t}_{g}") for g in range(4)] for t in range(NT)]
    kT = p.sb([64, 4, NTOK], BF16, "kT")
    V = p.sb([128, NT, 256], BF16, "V")
    ones_bf = p.sb([128, 64], BF16, "ones_bf")
    p.memset(ones_bf[:], 1.0, [ones_bf])
    p.mark()
    p.ps = Ring(k.pstiles)
    hT = p.sb([128, 8, NTOK], BF16, "hT")
    stage_norm(p, k, l, 0, list(range(NT)), hT)
    cosT = p.sb([64, NLAT], F32, "cosT")
    sinT = p.sb([64, NLAT], F32, "sinT")
    PT = p.sb([64, 64], F32, "PT")
    p.dma("sp", cosT[:], k.cin["cosT"], writes=[cosT])
    p.dma("sp", sinT[:], k.cin["sinT"], writes=[sinT])
    p.dma("sp", PT[:], k.cin["PT"], writes=[PT])
    wq = p.sb([128, 8, 1024], BF16, "wq")
    wk = p.sb([128, 8, 256], BF16, "wk")
    wv = p.sb([128, 8, 256], BF16, "wv")
    for kk in range(8):
        p.dma("pool", wq[:, kk, :], wqkv[kk * 128:(kk + 1) * 128, 0:1024], writes=[wq])
        p.dma("pool", wk[:, kk, :], wqkv[kk * 128:(kk + 1) * 128, 1024:1280], writes=[wk])
        p.dma("pool", wv[:, kk, :], wqkv[kk * 128:(kk + 1) * 128, 1280:1536], writes=[wv])
    if kind == 0:
        bT = p.sb([64, 24], F32, "bT")
        load_T(p, k, bT[:, :], bT, inp["win_b_qkv"][j].rearrange("(h d) -> h d", d=64), 24, C=64)
        bvrow = p.sb([1, 256], F32, "bvrow")
        p.dma("sp", bvrow[0:1, :], inp["win_b_qkv"][j:j + 1, 1280:1536], writes=[bvrow])
    else:
        gqk = p.sb([64, 2], F32, "gqk")
        load_T(p, k, gqk[:, 0:1], gqk, inp["glb_g_q"][j:j + 1, :], 1, C=64)
        load_T(p, k, gqk[:, 1:2], gqk, inp["glb_g_k"][j:j + 1, :], 1, C=64)
        avg64 = p.sb([64, 64], F32, "avg64")
        p.memset(avg64[:], 1.0 / 64, [avg64])
    q32r = p.ring(5, [64, 512], F32, "q32")
    sqr = p.ring(5, [64, 512], F32, "qsq")
    blocks = [(0, 256), (256, 512), (768, 512), (1280, 512), (1792, 512)]
    for hh in range(20):
        isq = hh < 16
        items = []
        for (t0, n) in blocks:
            ps = p.next_ps()
            for kk in range(8):
                lw = wq[:, kk, hh * 64:(hh + 1) * 64] if isq else wk[:, kk, (hh - 16) * 64:(hh - 15) * 64]
                p.mm(ps[0:64, 0:n], lw, hT[:, kk, t0:t0 + n], kk == 0, kk == 7, [wq if isq else wk, hT], [ps])
            q32 = q32r.next()
            if kind == 0:
                p.act(q32[:, 0:n], ps[0:64, 0:n], AF.Identity, [ps, bT], [q32], bias=bT[:, hh:hh + 1], scale=1.0)
            else:
                p.cp(q32[:, 0:n], ps[0:64, 0:n], [ps], [q32], eng="act")
            items.append((t0, n, q32, sqr.next()))
        if kind == 1:
            gcol = gqk[:, 0:1] if isq else gqk[:, 1:2]
            for (t0, n, q32, sq) in items:
                p.act(sq[:, 0:n], q32[:, 0:n], AF.Square, [q32], [sq])
            psns = []
            for (t0, n, q32, sq) in items:
                psn = p.next_ps()
                p.mm(psn[0:64, 0:n], avg64[:, :], sq[:, 0:n], True, True, [avg64, sq], [psn])
                psns.append(psn)
            for (t0, n, q32, sq), psn in zip(items, psns):
                p.ts(sq[:, 0:n], psn[0:64, 0:n], 1e-6, None, ALU.add, None, [psn], [sq])
                p.recip(sq[:, 0:n], sq[:, 0:n], [sq], [sq])
            for (t0, n, q32, sq) in items:
                p.act(sq[:, 0:n], sq[:, 0:n], AF.Sqrt, [sq], [sq])
            for (t0, n, q32, sq) in items:
                p.stt(q32[:, 0:n], q32[:, 0:n], gcol, sq[:, 0:n], ALU.mult, ALU.mult, [q32, gqk, sq], [q32])
        ps2s = {}
        for (t0, n, q32, sq) in items[1:]:
            ps2 = p.next_ps()
            p.mm(ps2[0:64, 0:n], PT[:, :], q32[:, 0:n], True, True, [PT, q32], [ps2])
            ps2s[t0] = ps2
        for (t0, n, q32, sq) in items[1:]:
            l0 = t0 - NCTX
            p.tt(sq[:, 0:n], ps2s[t0][0:64, 0:n], sinT[:, l0:l0 + n], ALU.mult, [ps2s[t0], sinT], [sq])
        for (t0, n, q32, sq) in items[1:]:
            l0 = t0 - NCTX
            p.tt(q32[:, 0:n], q32[:, 0:n], cosT[:, l0:l0 + n], ALU.mult, [q32, cosT], [q32], eng="pool")
        for (t0, n, q32, sq) in items:
            dst = qT[:, hh, t0:t0 + n] if isq else kT[:, hh - 16, t0:t0 + n]
            dW = [qTd[t][hh // 4] for t in range(t0 // 128, (t0 + n) // 128)] if isq else [kT]
            if t0 == 0:
                p.cp(dst, q32[:, 0:n], [q32], dW)
            else:
                p.tt(dst, q32[:, 0:n], sq[:, 0:n], ALU.add, [q32, sq], dW)
    for tt in range(NT):
        ps = p.next_ps()
        for kk in range(8):
            p.mm(ps[:, 0:256], hT[:, kk, tt * 128:(tt + 1) * 128], wv[:, kk, :], kk == 0, (kk == 7 and kind == 1), [hT, wv], [ps])
        if kind == 0:
            bcast_rows(p, k, ps[:, 0:256], ps, bvrow[0:1, :], bvrow, False, True)
        p.cp(V[:, tt, :], ps[:, 0:256], [ps], [V], eng="act")
    p.release()
    p.mark()
    p.ps = Ring(k.pstiles[0:4])
    if kind == 0:
        mlo = p.sb([128, 512], BF16, "mlo")
        mhi = p.sb([128, 512], BF16, "mhi")
        p.dma("pool", mlo[:], k.cin["mask_lo"], writes=[mlo])
        p.dma("pool", mhi[:], k.cin["mask_hi"], writes=[mhi])
        srow = p.sb([1, 16], F32, "srow")
        p.dma("sp", srow[0:1, :], inp["win_sink"][j:j + 1, :], writes=[srow])
        p.act(srow[0:1, :], srow[0:1, :], AF.Exp, [srow], [srow])
        sinkexp = p.sb([64, 16], F32, "sinkexp")
        ps = p.next_ps()
        p.mm(ps[0:64, 0:16], k.ones_row[0:1, 0:64], srow[0:1, :], True, True, [k.ones_row, srow], [ps])
        p.cp(sinkexp[:], ps[0:64, 0:16], [ps], [sinkexp])
    wo = p.sb([64, 16, 1024], BF16, "wo")
    wsrc = inp[wname + "_w_o"][j].rearrange("(h d) n -> d h n", d=64)
    for h in range(16):
        p.dma("pool", wo[:, h, :], wsrc[:, h, :], writes=[wo])
    ptr = p.ring(5, [128, 512], BF16, "Pt")
    rdr = p.ring(2, [64, 512], F32, "rd")
    qtiles = list(range(2, NT)) + ([0, 1] if need_ctx else [])
    its = []
    acc_i = 0
    for qt in qtiles:
        if qt < 2:
            keys = [0, 1]
        elif kind == 1:
            keys = list(range(NT))
        else:
            keys = [0, 1] + [kt for kt in (qt - 1, qt, qt + 1) if 2 <= kt < NT]
        for g in range(4):
            acc = (k.psx[(acc_i % 2) * 2], k.psx[(acc_i % 2) * 2 + 1])
            acc_i += 1
            for ki, kt in enumerate(keys):
                its.append((qt, g, ki, kt, len(keys), acc))

    def stage1(it):
        qt, g, ki, kt, nk, acc = it
        rhs = qT[:, 4 * g:4 * g + 4, qt * 128:(qt + 1) * 128]
        psS = p.next_ps()
        p.mm(psS[:, :].rearrange("p (a b) -> p a b", a=4), kT[:, g, kt * 128:(kt + 1) * 128], rhs, True, True, [kT, qTd[qt][g]], [psS])
        pt = ptr.next()
        p.act(pt[:], psS[:, :], AF.Exp, [psS], [pt], scale=0.125)
        if kind == 0 and qt >= 2 and kt >= 2 and kt == qt - 1:
            p.tt(pt[:], pt[:], mlo[:], ALU.mult, [pt, mlo], [pt])
        elif kind == 0 and qt >= 2 and kt >= 2 and kt == qt + 1:
            p.tt(pt[:], pt[:], mhi[:], ALU.mult, [pt, mhi], [pt])
        return pt

    def stage2(it, pt):
        qt, g, ki, kt, nk, (psO, psD) = it
        first, last = ki == 0, ki == nk - 1
        p.mm(psO[0:64, :], V[:, kt, g * 64:(g + 1) * 64], pt[:], first, last, [V, pt], [psO])
        p.mm(psD[0:64, :], ones_bf[:, :], pt[:], first, last, [ones_bf, pt], [psD])
        if not last:
            return
        rhs = qT[:, 4 * g:4 * g + 4, qt * 128:(qt + 1) * 128]
        rd = rdr.next()
        if kind == 0:
            for a in range(4):
                h = 4 * g + a
                p.ts(rd[:, a * 128:(a + 1) * 128], psD[0:64, a * 128:(a + 1) * 128], sinkexp[:, h:h + 1], None, ALU.add, None, [psD, sinkexp], [rd])
            p.recip(rd[:], rd[:], [rd], [rd])
        else:
            p.recip(rd[:], psD[0:64, :], [psD], [rd])
        p.tt(rhs, psO[0:64, :].rearrange("p (a b) -> p a b", a=4), rd[:, :].rearrange("p (a b) -> p a b", a=4), ALU.mult, [psO, rd], [qTd[qt][g]])

    queue = []
    for it in its:
        queue.append((it, stage1(it)))
        if len(queue) > 2:
            stage2(*queue.pop(0))
    while queue:
        stage2(*queue.pop(0))
    gts = {}
    for s_ in ((0, 1) if need_ctx else (0,)):
        g_ = p.sb([128, 1024], F32, "gt1")
        p.dma("sp", g_[:], k.MODS[l, s_, 2], reads=[k.MODSd], writes=[g_])
        gts[s_] = g_
    if kind == 0:
        borow = p.sb([1, 1024], F32, "borow")
        p.dma("sp", borow[0:1, :], inp["win_b_o"][j:j + 1, :], writes=[borow])
    xr = p.ring(2, [128, 1024], F32, "xo")
    yr = p.ring(2, [128, 512], F32, "yo")
    for qt in qtiles:
        s_ = 1 if qt < 2 else 0
        x = xr.next()
        p.dma("sp", x[:], k.X[qt * 128:(qt + 1) * 128, :], reads=[k.Xd], writes=[x])
        for nh in range(2):
            ps = p.next_ps()
            for h in range(16):
                p.mm(ps[:, :], qT[:, h, qt * 128:(qt + 1) * 128], wo[:, h, nh * 512:(nh + 1) * 512], h == 0, (h == 15 and kind == 1), [qTd[qt][h // 4], wo], [ps])
            if kind == 0:
                bcast_rows(p, k, ps[:, :], ps, borow[0:1, nh * 512:(nh + 1) * 512], borow, False, True)
            y = yr.next()
            p.tt(y[:], ps[:, :], gts[s_][:, nh * 512:(nh + 1) * 512], ALU.mult, [ps, gts[s_]], [y])
            p.tt(x[:, nh * 512:(nh + 1) * 512], x[:, nh * 512:(nh + 1) * 512], y[:], ALU.add, [x, y], [x])
        p.dma("sp", k.X[qt * 128:(qt + 1) * 128, :], x[:], reads=[x], writes=[k.Xd])
    p.release()
    p.release()


def stage_gdn(p, k, l, j, need_ctx):
    inp = k.inp
    nc = k.nc
    w_in = inp["gdn_w_in"][j]
    if not hasattr(k, "gd"):
        k.gd = {n: (nc.dram_tensor("gd_" + n, shp, F32).ap(), Dep("gd_" + n)) for n, shp in (
            ("QT", [8, 128, NTOK]), ("KT", [8, 128, NTOK]), ("Kt", [NTOK, 8, 128]), ("Vt", [NTOK, 8, 128]),
            ("Z", [NTOK, 1024]), ("O0", [NTOK, 1024]), ("O1", [NTOK, 1024]))}
    gd = k.gd
    p.mark()
    BG = p.sb([128, NT, 32], F32, "BG")
    p.mark()
    hT = p.sb([128, 8, NTOK], BF16, "hT")
    wbg = p.sb([128, 8, 32], F32, "wbg")
    for kk in range(8):
        p.dma("sp", wbg[:, kk, :], w_in[kk * 128:(kk + 1) * 128, 4096:4128], writes=[wbg])
    r2 = p.sb([1, 32], F32, "r2")
    p.dma("sp", r2[0:1, 0:16], inp["gdn_dt_bias"][j].rearrange("a b -> (a b)").rearrange("(o n) -> o n", o=1), writes=[r2])
    p.dma("sp", r2[0:1, 16:32], inp["gdn_a_log"][j].rearrange("a b -> (a b)").rearrange("(o n) -> o n", o=1), writes=[r2])
    p.act(r2[0:1, 16:32], r2[0:1, 16:32], AF.Exp, [r2], [r2])
    p.ts(r2[0:1, 16:32], r2[0:1, 16:32], -1.0, None, ALU.mult, None, [r2], [r2])
    dtb = p.sb([128, 32], F32, "dtb")
    ps = p.next_ps()
    bcast_rows(p, k, ps[:, 0:32], ps, r2[0:1, :], r2, True, True)
    p.cp(dtb[:], ps[:, 0:32], [ps], [dtb])
    tmpr = p.ring(2, [128, 32], F32, "bgtmp")

    def cb(tt, ps):
        t = tmpr.next()
        p.cp(t[:], ps[:, 0:32], [ps], [t], eng="act")
        p.act(BG[:, tt, 0:16], t[:, 0:16], AF.Sigmoid, [t], [BG])
        p.tt(t[:, 16:32], t[:, 16:32], dtb[:, 0:16], ALU.add, [t, dtb], [t])
        p.act(t[:, 16:32], t[:, 16:32], AF.Exp, [t], [t])
        p.act(t[:, 16:32], t[:, 16:32], AF.Ln, [t], [t], bias=1.0, scale=1.0)
        p.tt(BG[:, tt, 16:32], t[:, 16:32], dtb[:, 16:32], ALU.mult, [t, dtb], [BG])

    stage_norm(p, k, l, 0, list(range(NT)), hT, p32={"w": wbg, "n": 32, "cb": cb})
    cw = p.sb([128, 24 * 5], F32, "cw")
    for c in range(24):
        load_T(p, k, cw[:, c * 5:(c + 1) * 5], cw, inp["gdn_conv_w"][j][:, c * 128:(c + 1) * 128], 5)
    ones128 = p.sb([128, 128], F32, "ones128")
    p.memset(ones128[:], 1.0, [ones128])
    pcr = p.ring(2, [128, 260], F32, "pc")
    plr = p.ring(2, [128, 2052], F32, "pl")
    for t_ in pcr.tiles + plr.tiles:
        p.memset(t_[:], 0.0, [t_])
    ycr = p.ring(2, [128, 256], F32, "yc")
    ylr = p.ring(2, [128, 2048], F32, "yl")
    wcr = p.ring(2, [128, 8, 128], BF16, "wc")
    sqr = p.ring(2, [128, 512], F32, "gsq")
    rnr = p.ring(2, [128, 512], F32, "grn")
    tkr = p.ring(3, [128, 128], F32, "tok")
    segs = ((0, 256), (256, 2048))

    def proj(c):
        wc = wcr.next()
        for kk in range(8):
            p.dma("pool", wc[:, kk, :], w_in[kk * 128:(kk + 1) * 128, c * 128:(c + 1) * 128], writes=[wc])
        bufs = (pcr.next(), plr.next())
        for (t0, n), buf in zip(segs, bufs):
            for blk in range(0, n, 512):
                nb = min(512, n - blk)
                ps = p.next_ps()
                for kk in range(8):
                    p.mm(ps[:, 0:nb], wc[:, kk, :], hT[:, kk, t0 + blk:t0 + blk + nb], kk == 0, kk == 7, [wc, hT], [ps])
                p.cp(buf[:, 2 + blk:2 + blk + nb], ps[:, 0:nb], [ps], [buf], eng="act")
        return bufs

    def post(c, bufs):
        for (t0, n), buf, y in zip(segs, bufs, (ycr.next(), ylr.next())):
            p.ts(y[:, 0:n], buf[:, 0:n], cw[:, c * 5:c * 5 + 1], None, ALU.mult, None, [buf, cw], [y])
            for jj in range(1, 5):
                p.stt(y[:, 0:n], buf[:, jj:jj + n], cw[:, c * 5 + jj:c * 5 + jj + 1], y[:, 0:n], ALU.mult, ALU.add, [buf, cw, y], [y])
            p.act(y[:, 0:n], y[:, 0:n], AF.Silu, [y], [y])
            if c < 16:
                scale = (128.0 ** -0.5) if c < 8 else 1.0
                for blk in range(0, n, 512):
                    nb = min(512, n - blk)
                    sq = sqr.next()
                    p.act(sq[:, 0:nb], y[:, blk:blk + nb], AF.Square, [y], [sq])
                    ps = p.next_ps()
                    p.mm(ps[:, 0:nb], ones128[:, :], sq[:, 0:nb], True, True, [ones128, sq], [ps])
                    rn = rnr.next()
                    p.ts(rn[:, 0:nb], ps[:, 0:nb], 1e-6, None, ALU.add, None, [ps], [rn])
                    p.recip(rn[:, 0:nb], rn[:, 0:nb], [rn], [rn])
                    p.act(rn[:, 0:nb], rn[:, 0:nb], AF.Sqrt, [rn], [rn])
                    p.stt(y[:, blk:blk + nb], y[:, blk:blk + nb], scale, rn[:, 0:nb], ALU.mult, ALU.mult, [y, rn], [y])
                dst = gd["QT"] if c < 8 else gd["KT"]
                p.dma("sp", dst[0][c % 8, :, t0:t0 + n], y[:, 0:n], reads=[y], writes=[dst[1]])
            if c >= 8:
                dst = gd["Kt"] if c < 16 else gd["Vt"]
                for ti in range(n // 128):
                    ps = p.next_ps()
                    p.tr(ps[:, 0:128], y[:, ti * 128:(ti + 1) * 128], k.ident[:], [y, k.ident], [ps])
                    tk = tkr.next()
                    p.cp(tk[:], ps[:, 0:128], [ps], [tk], eng="act")
                    r0 = t0 + ti * 128
                    p.dma("sp", dst[0][r0:r0 + 128, c % 8, :], tk[:], reads=[tk], writes=[dst[1]])

    nxt = proj(0)
    for c in range(24):
        cur = nxt
        if c + 1 < 24:
            nxt = proj(c + 1)
        post(c, cur)
    wz = p.sb([128, 8, 1024], BF16, "wz")
    for kk in range(8):
        p.dma("pool", wz[:, kk, :], w_in[kk * 128:(kk + 1) * 128, 3072:4096], writes=[wz])
    ztr = p.ring(2, [128, 1024], F32, "zt")
    for tt in range(NT):
        zt = ztr.next()
        for nh in range(2):
            ps = p.next_ps()
            for kk in range(8):
                p.mm(ps[:, :], hT[:, kk, tt * 128:(tt + 1) * 128], wz[:, kk, nh * 512:(nh + 1) * 512], kk == 0, kk == 7, [hT, wz], [ps])
            p.act(zt[:, nh * 512:(nh + 1) * 512], ps[:, :], AF.Silu, [ps], [zt])
        p.dma("sp", gd["Z"][0][tt * 128:(tt + 1) * 128, :], zt[:], reads=[zt], writes=[gd["Z"][1]])
    p.release()
    p.mark()
    cn = {}
    for n_ in ("L0", "L1", "SelC0", "SelC1", "SelA0", "SelA1", "SelB0", "SelB1", "Mb0", "Mb1", "Mn0", "Mn1"):
        t_ = p.sb([128, 128], F32, "c" + n_)
        p.dma("sp", t_[:], k.cin["gd_" + n_], writes=[t_])
        cn[n_] = t_
    rowm = p.sb([128, 2], F32, "rowm")
    p.dma("sp", rowm[:], k.cin["gd_rowm"], writes=[rowm])
    ones128 = p.sb([128, 128], F32, "ones128b")
    p.memset(ones128[:], 1.0, [ones128])
    ident = k.ident
    S = [[p.sb([128, 128], F32, f"S{d}{h}") for h in range(8)] for d in range(2)]
    for d in range(2):
        for h in range(8):
            p.memset(S[d][h][:], 0.0, [S[d][h]], eng="pool")
    H = range(8)
    VH = [(d, h) for h in range(8) for d in range(2)]

    def mk(name):
        return {vh: p.sb([128, 128], F32, f"{name}{vh[0]}{vh[1]}") for vh in VH}
    kTh, qTh, kh, vh_ = mk("kTh"), mk("qTh"), mk("kh"), mk("vh")
    dg, E1, E2, Bm, iT = mk("dg"), mk("E1"), mk("E2"), mk("Bm"), mk("iT")
    Pa, Pb, PTa, PTb, RT = mk("Pa"), mk("Pb"), mk("PTa"), mk("PTb"), mk("RT")
    qs1, qs2, osb = mk("qs1"), mk("qs2"), mk("osb")
    vb, kbg, kdF, kdS, u, wT, v1, v2 = Bm, Pb, PTa, PTb, dg, E1, E2, Pa
    scr = p.ring(4, [128, 64], F32, "gsc")
    order = [list(range(NT)), [1, 0] + list(range(NT - 1, 1, -1))]
    rows = [slice(0, 64), slice(64, 128)]
    for s_ in range(NT):
        tts = [order[0][s_], order[1][s_]]
        fis = [0, 1]
        ses = [1, 0]
        scs = [scr.next(), scr.next()]
        for d in range(2):
            tt, sc, fi, se = tts[d], scs[d], fis[d], ses[d]
            ps = p.next_ps()
            p.mm(ps[:, 0:8], cn[f"L{d}"][:, :], BG[:, tt, 16 + d * 8:24 + d * 8], True, True, [cn[f"L{d}"], BG], [ps])
            p.cp(sc[:, 0:8], ps[:, 0:8], [ps], [sc])
            ps = p.next_ps()
            for ci, nm_ in enumerate(("SelC", "SelA", "SelB")):
                p.mm(ps[:, ci * 8:(ci + 1) * 8], cn[f"{nm_}{d}"][:, :], sc[:, 0:8], True, True, [cn[f"{nm_}{d}"], sc], [ps])
            p.cp(sc[:, 8:32], ps[:, 0:24], [ps], [sc])
            p.act(sc[:, 32:40], sc[:, 0:8], AF.Exp, [sc], [sc])
            p.tt(sc[:, 40:48], sc[:, 8:16], sc[:, 0:8], ALU.subtract, [sc], [sc])
            p.act(sc[:, 40:48], sc[:, 40:48], AF.Exp, [sc], [sc])
            p.act(sc[:, 16:32], sc[:, 16:32], AF.Exp, [sc], [sc])
            p.ts(sc[:, 48:56], sc[:, 40:48], rowm[:, se:se + 1], None, ALU.mult, None, [sc, rowm], [sc])
            p.ts(sc[:, 40:48], sc[:, 40:48], rowm[:, fi:fi + 1], None, ALU.mult, None, [sc, rowm], [sc])
            p.ts(sc[:, 56:64], BG[:, tt, d * 8:d * 8 + 8], -1.0, None, ALU.mult, None, [BG], [sc])
            p.tt(sc[:, 8:16], BG[:, tt, d * 8:d * 8 + 8], sc[:, 32:40], ALU.mult, [BG, sc], [sc])
        for (d, h) in VH:
            vh = (d, h)
            r0 = tts[d] * 128
            p.dma("sp", kTh[vh][:], gd["KT"][0][h, :, r0:r0 + 128], reads=[gd["KT"][1]], writes=[kTh[vh]])
            p.dma("sp", qTh[vh][:], gd["QT"][0][h, :, r0:r0 + 128], reads=[gd["QT"][1]], writes=[qTh[vh]])
            p.dma("sp", kh[vh][:], gd["Kt"][0][r0:r0 + 128, h, :], reads=[gd["Kt"][1]], writes=[kh[vh]])
            p.dma("sp", vh_[vh][:], gd["Vt"][0][r0:r0 + 128, h, :], reads=[gd["Vt"][1]], writes=[vh_[vh]])
        for (d, h) in VH:
            vh = (d, h)
            sc = scs[d]
            gcs = sc[:, h:h + 1]
            p.ts(dg[vh][:], ident[:], gcs, 0.0, ALU.mult, ALU.add, [ident, sc], [dg[vh]], eng="pool")
            psKK = p.next_ps()
            p.mm(psKK[:, 0:128], kTh[vh][:, :], kTh[vh][:, :], True, True, [kTh[vh]], [psKK])
            psKQ = p.next_ps()
            p.mm(psKQ[:, 0:128], kTh[vh][:, :], qTh[vh][:, :], True, True, [kTh[vh], qTh[vh]], [psKQ])
            psBc = p.next_ps()
            p.mm(psBc[:, 0:128], ones128[:, :], dg[vh][:, :], True, True, [ones128, dg[vh]], [psBc])
            p.stt(E1[vh][:], psBc[:, 0:128], gcs, cn[f"Mb{d}"][:], ALU.subtract, ALU.max, [psBc, sc, cn[f"Mb{d}"]], [E1[vh]])
            p.stt(E2[vh][:], psBc[:, 0:128], gcs, cn[f"Mn{d}"][:], ALU.subtract, ALU.min, [psBc, sc, cn[f"Mn{d}"]], [E2[vh]])
            p.act(E1[vh][:], E1[vh][:], AF.Exp, [E1[vh]], [E1[vh]], scale=-1.0)
            p.act(E2[vh][:], E2[vh][:], AF.Exp, [E2[vh]], [E2[vh]])
            p.stt(Bm[vh][:], psKK[:, 0:128], sc[:, 56 + h:57 + h], E1[vh][:], ALU.mult, ALU.mult, [psKK, sc, E1[vh]], [Bm[vh]])
            p.tt(iT[vh][:], psKQ[:, 0:128], E2[vh][:], ALU.mult, [psKQ, E2[vh]], [iT[vh]])
        for vh in VH:
            ps = p.next_ps()
            p.tr(ps[:, 0:128], Bm[vh][:, :], ident[:], [Bm[vh], ident], [ps])
            p.cp(PTa[vh][:], ps[:, 0:128], [ps], [PTa[vh]], eng="act")
            p.tt(RT[vh][:], PTa[vh][:], ident[:], ALU.add, [PTa[vh], ident], [RT[vh]])
        Pc, PTc, Pn, PTn = Bm, PTa, Pa, PTb
        for step in range(5):
            for vh in VH:
                ps1 = p.next_ps()
                p.mm(ps1[:, 0:128], PTc[vh][:, :], Pc[vh][:, :], True, True, [PTc[vh], Pc[vh]], [ps1])
                p.cp(Pn[vh][:], ps1[:, 0:128], [ps1], [Pn[vh]], eng="act")
                if step < 4:
                    ps2 = p.next_ps()
                    p.mm(ps2[:, 0:128], Pc[vh][:, :], PTc[vh][:, :], True, True, [Pc[vh], PTc[vh]], [ps2])
                    p.cp(PTn[vh][:], ps2[:, 0:128], [ps2], [PTn[vh]])
            for vh in VH:
                ps3 = p.next_ps()
                p.mm(ps3[:, 0:128], Pn[vh][:, :], RT[vh][:, :], True, True, [Pn[vh], RT[vh]], [ps3])
                p.tt(RT[vh][:], RT[vh][:], ps3[:, 0:128], ALU.add, [RT[vh], ps3], [RT[vh]])
            Pc, PTc = Pn, PTn
            Pn = Pb if Pn is Pa else Pa
            PTn = PTa if PTn is PTb else PTb
        for (d, h) in VH:
            vh = (d, h)
            sc, tt = scs[d], tts[d]
            p.act(vb[vh][:], vh_[vh][:], AF.Copy, [vh_[vh], BG], [vb[vh]], scale=BG[:, tt, d * 8 + h:d * 8 + h + 1])
            p.ts(kbg[vh][:], kh[vh][:], sc[:, 8 + h:9 + h], None, ALU.mult, None, [kh[vh], sc], [kbg[vh]])
            p.ts(kdF[vh][:], kh[vh][:], sc[:, 40 + h:41 + h], 0.0, ALU.mult, ALU.add, [kh[vh], sc], [kdF[vh]], eng="pool")
            p.act(kdS[vh][:], kh[vh][:], AF.Copy, [kh[vh], sc], [kdS[vh]], scale=sc[:, 48 + h:49 + h])
            psu = p.next_ps()
            p.mm(psu[:, 0:128], RT[vh][:, :], vb[vh][:, :], True, True, [RT[vh], vb[vh]], [psu])
            p.cp(u[vh][:], psu[:, 0:128], [psu], [u[vh]], eng="act")
            psw = p.next_ps()
            p.mm(psw[:, 0:128], kbg[vh][:, :], RT[vh][:, :], True, True, [kbg[vh], RT[vh]], [psw])
            p.cp(wT[vh][:], psw[:, 0:128], [psw], [wT[vh]])
        for (d, h) in VH:
            vh = (d, h)
            Sd = S[d][h]
            ps = p.next_ps()
            p.mm(ps[:, 0:128], wT[vh][:, :], Sd[:, :], True, True, [wT[vh], Sd], [ps])
            p.tt(v1[vh][:], u[vh][:], ps[:, 0:128], ALU.subtract, [u[vh], ps], [v1[vh]])
            ps = p.next_ps()
            p.mm(ps[:, 0:128], qTh[vh][:, :], Sd[:, :], True, True, [qTh[vh], Sd], [ps])
            p.cp(qs1[vh][:], ps[:, 0:128], [ps], [qs1[vh]], eng="act")
        for (d, h) in VH:
            vh = (d, h)
            Sd, sc, fi = S[d][h], scs[d], fis[d]
            ps = p.next_ps()
            p.mm(ps[:, 0:128], kdF[vh][:, :], v1[vh][:, :], True, True, [kdF[vh], v1[vh]], [ps])
            p.stt(Sd[:], Sd[:], sc[:, 16 + fi * 8 + h:17 + fi * 8 + h], ps[:, 0:128], ALU.mult, ALU.add, [Sd, sc, ps], [Sd])
        for (d, h) in VH:
            vh = (d, h)
            Sd = S[d][h]
            ps = p.next_ps()
            p.mm(ps[:, 0:128], wT[vh][:, :], Sd[:, :], True, True, [wT[vh], Sd], [ps])
            p.tt(v2[vh][:], u[vh][:], ps[:, 0:128], ALU.subtract, [u[vh], ps], [v2[vh]])
            ps = p.next_ps()
            p.mm(ps[:, 0:128], qTh[vh][:, :], Sd[:, :], True, True, [qTh[vh], Sd], [ps])
            p.cp(qs2[vh][:], ps[:, 0:128], [ps], [qs2[vh]], eng="act")
        for (d, h) in VH:
            vh = (d, h)
            Sd, sc, se = S[d][h], scs[d], ses[d]
            p.cp(v1[vh][rows[se], :], v2[vh][rows[se], :], [v2[vh]], [v1[vh]], eng="act")
            ps = p.next_ps()
            p.mm(ps[:, 0:128], kdS[vh][:, :], v2[vh][:, :], True, True, [kdS[vh], v2[vh]], [ps])
            p.stt(Sd[:], Sd[:], sc[:, 16 + se * 8 + h:17 + se * 8 + h], ps[:, 0:128], ALU.mult, ALU.add, [Sd, sc, ps], [Sd])
        for (d, h) in VH:
            vh = (d, h)
            sc, fi, se = scs[d], fis[d], ses[d]
            r0 = tts[d] * 128
            ps = p.next_ps()
            p.mm(ps[:, 0:128], iT[vh][:, :], v1[vh][:, :], True, True, [iT[vh], v1[vh]], [ps])
            p.cp(osb[vh][:], ps[:, 0:128], [ps], [osb[vh]], eng="act")
            for (rr, qs) in ((rows[fi], qs1), (rows[se], qs2)):
                p.stt(osb[vh][rr, :], qs[vh][rr, :], sc[rr, 32 + h:33 + h], osb[vh][rr, :], ALU.mult, ALU.add, [qs[vh], sc, osb[vh]], [osb[vh]])
            od = gd[f"O{d}"]
            p.dma("sp", od[0][r0:r0 + 128, h * 128:(h + 1) * 128], osb[vh][:], reads=[osb[vh]], writes=[od[1]])
    p.release()
    p.mark()
    grow = p.sb([1, 1024], F32, "gorow")
    for h in H:
        p.dma("sp", grow[0:1, h * 128:(h + 1) * 128], inp["gdn_g_out"][j:j + 1, :], writes=[grow])
    gob = p.sb([128, 1024], F32, "gob")
    for nh in range(2):
        ps = p.next_ps()
        bcast_rows(p, k, ps[:, :], ps, grow[0:1, nh * 512:(nh + 1) * 512], grow, True, True)
        p.cp(gob[:, nh * 512:(nh + 1) * 512], ps[:, :], [ps], [gob], eng="act")
    wo = p.sb([128, 8, 1024], BF16, "gwo")
    for kk in range(8):
        p.dma("pool", wo[:, kk, :], inp["gdn_w_o"][j, kk * 128:(kk + 1) * 128, :], writes=[wo])
    gts = {}
    for s_ in ((0, 1) if need_ctx else (0,)):
        g_ = p.sb([128, 1024], F32, "ggt1")
        p.dma("sp", g_[:], k.MODS[l, s_, 2], reads=[k.MODSd], writes=[g_])
        gts[s_] = g_
    o0r = p.ring(2, [128, 1024], F32, "o0")
    o1r = p.ring(2, [128, 1024], F32, "o1")
    zr = p.ring(2, [128, 1024], F32, "zz")
    xr = p.ring(2, [128, 1024], F32, "gx")
    sq = p.sb([128, 1024], F32, "gsq2")
    ssr = p.ring(2, [128, 8], F32, "gss")
    oTr = p.ring(2, [128, 8, 128], BF16, "goT")
    yr = p.ring(2, [128, 512], F32, "gy")
    tiles = list(range(NT)) if need_ctx else list(range(2, NT))
    for tt in tiles:
        s_ = 1 if tt < 2 else 0
        r0 = tt * 128
        o0, o1, zz, x = o0r.next(), o1r.next(), zr.next(), xr.next()
        p.dma("sp", o0[:], gd["O0"][0][r0:r0 + 128, :], reads=[gd["O0"][1]], writes=[o0])
        p.dma("sp", o1[:], gd["O1"][0][r0:r0 + 128, :], reads=[gd["O1"][1]], writes=[o1])
        p.dma("sp", zz[:], gd["Z"][0][r0:r0 + 128, :], reads=[gd["Z"][1]], writes=[zz])
        p.dma("sp", x[:], k.X[r0:r0 + 128, :], reads=[k.Xd], writes=[x])
        p.tt(o0[:], o0[:], o1[:], ALU.add, [o0, o1], [o0])
        p.act(sq[:], o0[:], AF.Square, [o0], [sq])
        ss = ssr.next()
        p.op("dve", lambda e, ss=ss: e.reduce_sum(out=ss[:], in_=sq[:, :].rearrange("p (h d) -> p h d", h=8), axis=AX.X), [sq], [ss])
        rstd_of(p, k, ss[:], 128, 1e-6, [ss], [ss])
        for h in H:
            p.ts(o0[:, h * 128:(h + 1) * 128], o0[:, h * 128:(h + 1) * 128], ss[:, h:h + 1], None, ALU.mult, None, [o0, ss], [o0])
        p.tt(o0[:], o0[:], gob[:], ALU.mult, [o0, gob], [o0], eng="pool")
        p.tt(o0[:], o0[:], zz[:], ALU.mult, [o0, zz], [o0])
        oT = oTr.next()
        for half in range(2):
            ps = p.next_ps()
            for q in range(4):
                kk = half * 4 + q
                p.tr(ps[:, q * 128:(q + 1) * 128], o0[:, kk * 128:(kk + 1) * 128], k.ident[:], [o0, k.ident], [ps])
            for q in range(4):
                p.cp(oT[:, half * 4 + q, :], ps[:, q * 128:(q + 1) * 128], [ps], [oT], eng="act")
        for nh in range(2):
            ps = p.next_ps()
            for kk in range(8):
                p.mm(ps[:, :], oT[:, kk, :], wo[:, kk, nh * 512:(nh + 1) * 512], kk == 0, kk == 7, [oT, wo], [ps])
            y = yr.next()
            p.tt(y[:], ps[:, :], gts[s_][:, nh * 512:(nh + 1) * 512], ALU.mult, [ps, gts[s_]], [y])
            p.tt(x[:, nh * 512:(nh + 1) * 512], x[:, nh * 512:(nh + 1) * 512], y[:], ALU.add, [x, y], [x])
        p.dma("sp", k.X[r0:r0 + 128, :], x[:], reads=[x], writes=[k.Xd])
    p.release()
    p.release()


def stage_final(p, k):
    p.mark()
    gr = p.sb([1, 1024], F32, "gfrow")
    p.dma("sp", gr[0:1, :], k.inp["g_final"].rearrange("(o n) -> o n", o=1), writes=[gr])
    gb = p.sb([128, 1024], F32, "gfb")
    for nh in range(2):
        ps = p.next_ps()
        bcast_rows(p, k, ps[:, :], ps, gr[0:1, nh * 512:(nh + 1) * 512], gr, True, True)
        p.cp(gb[:, nh * 512:(nh + 1) * 512], ps[:, :], [ps], [gb], eng="act")
    xr = p.ring(2, [128, 1024], F32, "xf")
    sq = p.sb([128, 1024], F32, "sqf")
    ssr = p.ring(2, [128, 1], F32, "ssf")
    for tt in range(2, NT):
        x = xr.next()
        p.dma("sp", x[:], k.X[tt * 128:(tt + 1) * 128, :], reads=[k.Xd], writes=[x])
        ss = ssr.next()
        p.act(sq[:], x[:], AF.Square, [x], [sq, ss], accum_out=ss[:])
        rstd_of(p, k, ss[:], D, 1e-6, [ss], [ss])
        p.stt(x[:], x[:], ss[:, 0:1], gb[:], ALU.mult, ALU.mult, [x, ss, gb], [x])
        p.dma("sp", k.out[(tt - 2) * 128:(tt - 1) * 128, :], x[:], reads=[x], writes=[k.outd])
    p.release()


def build(cfg=None):
    cfg = cfg or {"stages": "all"}
    nc = bass.Bass("TRN2", target_bir_lowering=False)
    k = K()
    k.nc = nc
    k.inp = LazyInputs(nc)
    consts = host_consts()
    k.cin = {n: nc.dram_tensor("k_" + n, list(v.shape), F32, kind="ExternalInput").ap() for n, v in consts.items()}
    k.out = nc.dram_tensor("out", [NLAT, D], F32, kind="ExternalOutput").ap()
    k.outd = Dep("out")
    k.X = nc.dram_tensor("Xres", [NTOK, D], F32).ap()
    k.Xd = Dep("X")
    k.MODS = nc.dram_tensor("MODS", [DEPTH, 2, 6, 128, 1024], F32).ap()
    k.MODSd = Dep("MODS")
    dbg = cfg.get("debug_out", {})
    k.dbg = {n: nc.dram_tensor("dbg_" + n, s, F32, kind="ExternalOutput").ap() for n, s in dbg.items()}
    k.dbgd = Dep("dbg")
    p = Prog(nc)
    k.pstiles = [p.psum([128, 512], F32, f"psb{i}") for i in range(8)]
    p.ps = Ring(k.pstiles)
    k.psx = k.pstiles[4:8]
    k.ident = p.sb([128, 128], F32, "ident")
    p.dma("sp", k.ident[:], k.cin["ident"], writes=[k.ident])
    k.ones_row = p.sb([1, 128], F32, "ones_row")
    p.memset(k.ones_row[:], 1.0, [k.ones_row])
    k.ltmp = p.ring(2, [128, 128], F32, "ltmp")
    stages = cfg["stages"]
    if stages == "all":
        stages = [("init",), ("mod", list(range(DEPTH)))]
        for l in range(DEPTH):
            stages += [("mixer", l), ("ffn", l)]
        stages += [("final",)]
    for st in stages:
        if st[0] == "init":
            stage_init(p, k)
        elif st[0] == "mod":
            stage_mod(p, k, st[1])
        elif st[0] == "ffn":
            l = st[1]
            tiles = list(range(NT)) if l < DEPTH - 1 else list(range(2, NT))
            p.mark()
            hT = p.sb([128, 8, NTOK], BF16, "hT")
            G = p.sb([128, NT, NE], F32, "G")
            stage_norm(p, k, l, 1, tiles, hT, router=({"G": G} if not cfg.get("no_router") else None))
            if "G" in k.dbg:
                for tt in tiles:
                    p.dma("sp", k.dbg["G"][tt * 128:(tt + 1) * 128, :], G[:, tt, :], reads=[G], writes=[k.dbgd])
            if not cfg.get("skip_moe"):
                stage_moe(p, k, l, tiles, hT, G)
            p.release()
        elif st[0] == "final":
            stage_final(p, k)
        elif st[0] == "dumpX":
            p.dma("sp", k.dbg["X"], k.X, reads=[k.Xd], writes=[k.dbgd])
        elif st[0] == "mixer":
            l = st[1]
            kind, j = l % 3, l // 3
            need_ctx = l < DEPTH - 1
            if kind in (0, 1):
                p.barrier()
                p.ps = Ring(k.pstiles[0:4])
                stage_attn(p, k, l, kind, j, need_ctx)
                p.ps = Ring(k.pstiles)
            else:
                stage_gdn(p, k, l, j, need_ctx)
    p.emit()
    k_used = list(k.inp.keys())
    return nc, consts, k_used


def from_mixers(p, k, l):
    raise NotImplementedError


_CACHE = {}


def kernel(**inputs):
    if "nc" not in _CACHE:
        _CACHE["nc"] = build()
    nc, consts, used = _CACHE["nc"]
    n = 8
    in_maps = []
    for b in range(n):
        m = {}
        for name in used:
            a = np.asarray(inputs[name], dtype=np.float32)
            if name in ("x", "c", "ctx"):
                a = a[b]
            m[name] = np.ascontiguousarray(a)
        for cn, cv in consts.items():
            m["k_" + cn] = cv
        in_maps.append(m)
    res = run_bass_kernel_spmd(nc, in_maps, core_ids=list(range(n)))
    return np.stack([r["out"] for r in res.results], axis=0).astype(np.float32)
```

```python
import numpy as np
import concourse.bass as bass
import concourse.mybir as mybir
from concourse.bass_utils import run_bass_kernel_spmd
from contextlib import ExitStack

F32 = mybir.dt.float32
BF16 = mybir.dt.bfloat16
ALU = mybir.AluOpType
AF = mybir.ActivationFunctionType
AX = mybir.AxisListType

D = 1024
NCTX = 256
NLAT = 2048
NTOK = NCTX + NLAT
NT = NTOK // 128
DEPTH = 4
NE = 32


class Dep:
    __slots__ = ("name", "w", "r")

    def __init__(self, name=""):
        self.name = name
        self.w = None
        self.r = []


class Tile:
    def __init__(self, t, dep=None):
        self.t = t
        self.dep = dep or Dep()

    def __getitem__(self, k):
        return self.t[k]


class Ring:
    def __init__(self, tiles):
        self.tiles = tiles
        self.i = 0

    def next(self):
        t = self.tiles[self.i % len(self.tiles)]
        self.i += 1
        return t


class Prog:
    ENG = ("pe", "act", "dve", "pool", "sp")

    def __init__(self, nc, n_dma_sems=24):
        self.nc = nc
        self.es = ExitStack()
        self.sem = {}
        self.cnt = {}
        self.ops = {e: [] for e in self.ENG}
        self.waited = {e: {} for e in self.ENG}
        for e in self.ENG:
            self.sem[e] = self.es.enter_context(nc.semaphore("s_" + e))
            self.cnt[e] = 0
        self.dq = {}
        for q in ("sp", "pool", "act"):
            n = n_dma_sems if q != "act" else 8
            sems = [self.es.enter_context(nc.semaphore(f"d_{q}{i}")) for i in range(n)]
            self.dq[q] = {"sems": sems, "tgt": [0] * n, "i": 0}
        self.semobj = {}
        for e in self.ENG:
            self.semobj[("e", e)] = self.sem[e]
        for q, d in self.dq.items():
            for i, s in enumerate(d["sems"]):
                self.semobj[("d", q, i)] = s
        self.stk = [ExitStack()]
        self.n_t = 0
        self.ps = None

    def sb(self, shape, dtype, name=None):
        self.n_t += 1
        name = f"{name or 't'}_{self.n_t}"
        t = self.stk[-1].enter_context(self.nc.sbuf_tensor(name, list(shape), dtype))
        return Tile(t, Dep(name))

    def ring(self, n, shape, dtype, name=None):
        return Ring([self.sb(shape, dtype, name) for _ in range(n)])

    def mark(self):
        self.stk.append(ExitStack())

    def release(self):
        self.barrier()
        self.stk.pop().close()

    def psum(self, shape, dtype=F32, name=None):
        self.n_t += 1
        name = name or f"ps{self.n_t}"
        t = self.nc.alloc_psum_tensor(name, list(shape), dtype)
        return Tile(t, Dep(name))

    @staticmethod
    def _d(x):
        return x.dep if isinstance(x, Tile) else x

    def _collect(self, reads, writes):
        need = {}
        for r in reads:
            d = self._d(r)
            if d.w is not None:
                k, v = d.w
                if need.get(k, 0) < v:
                    need[k] = v
        for w in writes:
            d = self._d(w)
            if d.w is not None:
                k, v = d.w
                if need.get(k, 0) < v:
                    need[k] = v
            for (k, v) in d.r:
                if need.get(k, 0) < v:
                    need[k] = v
        return need

    def _waits(self, eng, need):
        ws = []
        wd = self.waited[eng]
        for k, v in need.items():
            if eng == "pe" and k == ("e", "pe"):
                continue
            if wd.get(k, 0) < v:
                wd[k] = v
                ws.append((k, v))
        return ws

    def _commit(self, tok, reads, writes):
        for r in reads:
            d = self._d(r)
            d.r.append(tok)
            if len(d.r) > 48:
                m = {}
                for k, v in d.r:
                    if m.get(k, 0) < v:
                        m[k] = v
                d.r = list(m.items())
        for w in writes:
            d = self._d(w)
            d.w = tok
            d.r = []

    def op(self, eng, fn, reads=(), writes=()):
        need = self._collect(reads, writes)
        ws = self._waits(eng, need)
        self.cnt[eng] += 1
        tok = (("e", eng), self.cnt[eng])
        self.ops[eng].append((ws, fn, (("e", eng), 1)))
        self._commit(tok, reads, writes)
        return tok

    def dma(self, q, out, in_, reads=(), writes=(), **kw):
        need = self._collect(reads, writes)
        d = self.dq[q]
        i = d["i"] % len(d["sems"])
        d["i"] += 1
        key = ("d", q, i)
        if d["tgt"][i] > 0:
            need[key] = max(need.get(key, 0), d["tgt"][i])
        ws = self._waits(q, need)
        d["tgt"][i] += 16
        tok = (key, d["tgt"][i])
        self.ops[q].append((ws, (lambda e: e.dma_start(out=out, in_=in_, **kw)), (key, 16)))
        self._commit(tok, reads, writes)
        return tok

    def barrier(self):
        need = {}
        for e in self.ENG:
            if self.cnt[e] > 0:
                need[("e", e)] = self.cnt[e]
        for q, d in self.dq.items():
            for i, t in enumerate(d["tgt"]):
                if t > 0:
                    need[("d", q, i)] = t
        for e in self.ENG:
            ws = []
            wd = self.waited[e]
            for k, v in need.items():
                if k == ("e", e):
                    continue
                if wd.get(k, 0) < v:
                    wd[k] = v
                    ws.append((k, v))
            if ws:
                self.ops[e].append((ws, None, None))

    def act(self, out, in_, func, R, W, **kw):
        return self.op("act", lambda e: e.activation(out=out, in_=in_, func=func, **kw), R, W)

    def ts(self, out, in0, s1, s2, op0, op1, R, W, eng="dve"):
        if op1 is None:
            return self.op(eng, lambda e: e.tensor_scalar(out=out, in0=in0, scalar1=s1, scalar2=None, op0=op0), R, W)
        return self.op(eng, lambda e: e.tensor_scalar(out=out, in0=in0, scalar1=s1, scalar2=s2, op0=op0, op1=op1), R, W)

    def tt(self, out, in0, in1, op, R, W, eng="dve"):
        return self.op(eng, lambda e: e.tensor_tensor(out=out, in0=in0, in1=in1, op=op), R, W)

    def stt(self, out, in0, scalar, in1, op0, op1, R, W, eng="dve"):
        return self.op(eng, lambda e: e.scalar_tensor_tensor(out=out, in0=in0, scalar=scalar, in1=in1, op0=op0, op1=op1), R, W)

    def cp(self, out, in_, R, W, eng="dve"):
        if eng == "act":
            return self.op("act", lambda e: e.copy(out=out, in_=in_), R, W)
        return self.op(eng, lambda e: e.tensor_copy(out=out, in_=in_), R, W)

    def memset(self, ap, val, W, eng="dve"):
        return self.op(eng, lambda e: e.memset(ap, val), (), W)

    def mm(self, out, lhsT, rhs, start, stop, R, W):
        return self.op("pe", lambda e: e.matmul(out, lhsT=lhsT, rhs=rhs, start=start, stop=stop), R, W)

    def tr(self, out, in_, ident, R, W):
        return self.op("pe", lambda e: e.transpose(out=out, in_=in_, identity=ident), R, W)

    def recip(self, out, in_, R, W):
        return self.op("dve", lambda e: e.reciprocal(out=out, in_=in_), R, W)

    def next_ps(self):
        return self.ps.next()

    def emit(self):
        self.barrier()
        nc = self.nc
        with nc.Block() as block:
            def body(eng):
                def run(e):
                    for ws, fn, inc in self.ops[eng]:
                        for k, v in ws:
                            e.wait_ge(self.semobj[k], v)
                        if fn is not None:
                            ins = fn(e)
                            ins.then_inc(self.semobj[inc[0]], inc[1])
                return run
            block.tensor(body("pe"))
            block.scalar(body("act"))
            block.vector(body("dve"))
            block.gpsimd(body("pool"))
            block.sync(body("sp"))
        while self.stk:
            self.stk.pop().close()
        self.es.close()


INPUT_SHAPES = {
    "x": [NLAT, D], "c": [D], "ctx": [NCTX, D], "c_ctx": [D],
    "w_mod": [4, D, 6 * D], "b_mod": [4, 6 * D], "g_mix": [4, D], "g_ffn": [4, D],
    "win_w_qkv": [2, D, 1536], "win_b_qkv": [2, 1536], "win_sink": [2, 16], "win_w_o": [2, D, D], "win_b_o": [2, D],
    "glb_w_qkv": [1, D, 1536], "glb_g_q": [1, 64], "glb_g_k": [1, 64], "glb_w_o": [1, D, D],
    "gdn_w_in": [1, D, 4128], "gdn_conv_w": [1, 5, 3072], "gdn_a_log": [1, 2, 8], "gdn_dt_bias": [1, 2, 8],
    "gdn_g_out": [1, 128], "gdn_w_o": [1, D, D],
    "moe_w_router": [4, D, NE], "moe_b_router": [4, NE], "moe_w_up": [4, NE, D, 2 * D], "moe_b_up": [4, NE, 2 * D],
    "moe_w_down": [4, NE, D, D], "moe_b_down": [4, NE, D], "g_final": [D],
}


def host_consts():
    c = {}
    c["ident"] = np.eye(128, dtype=np.float32)
    a = np.arange(128)
    lo = (a[None, :] <= a[:, None]).astype(np.float32)
    hi = (a[:, None] <= a[None, :]).astype(np.float32)
    c["mask_lo"] = np.tile(lo, (1, 4))
    c["mask_hi"] = np.tile(hi, (1, 4))
    t = np.arange(NLAT)
    pos = np.stack([t // 64, t % 64], 0).astype(np.float32)
    inv = (10000.0 ** (-np.arange(0, 32, 2, dtype=np.float32) / 32)).astype(np.float32)
    cosT = np.zeros((64, NLAT), np.float32)
    sinT = np.zeros((64, NLAT), np.float32)
    PT = np.zeros((64, 64), np.float32)
    for d in range(64):
        ax, r = d // 32, d % 32
        half, f = r // 16, r % 16
        ang = (pos[ax] * inv[f]).astype(np.float32)
        cosT[d] = np.cos(ang)
        sinT[d] = np.sin(ang)
        if half == 0:
            PT[d + 16, d] = -1.0
        else:
            PT[d - 16, d] = 1.0
    c["cosT"] = cosT
    c["sinT"] = sinT
    c["PT"] = PT
    idx = np.arange(128)
    ch = idx // 64
    same = ch[:, None] == ch[None, :]
    le = idx[:, None] <= idx[None, :]
    ge = idx[:, None] >= idx[None, :]
    lt = idx[:, None] < idx[None, :]
    gt = idx[:, None] > idx[None, :]
    f32 = np.float32
    c["gd_L0"] = (same & le).astype(f32)
    c["gd_L1"] = (same & ge).astype(f32)
    for d, last in ((0, (63, 127)), (1, (0, 64))):
        lastv = np.array([last[ci] for ci in ch])
        c[f"gd_SelC{d}"] = (idx[:, None] == lastv[None, :]).astype(f32)
        c[f"gd_SelA{d}"] = np.repeat((idx == last[0]).astype(f32)[:, None], 128, 1)
        c[f"gd_SelB{d}"] = np.repeat((idx == last[1]).astype(f32)[:, None], 128, 1)
    c["gd_Ms0"] = (same & gt).astype(f32)
    c["gd_Ms1"] = (same & lt).astype(f32)
    c["gd_MiT0"] = (same & le).astype(f32)
    c["gd_MiT1"] = (same & ge).astype(f32)
    c["gd_rowm"] = np.stack([(idx < 64), (idx >= 64)], 1).astype(f32)
    for d in (0, 1):
        c[f"gd_Mb{d}"] = ((1.0 - c[f"gd_Ms{d}"]) * 1e4).astype(f32)
        c[f"gd_Mn{d}"] = ((c[f"gd_MiT{d}"] - 1.0) * 1e4).astype(f32)
    return c


class K:
    pass


class LazyInputs(dict):
    def __init__(self, nc):
        super().__init__()
        self.nc = nc

    def __missing__(self, n):
        ap = self.nc.dram_tensor(n, INPUT_SHAPES[n], F32, kind="ExternalInput").ap()
        self[n] = ap
        return ap


def load_T(p, k, dst_ap, dst_tile, src_rows_ap, R, src_dep=None, C=128):
    tmp = k.ltmp.next()
    p.dma("sp", tmp[0:R, 0:C], src_rows_ap, reads=[src_dep] if src_dep else (), writes=[tmp])
    ps = p.next_ps()
    p.tr(ps[0:C, 0:R], tmp[0:R, 0:C], k.ident[0:R, 0:R], [tmp, k.ident], [ps])
    p.cp(dst_ap, ps[0:C, 0:R], [ps], [dst_tile])


def bcast_rows(p, k, ps_ap, ps_tile, row_ap, row_tile, start, stop):
    p.mm(ps_ap, k.ones_row[0:1, :], row_ap, start, stop, [k.ones_row, row_tile], [ps_tile])


def stage_init(p, k):
    p.dma("sp", k.X[0:NCTX, :], k.inp["ctx"], reads=(), writes=[k.Xd])
    p.dma("sp", k.X[NCTX:NTOK, :], k.inp["x"], reads=(), writes=[k.Xd])


def stage_mod(p, k, layers):
    p.mark()
    inp = k.inp
    craw = p.sb([128, 16], F32, "craw")
    load_T(p, k, craw[:, 0:8], craw, inp["c"].rearrange("(k q) -> k q", q=128), 8)
    load_T(p, k, craw[:, 8:16], craw, inp["c_ctx"].rearrange("(k q) -> k q", q=128), 8)
    csil = p.sb([128, 16], F32, "csil")
    p.act(csil[:], craw[:], AF.Silu, [craw], [csil])
    ones_bf = p.sb([128, 128], F32, "ones128")
    p.memset(ones_bf[:], 1.0, [ones_bf])
    lhs = p.sb([128, 16, 128], BF16, "modlhs")
    for j in range(16):
        p.ts(lhs[:, j, :], ones_bf[:], csil[:, j:j + 1], None, ALU.mult, None, [ones_bf, csil], [lhs])
    wring = p.ring(2, [128, 8, 512], BF16, "wmod")
    brow = p.ring(2, [1, 512], F32, "bmodrow")
    grow = p.ring(2, [1, 1024], F32, "grow")
    oring = p.ring(3, [128, 512], F32, "modo")
    gb = [p.sb([128, 1024], F32, "gb0"), p.sb([128, 1024], F32, "gb1")]
    for l in layers:
        for gi, gname in enumerate(("g_mix", "g_ffn")):
            gr = grow.next()
            p.dma("sp", gr[0:1, :], inp[gname][l:l + 1, :], writes=[gr])
            for nh in range(2):
                ps = p.next_ps()
                bcast_rows(p, k, ps[:, :], ps, gr[0:1, nh * 512:(nh + 1) * 512], gr, True, True)
                p.cp(gb[gi][:, nh * 512:(nh + 1) * 512], ps[:, :], [ps], [gb[gi]], eng="act")
        for j in range(6):
            for nh in range(2):
                n0 = j * 1024 + nh * 512
                wt = wring.next()
                p.dma("pool", wt[:, :, :], inp["w_mod"][l].rearrange("(k q) n -> q k n", q=128)[:, :, n0:n0 + 512], writes=[wt])
                br = brow.next()
                p.dma("sp", br[0:1, :], inp["b_mod"][l:l + 1, n0:n0 + 512], writes=[br])
                for s in range(2):
                    ps = p.next_ps()
                    for kk in range(8):
                        p.mm(ps[:, :], lhs[:, s * 8 + kk, :], wt[:, kk, :], kk == 0, False, [lhs, wt], [ps])
                    bcast_rows(p, k, ps[:, :], ps, br[0:1, :], br, False, True)
                    o = oring.next()
                    if j in (1, 4):
                        g = gb[0] if j == 1 else gb[1]
                        p.stt(o[:], ps[:, :], 1.0, g[:, nh * 512:(nh + 1) * 512], ALU.add, ALU.mult, [ps, g], [o])
                    else:
                        p.cp(o[:], ps[:, :], [ps], [o], eng="act")
                    p.dma("sp", k.MODS[l, s, j, :, nh * 512:(nh + 1) * 512], o[:], reads=[o], writes=[k.MODSd])
    p.release()


def rstd_of(p, k, ss, n, eps, R, W):
    p.ts(ss, ss, 1.0 / n, eps, ALU.mult, ALU.add, R, W)
    p.recip(ss, ss, R, W)
    p.act(ss, ss, AF.Sqrt, R, W)


def stage_norm(p, k, l, which, tiles, hT, router=None, p32=None):
    p.mark()
    jsh, jA = (0, 1) if which == 0 else (3, 4)
    modt = {}
    for s in (0, 1):
        a = p.sb([128, 1024], F32, "modA")
        b = p.sb([128, 1024], F32, "modS")
        p.dma("sp", a[:], k.MODS[l, s, jA], reads=[k.MODSd], writes=[a])
        p.dma("sp", b[:], k.MODS[l, s, jsh], reads=[k.MODSd], writes=[b])
        modt[s] = (a, b)
    xr = p.ring(3, [128, 1024], F32, "xn")
    hr = p.ring(3, [128, 1024], F32, "hn")
    sqr = p.ring(2, [128, 1024], F32, "sq")
    ssr = p.ring(3, [128, 1], F32, "ss")
    need32 = router is not None or p32 is not None
    if need32:
        h32r = p.ring(3, [128, 8, 128], F32, "h32")
    if router is not None:
        wr = p.sb([128, 8, NE], F32, "wr")
        for kk in range(8):
            p.dma("sp", wr[:, kk, :], k.inp["moe_w_router"][l, kk * 128:(kk + 1) * 128, :], writes=[wr])
        brr = p.sb([1, NE], F32, "brr")
        p.dma("sp", brr[0:1, :], k.inp["moe_b_router"][l:l + 1, :], writes=[brr])
        lgr = p.ring(2, [128, NE], F32, "lg")
        m8r = p.ring(2, [128, 8], F32, "m8")
        er = p.ring(2, [128, NE], F32, "eg")
        smr = p.ring(4, [128, 1], F32, "sm")

    def s1(tt):
        s = 1 if tt < 2 else 0
        A, S = modt[s]
        x = xr.next()
        p.dma("sp", x[:], k.X[tt * 128:(tt + 1) * 128, :], reads=[k.Xd], writes=[x])
        ss = ssr.next()
        sq = sqr.next()
        p.act(sq[:], x[:], AF.Square, [x], [sq, ss], accum_out=ss[:])
        rstd_of(p, k, ss[:], D, 1e-6, [ss], [ss])
        h = hr.next()
        p.stt(h[:], x[:], ss[:, 0:1], A[:], ALU.mult, ALU.mult, [x, ss, A], [h])
        p.tt(h[:], h[:], S[:], ALU.add, [h, S], [h])
        return h

    def s2(tt, h):
        h32 = h32r.next() if need32 else None
        for half in range(2):
            ps = p.next_ps()
            for q in range(4):
                kk = half * 4 + q
                p.tr(ps[:, q * 128:(q + 1) * 128], h[:, kk * 128:(kk + 1) * 128], k.ident[:], [h, k.ident], [ps])
            for q in range(4):
                if need32:
                    p.cp(h32[:, half * 4 + q, :], ps[:, q * 128:(q + 1) * 128], [ps], [h32], eng="act")
                    p.cp(hT[:, half * 4 + q, tt * 128:(tt + 1) * 128], h32[:, half * 4 + q, :], [h32], [hT])
                else:
                    p.cp(hT[:, half * 4 + q, tt * 128:(tt + 1) * 128], ps[:, q * 128:(q + 1) * 128], [ps], [hT], eng="act")
        return h32

    def s3(tt, h32):
        if p32 is not None:
            ps = p.next_ps()
            n = p32["n"]
            for kk in range(8):
                p.mm(ps[:, 0:n], h32[:, kk, :], p32["w"][:, kk, :], kk == 0, kk == 7, [h32, p32["w"]], [ps])
            p32["cb"](tt, ps)
        if router is not None:
            G = router["G"]
            ps = p.next_ps()
            for kk in range(8):
                p.mm(ps[:, 0:NE], h32[:, kk, :], wr[:, kk, :], kk == 0, False, [h32, wr], [ps])
            bcast_rows(p, k, ps[:, 0:NE], ps, brr[0:1, :], brr, False, True)
            lg = lgr.next()
            p.cp(lg[:], ps[:, 0:NE], [ps], [lg])
            m8 = m8r.next()
            p.op("dve", lambda e, m8=m8, lg=lg: e.max(out=m8[:], in_=lg[:]), [lg], [m8])
            nm = smr.next()
            p.ts(nm[:], m8[:, 0:1], -1.0, None, ALU.mult, None, [m8], [nm])
            eg = er.next()
            p.act(eg[:], lg[:], AF.Exp, [lg, nm], [eg], bias=nm[:, 0:1], scale=1.0)
            sm = smr.next()
            p.stt(eg[:], lg[:], m8[:, 3:4], eg[:], ALU.is_ge, ALU.mult, [lg, m8, eg], [eg])
            p.op("dve", lambda e, sm=sm, eg=eg: e.reduce_sum(out=sm[:], in_=eg[:], axis=AX.X), [eg], [sm])
            p.recip(sm[:], sm[:], [sm], [sm])
            p.ts(G[:, tt, :], eg[:], sm[:, 0:1], None, ALU.mult, None, [eg, sm], [G])

    q1, q2 = [], []
    for tt in tiles:
        q1.append((tt, s1(tt)))
        if len(q1) > 1:
            t_, h_ = q1.pop(0)
            q2.append((t_, s2(t_, h_)))
        if need32 and len(q2) > 1:
            s3(*q2.pop(0))
    while q1:
        t_, h_ = q1.pop(0)
        q2.append((t_, s2(t_, h_)))
    if need32:
        while q2:
            s3(*q2.pop(0))
    p.release()


def stage_moe(p, k, l, tiles, hT, G):
    p.mark()
    inp = k.inp
    t0 = tiles[0]
    ntile = len(tiles)
    acc = p.sb([128, ntile, 1024], F32, "acc")
    bupT = p.sb([128, NE * 16], F32, "bupT")
    bsrc = inp["moe_b_up"][l].rearrange("e (m q) -> (e m) q", q=128)
    for i in range(4):
        load_T(p, k, bupT[:, i * 128:(i + 1) * 128], bupT, bsrc[i * 128:(i + 1) * 128, :], 128)
    p.mark()
    bd = p.sb([NE, 1024], F32, "bd")
    p.dma("sp", bd[:], inp["moe_b_down"][l], writes=[bd])
    gtr = p.ring(2, [NE, 128], F32, "GT")
    for ti, tt in enumerate(tiles):
        ps = p.next_ps()
        p.tr(ps[0:NE, 0:128], G[:, tt, :], k.ident[:], [G, k.ident], [ps])
        gt = gtr.next()
        p.cp(gt[:], ps[0:NE, 0:128], [ps], [gt])
        for nh in range(2):
            ps2 = p.next_ps()
            p.mm(ps2[:, :], gt[:, :], bd[:, nh * 512:(nh + 1) * 512], True, True, [gt, bd], [ps2])
            p.cp(acc[:, ti, nh * 512:(nh + 1) * 512], ps2[:, :], [ps2], [acc], eng="act")
    p.release()
    p.mark()
    blocks = []
    i = 0
    while i < ntile:
        n = min(4, ntile - i)
        blocks.append((i, n))
        i += n
    wur = p.ring(2, [128, 8, 2, 512], BF16, "wu")
    wdr = p.ring(2, [128, 4, 1024], BF16, "wd")
    aTr = [p.ring(3, [128, 512], BF16, f"aT{m}") for m in range(4)]
    t1r = p.ring(3, [128, 512], F32, "t1")
    sgr = p.ring(2, [128, 512], F32, "sg")
    t2r = p.ring(3, [128, 512], F32, "t2")
    accd = [[Dep(f"acc{ti}_{nh}") for nh in range(2)] for ti in range(ntile)]
    for ti in range(ntile):
        for nh in range(2):
            accd[ti][nh].w = acc.dep.w
    pending = []

    def make_down(aT, wd, e, b0, bn):
        def emit():
            for j in range(bn):
                ti = b0 + j
                for nh in range(2):
                    po = p.next_ps()
                    for kk in range(4):
                        p.mm(po[:, :], aT[kk][:, j * 128:(j + 1) * 128], wd[:, kk, nh * 512:(nh + 1) * 512], kk == 0, kk == 3, [aT[kk], wd], [po])
                    av = acc[:, ti, nh * 512:(nh + 1) * 512]
                    p.stt(av, po[:, :], G[:, t0 + ti, e:e + 1], av, ALU.mult, ALU.add, [po, G, accd[ti][nh]], [accd[ti][nh]])
        return emit

    for e in range(NE):
        for half in range(2):
            wu = wur.next()
            wd = wdr.next()
            src = inp["moe_w_up"][l, e].rearrange("r (g h c) -> r g h c", g=2, h=2)
            for kk in range(8):
                p.dma("pool", wu[:, kk, :, :], src[kk * 128:(kk + 1) * 128, :, half, :], writes=[wu])
            for kk in range(4):
                r0 = half * 512 + kk * 128
                p.dma("pool", wd[:, kk, :], inp["moe_w_down"][l, e, r0:r0 + 128, :], writes=[wd])
            for (b0, bn) in blocks:
                tok0 = (t0 + b0) * 128
                ntok = bn * 128
                aT = []
                for m in range(4):
                    pa = p.next_ps()
                    for kk in range(8):
                        p.mm(pa[:, 0:ntok], wu[:, kk, 0, m * 128:(m + 1) * 128], hT[:, kk, tok0:tok0 + ntok], kk == 0, kk == 7, [wu, hT], [pa])
                    pb = p.next_ps()
                    for kk in range(8):
                        p.mm(pb[:, 0:ntok], wu[:, kk, 1, m * 128:(m + 1) * 128], hT[:, kk, tok0:tok0 + ntok], kk == 0, kk == 7, [wu, hT], [pb])
                    ca = e * 16 + half * 4 + m
                    cb = ca + 8
                    t1 = t1r.next()
                    p.ts(t1[:, 0:ntok], pa[:, 0:ntok], bupT[:, ca:ca + 1], 7.0, ALU.add, ALU.min, [pa, bupT], [t1])
                    sg = sgr.next()
                    p.act(sg[:, 0:ntok], t1[:, 0:ntok], AF.Sigmoid, [t1], [sg], scale=1.702)
                    t2 = t2r.next()
                    p.act(t2[:, 0:ntok], pb[:, 0:ntok], AF.Identity, [pb, bupT], [t2], bias=bupT[:, cb:cb + 1], scale=1.0)
                    p.ts(t2[:, 0:ntok], t2[:, 0:ntok], -7.0, 7.0, ALU.max, ALU.min, [t2], [t2])
                    p.tt(t1[:, 0:ntok], t1[:, 0:ntok], sg[:, 0:ntok], ALU.mult, [t1, sg], [t1])
                    a = aTr[m].next()
                    p.stt(a[:, 0:ntok], t2[:, 0:ntok], 1.0, t1[:, 0:ntok], ALU.add, ALU.mult, [t2, t1], [a])
                    aT.append(a)
                if pending:
                    pending.pop(0)()
                pending.append(make_down(aT, wd, e, b0, bn))
    while pending:
        pending.pop(0)()
    p.release()
    gts = {}
    for s in (0, 1):
        g = p.sb([128, 1024], F32, "gt2")
        p.dma("sp", g[:], k.MODS[l, s, 5], reads=[k.MODSd], writes=[g])
        gts[s] = g
    xr = p.ring(2, [128, 1024], F32, "xres")
    for ti, tt in enumerate(tiles):
        s = 1 if tt < 2 else 0
        x = xr.next()
        p.dma("sp", x[:], k.X[tt * 128:(tt + 1) * 128, :], reads=[k.Xd], writes=[x])
        p.tt(acc[:, ti, :], acc[:, ti, :], gts[s][:], ALU.mult, [accd[ti][0], accd[ti][1], gts[s]], [accd[ti][0], accd[ti][1]])
        p.tt(x[:], x[:], acc[:, ti, :], ALU.add, [x, accd[ti][0], accd[ti][1]], [x])
        p.dma("sp", k.X[tt * 128:(tt + 1) * 128, :], x[:], reads=[x], writes=[k.Xd])
    p.release()


def stage_attn(p, k, l, kind, j, need_ctx):
    inp = k.inp
    wname = "win" if kind == 0 else "glb"
    wqkv = inp[wname + "_w_qkv"][j]
    p.mark()
    qT = p.sb([128, 16, NTOK], BF16, "qT")
    qTd = [[Dep(f"qT{t}_{g}") for g in range(4)] for t in range(NT)]
    kT = p.sb([128, 4, NTOK], BF16, "kT")
    V = p.sb([128, NT, 4, 128], BF16, "V")
    p.memset(qT[64:128, :, :], 0.0, [qT], eng="pool")
    p.memset(kT[64:128, :, :], 0.0, [kT], eng="pool")
    p.memset(V[:, :, :, :], 1.0, [V], eng="pool")
    ones_bf = p.sb([128, 64], BF16, "ones_bf")
    p.memset(ones_bf[:], 1.0, [ones_bf])
    p.mark()
    p.ps = Ring(k.pstiles)
    hT = p.sb([128, 8, NTOK], BF16, "hT")
    stage_norm(p, k, l, 0, list(range(NT)), hT)
    cosT = p.sb([64, NLAT], F32, "cosT")
    sinT = p.sb([64, NLAT], F32, "sinT")
    PT = p.sb([64, 64], F32, "PT")
    p.dma("sp", cosT[:], k.cin["cosT"], writes=[cosT])
    p.dma("sp", sinT[:], k.cin["sinT"], writes=[sinT])
    p.dma("sp", PT[:], k.cin["PT"], writes=[PT])
    wq = p.sb([128, 8, 1088], BF16, "wq")
    wk = p.sb([128, 8, 512], BF16, "wkv")
    for kk in range(8):
        p.dma("pool", wq[:, kk, :], wqkv[kk * 128:(kk + 1) * 128, 0:1088], writes=[wq])
        p.dma("pool", wk[:, kk, :], wqkv[kk * 128:(kk + 1) * 128, 1024:1536], writes=[wk])
    if kind == 0:
        bT = p.sb([64, 24], F32, "bT")
        load_T(p, k, bT[:, :], bT, inp["win_b_qkv"][j].rearrange("(h d) -> h d", d=64), 24, C=64)
        bvrow = p.sb([1, 256], F32, "bvrow")
        p.dma("sp", bvrow[0:1, :], inp["win_b_qkv"][j:j + 1, 1280:1536], writes=[bvrow])
    else:
        gqk = p.sb([64, 2], F32, "gqk")
        load_T(p, k, gqk[:, 0:1], gqk, inp["glb_g_q"][j:j + 1, :], 1, C=64)
        load_T(p, k, gqk[:, 1:2], gqk, inp["glb_g_k"][j:j + 1, :], 1, C=64)
        avg64 = p.sb([64, 64], F32, "avg64")
        p.memset(avg64[:], 1.0 / 64, [avg64])
    q32r = p.ring(5, [64, 512], F32, "q32")
    sqr = p.ring(5 if kind == 1 else 4, [64, 512], F32, "qsq")
    blocks = [(0, 256), (256, 512), (768, 512), (1280, 512), (1792, 512)]
    for hh in range(20):
        isq = hh < 16
        items = []
        for (t0, n) in blocks:
            ps = p.next_ps()
            for kk in range(8):
                lw = wq[:, kk, hh * 64:hh * 64 + 128] if isq else wk[:, kk, (hh - 16) * 64:(hh - 16) * 64 + 128]
                p.mm(ps[:, 0:n], lw, hT[:, kk, t0:t0 + n], kk == 0, kk == 7, [wq if isq else wk, hT], [ps])
            q32 = q32r.next()
            if kind == 0:
                p.act(q32[:, 0:n], ps[0:64, 0:n], AF.Identity, [ps, bT], [q32], bias=bT[:, hh:hh + 1], scale=1.0)
            else:
                p.cp(q32[:, 0:n], ps[0:64, 0:n], [ps], [q32], eng="act")
            items.append((t0, n, q32, sqr.next()))
        if kind == 1:
            gcol = gqk[:, 0:1] if isq else gqk[:, 1:2]
            for (t0, n, q32, sq) in items:
                p.act(sq[:, 0:n], q32[:, 0:n], AF.Square, [q32], [sq])
            psns = []
            for (t0, n, q32, sq) in items:
                psn = p.next_ps()
                p.mm(psn[0:64, 0:n], avg64[:, :], sq[:, 0:n], True, True, [avg64, sq], [psn])
                psns.append(psn)
            for (t0, n, q32, sq), psn in zip(items, psns):
                p.ts(sq[:, 0:n], psn[0:64, 0:n], 1e-6, None, ALU.add, None, [psn], [sq])
                p.recip(sq[:, 0:n], sq[:, 0:n], [sq], [sq])
            for (t0, n, q32, sq) in items:
                p.act(sq[:, 0:n], sq[:, 0:n], AF.Sqrt, [sq], [sq])
            for (t0, n, q32, sq) in items:
                p.stt(q32[:, 0:n], q32[:, 0:n], gcol, sq[:, 0:n], ALU.mult, ALU.mult, [q32, gqk, sq], [q32])
        ps2s = {}
        for (t0, n, q32, sq) in items[1:]:
            ps2 = p.next_ps()
            p.mm(ps2[0:64, 0:n], PT[:, :], q32[:, 0:n], True, True, [PT, q32], [ps2])
            ps2s[t0] = ps2
        for (t0, n, q32, sq) in items[1:]:
            l0 = t0 - NCTX
            p.tt(sq[:, 0:n], ps2s[t0][0:64, 0:n], sinT[:, l0:l0 + n], ALU.mult, [ps2s[t0], sinT], [sq])
        for (t0, n, q32, sq) in items[1:]:
            l0 = t0 - NCTX
            p.tt(q32[:, 0:n], q32[:, 0:n], cosT[:, l0:l0 + n], ALU.mult, [q32, cosT], [q32], eng="pool")
        for (t0, n, q32, sq) in items:
            dst = qT[0:64, hh, t0:t0 + n] if isq else kT[0:64, hh - 16, t0:t0 + n]
            dW = [qTd[t][hh // 4] for t in range(t0 // 128, (t0 + n) // 128)] if isq else [kT]
            if t0 == 0:
                p.cp(dst, q32[:, 0:n], [q32], dW)
            else:
                p.tt(dst, q32[:, 0:n], sq[:, 0:n], ALU.add, [q32, sq], dW)
    for tt in range(NT):
        ps = p.next_ps()
        for kk in range(8):
            p.mm(ps[:, 0:256], hT[:, kk, tt * 128:(tt + 1) * 128], wk[:, kk, 256:512], kk == 0, (kk == 7 and kind == 1), [hT, wk], [ps])
        if kind == 0:
            bcast_rows(p, k, ps[:, 0:256], ps, bvrow[0:1, :], bvrow, False, True)
        p.cp(V[:, tt, :, 0:64], ps[:, 0:256].rearrange("p (g d) -> p g d", g=4), [ps], [V])
    p.release()
    p.mark()
    p.ps = Ring(k.pstiles[0:4])
    if kind == 0:
        mlo = p.sb([128, 512], BF16, "mlo")
        mhi = p.sb([128, 512], BF16, "mhi")
        p.dma("pool", mlo[:], k.cin["mask_lo"], writes=[mlo])
        p.dma("pool", mhi[:], k.cin["mask_hi"], writes=[mhi])
        srow = p.sb([1, 16], F32, "srow")
        p.dma("sp", srow[0:1, :], inp["win_sink"][j:j + 1, :], writes=[srow])
        p.act(srow[0:1, :], srow[0:1, :], AF.Exp, [srow], [srow])
        sinkexp = p.sb([128, 16], F32, "sinkexp")
        ps = p.next_ps()
        p.mm(ps[:, 0:16], k.ones_row[0:1, :], srow[0:1, :], True, True, [k.ones_row, srow], [ps])
        p.cp(sinkexp[:], ps[:, 0:16], [ps], [sinkexp])
    wo = p.sb([128, 16, 1024], BF16, "wo")
    p.memset(wo[64:128, :, :], 0.0, [wo])
    wsrc = inp[wname + "_w_o"][j].rearrange("(h d) n -> d h n", d=64)
    for h in range(16):
        p.dma("pool", wo[0:64, h, :], wsrc[:, h, :], writes=[wo])
    ptr = p.ring(5, [128, 512], BF16, "Pt")
    rdr = p.ring(2, [128, 512], F32, "rd")
    qtiles = list(range(2, NT)) + ([0, 1] if need_ctx else [])
    its = []
    acc_i = 0
    for qt in qtiles:
        if qt < 2:
            keys = [0, 1]
        elif kind == 1:
            keys = list(range(NT))
        else:
            keys = [0, 1] + [kt for kt in (qt - 1, qt, qt + 1) if 2 <= kt < NT]
        for g in range(4):
            acc = k.psx[acc_i % 4]
            acc_i += 1
            for ki, kt in enumerate(keys):
                its.append((qt, g, ki, kt, len(keys), acc))

    def stage1(it):
        qt, g, ki, kt, nk, acc = it
        rhs = qT[:, 4 * g:4 * g + 4, qt * 128:(qt + 1) * 128]
        psS = p.next_ps()
        p.mm(psS[:, :].rearrange("p (a b) -> p a b", a=4), kT[:, g, kt * 128:(kt + 1) * 128], rhs, True, True, [kT, qTd[qt][g]], [psS])
        pt = ptr.next()
        p.act(pt[:], psS[:, :], AF.Exp, [psS], [pt], scale=0.125)
        if kind == 0 and qt >= 2 and kt >= 2 and kt == qt - 1:
            p.tt(pt[:], pt[:], mlo[:], ALU.mult, [pt, mlo], [pt])
        elif kind == 0 and qt >= 2 and kt >= 2 and kt == qt + 1:
            p.tt(pt[:], pt[:], mhi[:], ALU.mult, [pt, mhi], [pt])
        return pt

    def stage2(it, pt):
        qt, g, ki, kt, nk, psO = it
        first, last = ki == 0, ki == nk - 1
        p.mm(psO[:, :], V[:, kt, g, :], pt[:], first, last, [V, pt], [psO])
        if not last:
            return
        rd = rdr.next()
        if kind == 0:
            for a_ in range(4):
                h = 4 * g + a_
                p.ts(rd[64:128, a_ * 128:(a_ + 1) * 128], psO[64:128, a_ * 128:(a_ + 1) * 128], sinkexp[64:128, h:h + 1], None, ALU.add, None, [psO, sinkexp], [rd])
            p.recip(rd[64:128, :], rd[64:128, :], [rd], [rd])
        else:
            p.recip(rd[64:128, :], psO[64:128, :], [psO], [rd])
        dst = qT[0:64, 4 * g:4 * g + 4, qt * 128:(qt + 1) * 128]
        p.tt(dst, psO[0:64, :].rearrange("p (a b) -> p a b", a=4), rd[64:128, :].rearrange("p (a b) -> p a b", a=4), ALU.mult, [psO, rd], [qTd[qt][g]])

    queue = []
    for it in its:
        queue.append((it, stage1(it)))
        if len(queue) > 2:
            stage2(*queue.pop(0))
    while queue:
        stage2(*queue.pop(0))
    gts = {}
    for s_ in ((0, 1) if need_ctx else (0,)):
        g_ = p.sb([128, 1024], F32, "gt1")
        p.dma("sp", g_[:], k.MODS[l, s_, 2], reads=[k.MODSd], writes=[g_])
        gts[s_] = g_
    if kind == 0:
        borow = p.sb([1, 1024], F32, "borow")
        p.dma("sp", borow[0:1, :], inp["win_b_o"][j:j + 1, :], writes=[borow])
    xr = p.ring(2, [128, 1024], F32, "xo")
    yr = p.ring(2, [128, 512], F32, "yo")
    for qt in qtiles:
        s_ = 1 if qt < 2 else 0
        x = xr.next()
        p.dma("sp", x[:], k.X[qt * 128:(qt + 1) * 128, :], reads=[k.Xd], writes=[x])
        for nh in range(2):
            ps = p.next_ps()
            for h in range(16):
                p.mm(ps[:, :], qT[:, h, qt * 128:(qt + 1) * 128], wo[:, h, nh * 512:(nh + 1) * 512], h == 0, (h == 15 and kind == 1), [qTd[qt][h // 4], wo], [ps])
            if kind == 0:
                bcast_rows(p, k, ps[:, :], ps, borow[0:1, nh * 512:(nh + 1) * 512], borow, False, True)
            y = yr.next()
            p.tt(y[:], ps[:, :], gts[s_][:, nh * 512:(nh + 1) * 512], ALU.mult, [ps, gts[s_]], [y])
            p.tt(x[:, nh * 512:(nh + 1) * 512], x[:, nh * 512:(nh + 1) * 512], y[:], ALU.add, [x, y], [x])
        p.dma("sp", k.X[qt * 128:(qt + 1) * 128, :], x[:], reads=[x], writes=[k.Xd])
    p.release()
    p.release()


def stage_gdn(p, k, l, j, need_ctx):
    inp = k.inp
    nc = k.nc
    w_in = inp["gdn_w_in"][j]
    if not hasattr(k, "gd"):
        k.gd = {n: (nc.dram_tensor("gd_" + n, shp, F32).ap(), Dep("gd_" + n)) for n, shp in (
            ("QT", [8, 128, NTOK]), ("KT", [8, 128, NTOK]), ("Kt", [NTOK, 8, 128]), ("Vt", [NTOK, 8, 128]),
            ("Z", [NTOK, 1024]), ("O0", [NTOK, 1024]), ("O1", [NTOK, 1024]))}
    gd = k.gd
    p.mark()
    BG = p.sb([128, NT, 32], F32, "BG")
    p.mark()
    hT = p.sb([128, 8, NTOK], BF16, "hT")
    wbg = p.sb([128, 8, 32], F32, "wbg")
    for kk in range(8):
        p.dma("sp", wbg[:, kk, :], w_in[kk * 128:(kk + 1) * 128, 4096:4128], writes=[wbg])
    r2 = p.sb([1, 32], F32, "r2")
    p.dma("sp", r2[0:1, 0:16], inp["gdn_dt_bias"][j].rearrange("a b -> (a b)").rearrange("(o n) -> o n", o=1), writes=[r2])
    p.dma("sp", r2[0:1, 16:32], inp["gdn_a_log"][j].rearrange("a b -> (a b)").rearrange("(o n) -> o n", o=1), writes=[r2])
    p.act(r2[0:1, 16:32], r2[0:1, 16:32], AF.Exp, [r2], [r2])
    p.ts(r2[0:1, 16:32], r2[0:1, 16:32], -1.0, None, ALU.mult, None, [r2], [r2])
    dtb = p.sb([128, 32], F32, "dtb")
    ps = p.next_ps()
    bcast_rows(p, k, ps[:, 0:32], ps, r2[0:1, :], r2, True, True)
    p.cp(dtb[:], ps[:, 0:32], [ps], [dtb])
    tmpr = p.ring(2, [128, 32], F32, "bgtmp")

    def cb(tt, ps):
        t = tmpr.next()
        p.cp(t[:], ps[:, 0:32], [ps], [t], eng="act")
        p.act(BG[:, tt, 0:16], t[:, 0:16], AF.Sigmoid, [t], [BG])
        p.tt(t[:, 16:32], t[:, 16:32], dtb[:, 0:16], ALU.add, [t, dtb], [t])
        p.act(t[:, 16:32], t[:, 16:32], AF.Exp, [t], [t])
        p.act(t[:, 16:32], t[:, 16:32], AF.Ln, [t], [t], bias=1.0, scale=1.0)
        p.tt(BG[:, tt, 16:32], t[:, 16:32], dtb[:, 16:32], ALU.mult, [t, dtb], [BG])

    stage_norm(p, k, l, 0, list(range(NT)), hT, p32={"w": wbg, "n": 32, "cb": cb})
    cw = p.sb([128, 24 * 5], F32, "cw")
    for c in range(24):
        load_T(p, k, cw[:, c * 5:(c + 1) * 5], cw, inp["gdn_conv_w"][j][:, c * 128:(c + 1) * 128], 5)
    ones128 = p.sb([128, 128], F32, "ones128")
    p.memset(ones128[:], 1.0, [ones128])
    pcr = p.ring(2, [128, 260], F32, "pc")
    plr = p.ring(2, [128, 2052], F32, "pl")
    for t_ in pcr.tiles + plr.tiles:
        p.memset(t_[:], 0.0, [t_])
    ycr = p.ring(2, [128, 256], F32, "yc")
    ylr = p.ring(2, [128, 2048], F32, "yl")
    wcr = p.ring(2, [128, 8, 128], BF16, "wc")
    sqr = p.ring(6, [128, 512], F32, "gsq")
    tkr = p.ring(3, [128, 128], F32, "tok")
    segs = ((0, 256), (256, 2048))

    def proj(c):
        wc = wcr.next()
        for kk in range(8):
            p.dma("pool", wc[:, kk, :], w_in[kk * 128:(kk + 1) * 128, c * 128:(c + 1) * 128], writes=[wc])
        bufs = (pcr.next(), plr.next())
        for (t0, n), buf in zip(segs, bufs):
            for blk in range(0, n, 512):
                nb = min(512, n - blk)
                ps = p.next_ps()
                for kk in range(8):
                    p.mm(ps[:, 0:nb], wc[:, kk, :], hT[:, kk, t0 + blk:t0 + blk + nb], kk == 0, kk == 7, [wc, hT], [ps])
                p.cp(buf[:, 2 + blk:2 + blk + nb], ps[:, 0:nb], [ps], [buf], eng="act")
        return bufs

    def post(c, bufs):
        for (t0, n), buf, y in zip(segs, bufs, (ycr.next(), ylr.next())):
            p.ts(y[:, 0:n], buf[:, 0:n], cw[:, c * 5:c * 5 + 1], None, ALU.mult, None, [buf, cw], [y])
            for jj in range(1, 5):
                p.stt(y[:, 0:n], buf[:, jj:jj + n], cw[:, c * 5 + jj:c * 5 + jj + 1], y[:, 0:n], ALU.mult, ALU.add, [buf, cw, y], [y])
            p.act(y[:, 0:n], y[:, 0:n], AF.Silu, [y], [y])
            if c < 16:
                scale = (128.0 ** -0.5) if c < 8 else 1.0
                bl = [(blk, min(512, n - blk)) for blk in range(0, n, 512)]
                sqs = [sqr.next() for _ in bl]
                for (blk, nb), sq in zip(bl, sqs):
                    p.act(sq[:, 0:nb], y[:, blk:blk + nb], AF.Square, [y], [sq])
                pss = []
                for (blk, nb), sq in zip(bl, sqs):
                    ps = p.next_ps()
                    p.mm(ps[:, 0:nb], ones128[:, :], sq[:, 0:nb], True, True, [ones128, sq], [ps])
                    pss.append(ps)
                for (blk, nb), sq, ps in zip(bl, sqs, pss):
                    p.ts(sq[:, 0:nb], ps[:, 0:nb], 1e-6, None, ALU.add, None, [ps], [sq])
                    p.recip(sq[:, 0:nb], sq[:, 0:nb], [sq], [sq])
                for (blk, nb), sq in zip(bl, sqs):
                    p.act(sq[:, 0:nb], sq[:, 0:nb], AF.Sqrt, [sq], [sq])
                for (blk, nb), sq in zip(bl, sqs):
                    p.stt(y[:, blk:blk + nb], y[:, blk:blk + nb], scale, sq[:, 0:nb], ALU.mult, ALU.mult, [y, sq], [y])
                dst = gd["QT"] if c < 8 else gd["KT"]
                p.dma("sp", dst[0][c % 8, :, t0:t0 + n], y[:, 0:n], reads=[y], writes=[dst[1]])
            if c >= 8:
                dst = gd["Kt"] if c < 16 else gd["Vt"]
                for ti in range(n // 128):
                    ps = p.next_ps()
                    p.tr(ps[:, 0:128], y[:, ti * 128:(ti + 1) * 128], k.ident[:], [y, k.ident], [ps])
                    tk = tkr.next()
                    p.cp(tk[:], ps[:, 0:128], [ps], [tk], eng="act")
                    r0 = t0 + ti * 128
                    p.dma("sp", dst[0][r0:r0 + 128, c % 8, :], tk[:], reads=[tk], writes=[dst[1]])

    nxt = proj(0)
    for c in range(24):
        cur = nxt
        if c + 1 < 24:
            nxt = proj(c + 1)
        post(c, cur)
    wz = p.sb([128, 8, 1024], BF16, "wz")
    for kk in range(8):
        p.dma("pool", wz[:, kk, :], w_in[kk * 128:(kk + 1) * 128, 3072:4096], writes=[wz])
    ztr = p.ring(2, [128, 1024], F32, "zt")
    for tt in range(NT):
        zt = ztr.next()
        for nh in range(2):
            ps = p.next_ps()
            for kk in range(8):
                p.mm(ps[:, :], hT[:, kk, tt * 128:(tt + 1) * 128], wz[:, kk, nh * 512:(nh + 1) * 512], kk == 0, kk == 7, [hT, wz], [ps])
            p.act(zt[:, nh * 512:(nh + 1) * 512], ps[:, :], AF.Silu, [ps], [zt])
        p.dma("sp", gd["Z"][0][tt * 128:(tt + 1) * 128, :], zt[:], reads=[zt], writes=[gd["Z"][1]])
    p.release()
    p.mark()
    cn = {}
    for n_ in ("L0", "L1", "SelC0", "SelC1", "SelA0", "SelA1", "SelB0", "SelB1", "Mb0", "Mb1", "Mn0", "Mn1"):
        t_ = p.sb([128, 128], F32, "c" + n_)
        p.dma("sp", t_[:], k.cin["gd_" + n_], writes=[t_])
        cn[n_] = t_
    rowm = p.sb([128, 2], F32, "rowm")
    p.dma("sp", rowm[:], k.cin["gd_rowm"], writes=[rowm])
    ones128 = p.sb([128, 128], F32, "ones128b")
    p.memset(ones128[:], 1.0, [ones128])
    ident = k.ident
    S = [[p.sb([128, 128], F32, f"S{d}{h}") for h in range(8)] for d in range(2)]
    for d in range(2):
        for h in range(8):
            p.memset(S[d][h][:], 0.0, [S[d][h]], eng="pool")
    H = range(8)
    VH = [(d, h) for h in range(8) for d in range(2)]

    def mk(name):
        return {vh: p.sb([128, 128], F32, f"{name}{vh[0]}{vh[1]}") for vh in VH}
    kTh, qTh, kh, vh_ = mk("kTh"), mk("qTh"), mk("kh"), mk("vh")
    dg, E1, E2, Bm, iT = mk("dg"), mk("E1"), mk("E2"), mk("Bm"), mk("iT")
    Pa, Pb, PTa, PTb, RT = mk("Pa"), mk("Pb"), mk("PTa"), mk("PTb"), mk("RT")
    qs1, qs2, osb = mk("qs1"), mk("qs2"), mk("osb")
    vb, kbg, kdF, kdS, u, wT, v1, v2 = Bm, Pb, PTa, PTb, dg, E1, E2, Pa
    scr = p.ring(4, [128, 64], F32, "gsc")
    order = [list(range(NT)), [1, 0] + list(range(NT - 1, 1, -1))]
    rows = [slice(0, 64), slice(64, 128)]
    for s_ in range(NT):
        tts = [order[0][s_], order[1][s_]]
        fis = [0, 1]
        ses = [1, 0]
        scs = [scr.next(), scr.next()]
        for d in range(2):
            tt, sc, fi, se = tts[d], scs[d], fis[d], ses[d]
            ps = p.next_ps()
            p.mm(ps[:, 0:8], cn[f"L{d}"][:, :], BG[:, tt, 16 + d * 8:24 + d * 8], True, True, [cn[f"L{d}"], BG], [ps])
            p.cp(sc[:, 0:8], ps[:, 0:8], [ps], [sc])
            ps = p.next_ps()
            for ci, nm_ in enumerate(("SelC", "SelA", "SelB")):
                p.mm(ps[:, ci * 8:(ci + 1) * 8], cn[f"{nm_}{d}"][:, :], sc[:, 0:8], True, True, [cn[f"{nm_}{d}"], sc], [ps])
            p.cp(sc[:, 8:32], ps[:, 0:24], [ps], [sc])
            p.act(sc[:, 32:40], sc[:, 0:8], AF.Exp, [sc], [sc])
            p.tt(sc[:, 40:48], sc[:, 8:16], sc[:, 0:8], ALU.subtract, [sc], [sc])
            p.act(sc[:, 40:48], sc[:, 40:48], AF.Exp, [sc], [sc])
            p.act(sc[:, 16:32], sc[:, 16:32], AF.Exp, [sc], [sc])
            p.ts(sc[:, 48:56], sc[:, 40:48], rowm[:, se:se + 1], None, ALU.mult, None, [sc, rowm], [sc])
            p.ts(sc[:, 40:48], sc[:, 40:48], rowm[:, fi:fi + 1], None, ALU.mult, None, [sc, rowm], [sc])
            p.ts(sc[:, 56:64], BG[:, tt, d * 8:d * 8 + 8], -1.0, None, ALU.mult, None, [BG], [sc])
            p.tt(sc[:, 8:16], BG[:, tt, d * 8:d * 8 + 8], sc[:, 32:40], ALU.mult, [BG, sc], [sc])
        for (d, h) in VH:
            vh = (d, h)
            r0 = tts[d] * 128
            p.dma("sp", kTh[vh][:], gd["KT"][0][h, :, r0:r0 + 128], reads=[gd["KT"][1]], writes=[kTh[vh]])
            p.dma("sp", qTh[vh][:], gd["QT"][0][h, :, r0:r0 + 128], reads=[gd["QT"][1]], writes=[qTh[vh]])
            p.dma("sp", kh[vh][:], gd["Kt"][0][r0:r0 + 128, h, :], reads=[gd["Kt"][1]], writes=[kh[vh]])
            p.dma("sp", vh_[vh][:], gd["Vt"][0][r0:r0 + 128, h, :], reads=[gd["Vt"][1]], writes=[vh_[vh]])
        for (d, h) in VH:
            vh = (d, h)
            sc = scs[d]
            gcs = sc[:, h:h + 1]
            p.ts(dg[vh][:], ident[:], gcs, 0.0, ALU.mult, ALU.add, [ident, sc], [dg[vh]], eng="pool")
            psKK = p.next_ps()
            p.mm(psKK[:, 0:128], kTh[vh][:, :], kTh[vh][:, :], True, True, [kTh[vh]], [psKK])
            psKQ = p.next_ps()
            p.mm(psKQ[:, 0:128], kTh[vh][:, :], qTh[vh][:, :], True, True, [kTh[vh], qTh[vh]], [psKQ])
            psBc = p.next_ps()
            p.mm(psBc[:, 0:128], ones128[:, :], dg[vh][:, :], True, True, [ones128, dg[vh]], [psBc])
            p.stt(E1[vh][:], psBc[:, 0:128], gcs, cn[f"Mb{d}"][:], ALU.subtract, ALU.max, [psBc, sc, cn[f"Mb{d}"]], [E1[vh]])
            p.stt(E2[vh][:], psBc[:, 0:128], gcs, cn[f"Mn{d}"][:], ALU.subtract, ALU.min, [psBc, sc, cn[f"Mn{d}"]], [E2[vh]])
            p.act(E1[vh][:], E1[vh][:], AF.Exp, [E1[vh]], [E1[vh]], scale=-1.0)
            p.act(E2[vh][:], E2[vh][:], AF.Exp, [E2[vh]], [E2[vh]])
            p.stt(Bm[vh][:], psKK[:, 0:128], sc[:, 56 + h:57 + h], E1[vh][:], ALU.mult, ALU.mult, [psKK, sc, E1[vh]], [Bm[vh]])
            p.tt(iT[vh][:], psKQ[:, 0:128], E2[vh][:], ALU.mult, [psKQ, E2[vh]], [iT[vh]])
        for vh in VH:
            ps = p.next_ps()
            p.tr(ps[:, 0:128], Bm[vh][:, :], ident[:], [Bm[vh], ident], [ps])
            p.cp(PTa[vh][:], ps[:, 0:128], [ps], [PTa[vh]], eng="act")
            p.tt(RT[vh][:], PTa[vh][:], ident[:], ALU.add, [PTa[vh], ident], [RT[vh]])
        Pc, PTc, Pn, PTn = Bm, PTa, Pa, PTb
        for step in range(5):
            for vh in VH:
                ps1 = p.next_ps()
                p.mm(ps1[:, 0:128], PTc[vh][:, :], Pc[vh][:, :], True, True, [PTc[vh], Pc[vh]], [ps1])
                p.cp(Pn[vh][:], ps1[:, 0:128], [ps1], [Pn[vh]], eng="act")
                if step < 4:
                    ps2 = p.next_ps()
                    p.mm(ps2[:, 0:128], Pc[vh][:, :], PTc[vh][:, :], True, True, [Pc[vh], PTc[vh]], [ps2])
                    p.cp(PTn[vh][:], ps2[:, 0:128], [ps2], [PTn[vh]])
            for vh in VH:
                ps3 = p.next_ps()
                p.mm(ps3[:, 0:128], Pn[vh][:, :], RT[vh][:, :], True, True, [Pn[vh], RT[vh]], [ps3])
                p.tt(RT[vh][:], RT[vh][:], ps3[:, 0:128], ALU.add, [RT[vh], ps3], [RT[vh]])
            Pc, PTc = Pn, PTn
            Pn = Pb if Pn is Pa else Pa
            PTn = PTa if PTn is PTb else PTb
        for (d, h) in VH:
            vh = (d, h)
            sc, tt = scs[d], tts[d]
            p.act(vb[vh][:], vh_[vh][:], AF.Copy, [vh_[vh], BG], [vb[vh]], scale=BG[:, tt, d * 8 + h:d * 8 + h + 1])
            p.ts(kbg[vh][:], kh[vh][:], sc[:, 8 + h:9 + h], None, ALU.mult, None, [kh[vh], sc], [kbg[vh]])
            p.ts(kdF[vh][:], kh[vh][:], sc[:, 40 + h:41 + h], 0.0, ALU.mult, ALU.add, [kh[vh], sc], [kdF[vh]], eng="pool")
            p.act(kdS[vh][:], kh[vh][:], AF.Copy, [kh[vh], sc], [kdS[vh]], scale=sc[:, 48 + h:49 + h])
            psu = p.next_ps()
            p.mm(psu[:, 0:128], RT[vh][:, :], vb[vh][:, :], True, True, [RT[vh], vb[vh]], [psu])
            p.cp(u[vh][:], psu[:, 0:128], [psu], [u[vh]], eng="act")
            psw = p.next_ps()
            p.mm(psw[:, 0:128], kbg[vh][:, :], RT[vh][:, :], True, True, [kbg[vh], RT[vh]], [psw])
            p.cp(wT[vh][:], psw[:, 0:128], [psw], [wT[vh]])
        for (d, h) in VH:
            vh = (d, h)
            Sd = S[d][h]
            ps = p.next_ps()
            p.mm(ps[:, 0:128], wT[vh][:, :], Sd[:, :], True, True, [wT[vh], Sd], [ps])
            p.tt(v1[vh][:], u[vh][:], ps[:, 0:128], ALU.subtract, [u[vh], ps], [v1[vh]])
            ps = p.next_ps()
            p.mm(ps[:, 0:128], qTh[vh][:, :], Sd[:, :], True, True, [qTh[vh], Sd], [ps])
            p.cp(qs1[vh][:], ps[:, 0:128], [ps], [qs1[vh]], eng="act")
        for (d, h) in VH:
            vh = (d, h)
            Sd, sc, fi = S[d][h], scs[d], fis[d]
            ps = p.next_ps()
            p.mm(ps[:, 0:128], kdF[vh][:, :], v1[vh][:, :], True, True, [kdF[vh], v1[vh]], [ps])
            p.stt(Sd[:], Sd[:], sc[:, 16 + fi * 8 + h:17 + fi * 8 + h], ps[:, 0:128], ALU.mult, ALU.add, [Sd, sc, ps], [Sd])
        for (d, h) in VH:
            vh = (d, h)
            Sd = S[d][h]
            ps = p.next_ps()
            p.mm(ps[:, 0:128], wT[vh][:, :], Sd[:, :], True, True, [wT[vh], Sd], [ps])
            p.tt(v2[vh][:], u[vh][:], ps[:, 0:128], ALU.subtract, [u[vh], ps], [v2[vh]])
            ps = p.next_ps()
            p.mm(ps[:, 0:128], qTh[vh][:, :], Sd[:, :], True, True, [qTh[vh], Sd], [ps])
            p.cp(qs2[vh][:], ps[:, 0:128], [ps], [qs2[vh]], eng="act")
        for (d, h) in VH:
            vh = (d, h)
            Sd, sc, se = S[d][h], scs[d], ses[d]
            p.cp(v1[vh][rows[se], :], v2[vh][rows[se], :], [v2[vh]], [v1[vh]], eng="act")
            ps = p.next_ps()
            p.mm(ps[:, 0:128], kdS[vh][:, :], v2[vh][:, :], True, True, [kdS[vh], v2[vh]], [ps])
            p.stt(Sd[:], Sd[:], sc[:, 16 + se * 8 + h:17 + se * 8 + h], ps[:, 0:128], ALU.mult, ALU.add, [Sd, sc, ps], [Sd])
        for (d, h) in VH:
            vh = (d, h)
            sc, fi, se = scs[d], fis[d], ses[d]
            r0 = tts[d] * 128
            ps = p.next_ps()
            p.mm(ps[:, 0:128], iT[vh][:, :], v1[vh][:, :], True, True, [iT[vh], v1[vh]], [ps])
            p.cp(osb[vh][:], ps[:, 0:128], [ps], [osb[vh]], eng="act")
            for (rr, qs) in ((rows[fi], qs1), (rows[se], qs2)):
                p.stt(osb[vh][rr, :], qs[vh][rr, :], sc[rr, 32 + h:33 + h], osb[vh][rr, :], ALU.mult, ALU.add, [qs[vh], sc, osb[vh]], [osb[vh]])
            od = gd[f"O{d}"]
            p.dma("sp", od[0][r0:r0 + 128, h * 128:(h + 1) * 128], osb[vh][:], reads=[osb[vh]], writes=[od[1]])
    p.release()
    p.mark()
    grow = p.sb([1, 1024], F32, "gorow")
    for h in H:
        p.dma("sp", grow[0:1, h * 128:(h + 1) * 128], inp["gdn_g_out"][j:j + 1, :], writes=[grow])
    gob = p.sb([128, 1024], F32, "gob")
    for nh in range(2):
        ps = p.next_ps()
        bcast_rows(p, k, ps[:, :], ps, grow[0:1, nh * 512:(nh + 1) * 512], grow, True, True)
        p.cp(gob[:, nh * 512:(nh + 1) * 512], ps[:, :], [ps], [gob], eng="act")
    wo = p.sb([128, 8, 1024], BF16, "gwo")
    for kk in range(8):
        p.dma("pool", wo[:, kk, :], inp["gdn_w_o"][j, kk * 128:(kk + 1) * 128, :], writes=[wo])
    gts = {}
    for s_ in ((0, 1) if need_ctx else (0,)):
        g_ = p.sb([128, 1024], F32, "ggt1")
        p.dma("sp", g_[:], k.MODS[l, s_, 2], reads=[k.MODSd], writes=[g_])
        gts[s_] = g_
    o0r = p.ring(2, [128, 1024], F32, "o0")
    o1r = p.ring(2, [128, 1024], F32, "o1")
    zr = p.ring(2, [128, 1024], F32, "zz")
    xr = p.ring(2, [128, 1024], F32, "gx")
    sq = p.sb([128, 1024], F32, "gsq2")
    ssr = p.ring(2, [128, 8], F32, "gss")
    oTr = p.ring(2, [128, 8, 128], BF16, "goT")
    yr = p.ring(2, [128, 512], F32, "gy")
    tiles = list(range(NT)) if need_ctx else list(range(2, NT))
    for tt in tiles:
        s_ = 1 if tt < 2 else 0
        r0 = tt * 128
        o0, o1, zz, x = o0r.next(), o1r.next(), zr.next(), xr.next()
        p.dma("sp", o0[:], gd["O0"][0][r0:r0 + 128, :], reads=[gd["O0"][1]], writes=[o0])
        p.dma("sp", o1[:], gd["O1"][0][r0:r0 + 128, :], reads=[gd["O1"][1]], writes=[o1])
        p.dma("sp", zz[:], gd["Z"][0][r0:r0 + 128, :], reads=[gd["Z"][1]], writes=[zz])
        p.dma("sp", x[:], k.X[r0:r0 + 128, :], reads=[k.Xd], writes=[x])
        p.tt(o0[:], o0[:], o1[:], ALU.add, [o0, o1], [o0])
        p.act(sq[:], o0[:], AF.Square, [o0], [sq])
        ss = ssr.next()
        p.op("dve", lambda e, ss=ss: e.reduce_sum(out=ss[:], in_=sq[:, :].rearrange("p (h d) -> p h d", h=8), axis=AX.X), [sq], [ss])
        rstd_of(p, k, ss[:], 128, 1e-6, [ss], [ss])
        for h in H:
            p.ts(o0[:, h * 128:(h + 1) * 128], o0[:, h * 128:(h + 1) * 128], ss[:, h:h + 1], None, ALU.mult, None, [o0, ss], [o0])
        p.tt(o0[:], o0[:], gob[:], ALU.mult, [o0, gob], [o0], eng="pool")
        p.tt(o0[:], o0[:], zz[:], ALU.mult, [o0, zz], [o0])
        oT = oTr.next()
        for half in range(2):
            ps = p.next_ps()
            for q in range(4):
                kk = half * 4 + q
                p.tr(ps[:, q * 128:(q + 1) * 128], o0[:, kk * 128:(kk + 1) * 128], k.ident[:], [o0, k.ident], [ps])
            for q in range(4):
                p.cp(oT[:, half * 4 + q, :], ps[:, q * 128:(q + 1) * 128], [ps], [oT], eng="act")
        for nh in range(2):
            ps = p.next_ps()
            for kk in range(8):
                p.mm(ps[:, :], oT[:, kk, :], wo[:, kk, nh * 512:(nh + 1) * 512], kk == 0, kk == 7, [oT, wo], [ps])
            y = yr.next()
            p.tt(y[:], ps[:, :], gts[s_][:, nh * 512:(nh + 1) * 512], ALU.mult, [ps, gts[s_]], [y])
            p.tt(x[:, nh * 512:(nh + 1) * 512], x[:, nh * 512:(nh + 1) * 512], y[:], ALU.add, [x, y], [x])
        p.dma("sp", k.X[r0:r0 + 128, :], x[:], reads=[x], writes=[k.Xd])
    p.release()
    p.release()


def stage_final(p, k):
    p.mark()
    gr = p.sb([1, 1024], F32, "gfrow")
    p.dma("sp", gr[0:1, :], k.inp["g_final"].rearrange("(o n) -> o n", o=1), writes=[gr])
    gb = p.sb([128, 1024], F32, "gfb")
    for nh in range(2):
        ps = p.next_ps()
        bcast_rows(p, k, ps[:, :], ps, gr[0:1, nh * 512:(nh + 1) * 512], gr, True, True)
        p.cp(gb[:, nh * 512:(nh + 1) * 512], ps[:, :], [ps], [gb], eng="act")
    xr = p.ring(2, [128, 1024], F32, "xf")
    sq = p.sb([128, 1024], F32, "sqf")
    ssr = p.ring(2, [128, 1], F32, "ssf")
    for tt in range(2, NT):
        x = xr.next()
        p.dma("sp", x[:], k.X[tt * 128:(tt + 1) * 128, :], reads=[k.Xd], writes=[x])
        ss = ssr.next()
        p.act(sq[:], x[:], AF.Square, [x], [sq, ss], accum_out=ss[:])
        rstd_of(p, k, ss[:], D, 1e-6, [ss], [ss])
        p.stt(x[:], x[:], ss[:, 0:1], gb[:], ALU.mult, ALU.mult, [x, ss, gb], [x])
        p.dma("sp", k.out[(tt - 2) * 128:(tt - 1) * 128, :], x[:], reads=[x], writes=[k.outd])
    p.release()


def build(cfg=None):
    cfg = cfg or {"stages": "all"}
    nc = bass.Bass("TRN2", target_bir_lowering=False)
    k = K()
    k.nc = nc
    k.inp = LazyInputs(nc)
    consts = host_consts()
    k.cin = {n: nc.dram_tensor("k_" + n, list(v.shape), F32, kind="ExternalInput").ap() for n, v in consts.items()}
    k.out = nc.dram_tensor("out", [NLAT, D], F32, kind="ExternalOutput").ap()
    k.outd = Dep("out")
    k.X = nc.dram_tensor("Xres", [NTOK, D], F32).ap()
    k.Xd = Dep("X")
    k.MODS = nc.dram_tensor("MODS", [DEPTH, 2, 6, 128, 1024], F32).ap()
    k.MODSd = Dep("MODS")
    dbg = cfg.get("debug_out", {})
    k.dbg = {n: nc.dram_tensor("dbg_" + n, s, F32, kind="ExternalOutput").ap() for n, s in dbg.items()}
    k.dbgd = Dep("dbg")
    p = Prog(nc)
    k.pstiles = [p.psum([128, 512], F32, f"psb{i}") for i in range(8)]
    p.ps = Ring(k.pstiles)
    k.psx = k.pstiles[4:8]
    k.ident = p.sb([128, 128], F32, "ident")
    p.dma("sp", k.ident[:], k.cin["ident"], writes=[k.ident])
    k.ones_row = p.sb([1, 128], F32, "ones_row")
    p.memset(k.ones_row[:], 1.0, [k.ones_row])
    k.ltmp = p.ring(2, [128, 128], F32, "ltmp")
    stages = cfg["stages"]
    if stages == "all":
        stages = [("init",), ("mod", list(range(DEPTH)))]
        for l in range(DEPTH):
            stages += [("mixer", l), ("ffn", l)]
        stages += [("final",)]
    for st in stages:
        if st[0] == "init":
            stage_init(p, k)
        elif st[0] == "mod":
            stage_mod(p, k, st[1])
        elif st[0] == "ffn":
            l = st[1]
            tiles = list(range(NT)) if l < DEPTH - 1 else list(range(2, NT))
            p.mark()
            hT = p.sb([128, 8, NTOK], BF16, "hT")
            G = p.sb([128, NT, NE], F32, "G")
            stage_norm(p, k, l, 1, tiles, hT, router=({"G": G} if not cfg.get("no_router") else None))
            if "G" in k.dbg:
                for tt in tiles:
                    p.dma("sp", k.dbg["G"][tt * 128:(tt + 1) * 128, :], G[:, tt, :], reads=[G], writes=[k.dbgd])
            if not cfg.get("skip_moe"):
                stage_moe(p, k, l, tiles, hT, G)
            p.release()
        elif st[0] == "final":
            stage_final(p, k)
        elif st[0] == "dumpX":
            p.dma("sp", k.dbg["X"], k.X, reads=[k.Xd], writes=[k.dbgd])
        elif st[0] == "mixer":
            l = st[1]
            kind, j = l % 3, l // 3
            need_ctx = l < DEPTH - 1
            if kind in (0, 1):
                p.barrier()
                p.ps = Ring(k.pstiles[0:4])
                stage_attn(p, k, l, kind, j, need_ctx)
                p.ps = Ring(k.pstiles)
            else:
                stage_gdn(p, k, l, j, need_ctx)
    p.emit()
    k_used = list(k.inp.keys())
    return nc, consts, k_used


def from_mixers(p, k, l):
    raise NotImplementedError


_CACHE = {}


def kernel(**inputs):
    if "nc" not in _CACHE:
        _CACHE["nc"] = build()
    nc, consts, used = _CACHE["nc"]
    n = 8
    in_maps = []
    for b in range(n):
        m = {}
        for name in used:
            a = np.asarray(inputs[name], dtype=np.float32)
            if name in ("x", "c", "ctx"):
                a = a[b]
            m[name] = np.ascontiguousarray(a)
        for cn, cv in consts.items():
            m["k_" + cn] = cv
        in_maps.append(m)
    res = run_bass_kernel_spmd(nc, in_maps, core_ids=list(range(n)))
    return np.stack([r["out"] for r in res.results], axis=0).astype(np.float32)
```

```python
import numpy as np
import concourse.bass as bass
import concourse.mybir as mybir
from concourse.bass_utils import run_bass_kernel_spmd
from contextlib import ExitStack

F32 = mybir.dt.float32
BF16 = mybir.dt.bfloat16
ALU = mybir.AluOpType
AF = mybir.ActivationFunctionType
AX = mybir.AxisListType

D = 1024
NCTX = 256
NLAT = 2048
NTOK = NCTX + NLAT
NT = NTOK // 128
DEPTH = 4
NE = 32


class Dep:
    __slots__ = ("name", "w", "r")

    def __init__(self, name=""):
        self.name = name
        self.w = None
        self.r = []


class Tile:
    def __init__(self, t, dep=None):
        self.t = t
        self.dep = dep or Dep()

    def __getitem__(self, k):
        return self.t[k]


class Ring:
    def __init__(self, tiles):
        self.tiles = tiles
        self.i = 0

    def next(self):
        t = self.tiles[self.i % len(self.tiles)]
        self.i += 1
        return t


class Prog:
    ENG = ("pe", "act", "dve", "pool", "sp")

    def __init__(self, nc, n_dma_sems=24):
        self.nc = nc
        self.es = ExitStack()
        self.sem = {}
        self.cnt = {}
        self.ops = {e: [] for e in self.ENG}
        self.waited = {e: {} for e in self.ENG}
        for e in self.ENG:
            self.sem[e] = self.es.enter_context(nc.semaphore("s_" + e))
            self.cnt[e] = 0
        self.dq = {}
        for q in ("sp", "pool", "act"):
            n = n_dma_sems if q != "act" else 8
            sems = [self.es.enter_context(nc.semaphore(f"d_{q}{i}")) for i in range(n)]
            self.dq[q] = {"sems": sems, "tgt": [0] * n, "i": 0}
        self.semobj = {}
        for e in self.ENG:
            self.semobj[("e", e)] = self.sem[e]
        for q, d in self.dq.items():
            for i, s in enumerate(d["sems"]):
                self.semobj[("d", q, i)] = s
        self.stk = [ExitStack()]
        self.n_t = 0
        self.ps = None

    def sb(self, shape, dtype, name=None):
        self.n_t += 1
        name = f"{name or 't'}_{self.n_t}"
        t = self.stk[-1].enter_context(self.nc.sbuf_tensor(name, list(shape), dtype))
        return Tile(t, Dep(name))

    def ring(self, n, shape, dtype, name=None):
        return Ring([self.sb(shape, dtype, name) for _ in range(n)])

    def mark(self):
        self.stk.append(ExitStack())

    def release(self):
        self.barrier()
        self.stk.pop().close()

    def psum(self, shape, dtype=F32, name=None):
        self.n_t += 1
        name = name or f"ps{self.n_t}"
        t = self.nc.alloc_psum_tensor(name, list(shape), dtype)
        return Tile(t, Dep(name))

    @staticmethod
    def _d(x):
        return x.dep if isinstance(x, Tile) else x

    def _collect(self, reads, writes):
        need = {}
        for r in reads:
            d = self._d(r)
            if d.w is not None:
                k, v = d.w
                if need.get(k, 0) < v:
                    need[k] = v
        for w in writes:
            d = self._d(w)
            if d.w is not None:
                k, v = d.w
                if need.get(k, 0) < v:
                    need[k] = v
            for (k, v) in d.r:
                if need.get(k, 0) < v:
                    need[k] = v
        return need

    def _waits(self, eng, need):
        ws = []
        wd = self.waited[eng]
        for k, v in need.items():
            if eng == "pe" and k == ("e", "pe"):
                continue
            if wd.get(k, 0) < v:
                wd[k] = v
                ws.append((k, v))
        return ws

    def _commit(self, tok, reads, writes):
        for r in reads:
            d = self._d(r)
            d.r.append(tok)
            if len(d.r) > 48:
                m = {}
                for k, v in d.r:
                    if m.get(k, 0) < v:
                        m[k] = v
                d.r = list(m.items())
        for w in writes:
            d = self._d(w)
            d.w = tok
            d.r = []

    def op(self, eng, fn, reads=(), writes=()):
        need = self._collect(reads, writes)
        ws = self._waits(eng, need)
        self.cnt[eng] += 1
        tok = (("e", eng), self.cnt[eng])
        self.ops[eng].append((ws, fn, (("e", eng), 1)))
        self._commit(tok, reads, writes)
        return tok

    def dma(self, q, out, in_, reads=(), writes=(), **kw):
        need = self._collect(reads, writes)
        d = self.dq[q]
        i = d["i"] % len(d["sems"])
        d["i"] += 1
        key = ("d", q, i)
        if d["tgt"][i] > 0:
            need[key] = max(need.get(key, 0), d["tgt"][i])
        ws = self._waits(q, need)
        d["tgt"][i] += 16
        tok = (key, d["tgt"][i])
        self.ops[q].append((ws, (lambda e: e.dma_start(out=out, in_=in_, **kw)), (key, 16)))
        self._commit(tok, reads, writes)
        return tok

    def barrier(self):
        need = {}
        for e in self.ENG:
            if self.cnt[e] > 0:
                need[("e", e)] = self.cnt[e]
        for q, d in self.dq.items():
            for i, t in enumerate(d["tgt"]):
                if t > 0:
                    need[("d", q, i)] = t
        for e in self.ENG:
            ws = []
            wd = self.waited[e]
            for k, v in need.items():
                if k == ("e", e):
                    continue
                if wd.get(k, 0) < v:
                    wd[k] = v
                    ws.append((k, v))
            if ws:
                self.ops[e].append((ws, None, None))

    def act(self, out, in_, func, R, W, **kw):
        return self.op("act", lambda e: e.activation(out=out, in_=in_, func=func, **kw), R, W)

    def ts(self, out, in0, s1, s2, op0, op1, R, W, eng="dve"):
        if op1 is None:
            return self.op(eng, lambda e: e.tensor_scalar(out=out, in0=in0, scalar1=s1, scalar2=None, op0=op0), R, W)
        return self.op(eng, lambda e: e.tensor_scalar(out=out, in0=in0, scalar1=s1, scalar2=s2, op0=op0, op1=op1), R, W)

    def tt(self, out, in0, in1, op, R, W, eng="dve"):
        return self.op(eng, lambda e: e.tensor_tensor(out=out, in0=in0, in1=in1, op=op), R, W)

    def stt(self, out, in0, scalar, in1, op0, op1, R, W, eng="dve"):
        return self.op(eng, lambda e: e.scalar_tensor_tensor(out=out, in0=in0, scalar=scalar, in1=in1, op0=op0, op1=op1), R, W)

    def cp(self, out, in_, R, W, eng="dve"):
        if eng == "act":
            return self.op("act", lambda e: e.copy(out=out, in_=in_), R, W)
        return self.op(eng, lambda e: e.tensor_copy(out=out, in_=in_), R, W)

    def memset(self, ap, val, W, eng="dve"):
        return self.op(eng, lambda e: e.memset(ap, val), (), W)

    def mm(self, out, lhsT, rhs, start, stop, R, W):
        return self.op("pe", lambda e: e.matmul(out, lhsT=lhsT, rhs=rhs, start=start, stop=stop), R, W)

    def tr(self, out, in_, ident, R, W):
        return self.op("pe", lambda e: e.transpose(out=out, in_=in_, identity=ident), R, W)

    def recip(self, out, in_, R, W):
        return self.op("dve", lambda e: e.reciprocal(out=out, in_=in_), R, W)

    def next_ps(self):
        return self.ps.next()

    def emit(self):
        self.barrier()
        nc = self.nc
        with nc.Block() as block:
            def body(eng):
                def run(e):
                    for ws, fn, inc in self.ops[eng]:
                        for k, v in ws:
                            e.wait_ge(self.semobj[k], v)
                        if fn is not None:
                            ins = fn(e)
                            ins.then_inc(self.semobj[inc[0]], inc[1])
                return run
            block.tensor(body("pe"))
            block.scalar(body("act"))
            block.vector(body("dve"))
            block.gpsimd(body("pool"))
            block.sync(body("sp"))
        while self.stk:
            self.stk.pop().close()
        self.es.close()


INPUT_SHAPES = {
    "x": [NLAT, D], "c": [D], "ctx": [NCTX, D], "c_ctx": [D],
    "w_mod": [4, D, 6 * D], "b_mod": [4, 6 * D], "g_mix": [4, D], "g_ffn": [4, D],
    "win_w_qkv": [2, D, 1536], "win_b_qkv": [2, 1536], "win_sink": [2, 16], "win_w_o": [2, D, D], "win_b_o": [2, D],
    "glb_w_qkv": [1, D, 1536], "glb_g_q": [1, 64], "glb_g_k": [1, 64], "glb_w_o": [1, D, D],
    "gdn_w_in": [1, D, 4128], "gdn_conv_w": [1, 5, 3072], "gdn_a_log": [1, 2, 8], "gdn_dt_bias": [1, 2, 8],
    "gdn_g_out": [1, 128], "gdn_w_o": [1, D, D],
    "moe_w_router": [4, D, NE], "moe_b_router": [4, NE], "moe_w_up": [4, NE, D, 2 * D], "moe_b_up": [4, NE, 2 * D],
    "moe_w_down": [4, NE, D, D], "moe_b_down": [4, NE, D], "g_final": [D],
}


def host_consts():
    c = {}
    c["ident"] = np.eye(128, dtype=np.float32)
    a = np.arange(128)
    lo = (a[None, :] <= a[:, None]).astype(np.float32)
    hi = (a[:, None] <= a[None, :]).astype(np.float32)
    c["mask_lo"] = np.tile(lo, (1, 4))
    c["mask_hi"] = np.tile(hi, (1, 4))
    t = np.arange(NLAT)
    pos = np.stack([t // 64, t % 64], 0).astype(np.float32)
    inv = (10000.0 ** (-np.arange(0, 32, 2, dtype=np.float32) / 32)).astype(np.float32)
    cosT = np.zeros((64, NLAT), np.float32)
    sinT = np.zeros((64, NLAT), np.float32)
    PT = np.zeros((64, 64), np.float32)
    for d in range(64):
        ax, r = d // 32, d % 32
        half, f = r // 16, r % 16
        ang = (pos[ax] * inv[f]).astype(np.float32)
        cosT[d] = np.cos(ang)
        sinT[d] = np.sin(ang)
        if half == 0:
            PT[d + 16, d] = -1.0
        else:
            PT[d - 16, d] = 1.0
    c["cosT"] = cosT
    c["sinT"] = sinT
    c["PT"] = PT
    idx = np.arange(128)
    ch = idx // 64
    same = ch[:, None] == ch[None, :]
    le = idx[:, None] <= idx[None, :]
    ge = idx[:, None] >= idx[None, :]
    lt = idx[:, None] < idx[None, :]
    gt = idx[:, None] > idx[None, :]
    f32 = np.float32
    c["gd_L0"] = (same & le).astype(f32)
    c["gd_L1"] = (same & ge).astype(f32)
    for d, last in ((0, (63, 127)), (1, (0, 64))):
        lastv = np.array([last[ci] for ci in ch])
        c[f"gd_SelC{d}"] = (idx[:, None] == lastv[None, :]).astype(f32)
        c[f"gd_SelA{d}"] = np.repeat((idx == last[0]).astype(f32)[:, None], 128, 1)
        c[f"gd_SelB{d}"] = np.repeat((idx == last[1]).astype(f32)[:, None], 128, 1)
    c["gd_Ms0"] = (same & gt).astype(f32)
    c["gd_Ms1"] = (same & lt).astype(f32)
    c["gd_MiT0"] = (same & le).astype(f32)
    c["gd_MiT1"] = (same & ge).astype(f32)
    c["gd_rowm"] = np.stack([(idx < 64), (idx >= 64)], 1).astype(f32)
    for d in (0, 1):
        c[f"gd_Mb{d}"] = ((1.0 - c[f"gd_Ms{d}"]) * 1e4).astype(f32)
        c[f"gd_Mn{d}"] = ((c[f"gd_MiT{d}"] - 1.0) * 1e4).astype(f32)
    return c


class K:
    pass


class LazyInputs(dict):
    def __init__(self, nc):
        super().__init__()
        self.nc = nc

    def __missing__(self, n):
        ap = self.nc.dram_tensor(n, INPUT_SHAPES[n], F32, kind="ExternalInput").ap()
        self[n] = ap
        return ap


def load_T(p, k, dst_ap, dst_tile, src_rows_ap, R, src_dep=None, C=128):
    tmp = k.ltmp.next()
    p.dma("sp", tmp[0:R, 0:C], src_rows_ap, reads=[src_dep] if src_dep else (), writes=[tmp])
    ps = p.next_ps()
    p.tr(ps[0:C, 0:R], tmp[0:R, 0:C], k.ident[0:R, 0:R], [tmp, k.ident], [ps])
    p.cp(dst_ap, ps[0:C, 0:R], [ps], [dst_tile])


def bcast_rows(p, k, ps_ap, ps_tile, row_ap, row_tile, start, stop):
    p.mm(ps_ap, k.ones_row[0:1, :], row_ap, start, stop, [k.ones_row, row_tile], [ps_tile])


def stage_init(p, k):
    p.dma("sp", k.X[0:NCTX, :], k.inp["ctx"], reads=(), writes=[k.Xd])
    p.dma("sp", k.X[NCTX:NTOK, :], k.inp["x"], reads=(), writes=[k.Xd])


def stage_mod(p, k, layers):
    p.mark()
    inp = k.inp
    craw = p.sb([128, 16], F32, "craw")
    load_T(p, k, craw[:, 0:8], craw, inp["c"].rearrange("(k q) -> k q", q=128), 8)
    load_T(p, k, craw[:, 8:16], craw, inp["c_ctx"].rearrange("(k q) -> k q", q=128), 8)
    csil = p.sb([128, 16], F32, "csil")
    p.act(csil[:], craw[:], AF.Silu, [craw], [csil])
    ones_bf = p.sb([128, 128], F32, "ones128")
    p.memset(ones_bf[:], 1.0, [ones_bf])
    lhs = p.sb([128, 16, 128], BF16, "modlhs")
    for j in range(16):
        p.ts(lhs[:, j, :], ones_bf[:], csil[:, j:j + 1], None, ALU.mult, None, [ones_bf, csil], [lhs])
    wring = p.ring(4, [128, 8, 512], BF16, "wmod")
    brow = p.ring(4, [1, 512], F32, "bmodrow")
    grow = p.ring(2, [1, 1024], F32, "grow")
    oring = p.ring(3, [128, 512], F32, "modo")
    gb = [p.sb([128, 1024], F32, "gb0"), p.sb([128, 1024], F32, "gb1")]
    for l in layers:
        for gi, gname in enumerate(("g_mix", "g_ffn")):
            gr = grow.next()
            p.dma("sp", gr[0:1, :], inp[gname][l:l + 1, :], writes=[gr])
            for nh in range(2):
                ps = p.next_ps()
                bcast_rows(p, k, ps[:, :], ps, gr[0:1, nh * 512:(nh + 1) * 512], gr, True, True)
                p.cp(gb[gi][:, nh * 512:(nh + 1) * 512], ps[:, :], [ps], [gb[gi]], eng="act")
        for j in range(6):
            for nh in range(2):
                n0 = j * 1024 + nh * 512
                wt = wring.next()
                p.dma("pool", wt[:, :, :], inp["w_mod"][l].rearrange("(k q) n -> q k n", q=128)[:, :, n0:n0 + 512], writes=[wt])
                br = brow.next()
                p.dma("sp", br[0:1, :], inp["b_mod"][l:l + 1, n0:n0 + 512], writes=[br])
                for s in range(2):
                    ps = p.next_ps()
                    for kk in range(8):
                        p.mm(ps[:, :], lhs[:, s * 8 + kk, :], wt[:, kk, :], kk == 0, False, [lhs, wt], [ps])
                    bcast_rows(p, k, ps[:, :], ps, br[0:1, :], br, False, True)
                    o = oring.next()
                    if j in (1, 4):
                        g = gb[0] if j == 1 else gb[1]
                        p.stt(o[:], ps[:, :], 1.0, g[:, nh * 512:(nh + 1) * 512], ALU.add, ALU.mult, [ps, g], [o])
                    else:
                        p.cp(o[:], ps[:, :], [ps], [o], eng="act")
                    p.dma("sp", k.MODS[l, s, j, :, nh * 512:(nh + 1) * 512], o[:], reads=[o], writes=[k.MODSd])
    p.release()


def rstd_of(p, k, ss, n, eps, R, W):
    p.ts(ss, ss, 1.0 / n, eps, ALU.mult, ALU.add, R, W)
    p.recip(ss, ss, R, W)
    p.act(ss, ss, AF.Sqrt, R, W)


def stage_norm(p, k, l, which, tiles, hT, router=None, p32=None):
    p.mark()
    jsh, jA = (0, 1) if which == 0 else (3, 4)
    modt = {}
    for s in (0, 1):
        a = p.sb([128, 1024], F32, "modA")
        b = p.sb([128, 1024], F32, "modS")
        p.dma("sp", a[:], k.MODS[l, s, jA], reads=[k.MODSd], writes=[a])
        p.dma("sp", b[:], k.MODS[l, s, jsh], reads=[k.MODSd], writes=[b])
        modt[s] = (a, b)
    xr = p.ring(3, [128, 1024], F32, "xn")
    hr = p.ring(3, [128, 1024], F32, "hn")
    sqr = p.ring(2, [128, 1024], F32, "sq")
    ssr = p.ring(3, [128, 1], F32, "ss")
    need32 = router is not None or p32 is not None
    if need32:
        h32r = p.ring(3, [128, 8, 128], F32, "h32")
    if router is not None:
        wr = p.sb([128, 8, NE], F32, "wr")
        for kk in range(8):
            p.dma("sp", wr[:, kk, :], k.inp["moe_w_router"][l, kk * 128:(kk + 1) * 128, :], writes=[wr])
        brr = p.sb([1, NE], F32, "brr")
        p.dma("sp", brr[0:1, :], k.inp["moe_b_router"][l:l + 1, :], writes=[brr])
        lgr = p.ring(2, [128, NE], F32, "lg")
        m8r = p.ring(2, [128, 8], F32, "m8")
        er = p.ring(2, [128, NE], F32, "eg")
        smr = p.ring(4, [128, 1], F32, "sm")

    def s1(tt):
        s = 1 if tt < 2 else 0
        A, S = modt[s]
        x = xr.next()
        p.dma("sp", x[:], k.X[tt * 128:(tt + 1) * 128, :], reads=[k.Xd], writes=[x])
        ss = ssr.next()
        sq = sqr.next()
        p.act(sq[:], x[:], AF.Square, [x], [sq, ss], accum_out=ss[:])
        rstd_of(p, k, ss[:], D, 1e-6, [ss], [ss])
        h = hr.next()
        p.stt(h[:], x[:], ss[:, 0:1], A[:], ALU.mult, ALU.mult, [x, ss, A], [h])
        p.tt(h[:], h[:], S[:], ALU.add, [h, S], [h])
        return h

    def s2(tt, h):
        h32 = h32r.next() if need32 else None
        for half in range(2):
            ps = p.next_ps()
            for q in range(4):
                kk = half * 4 + q
                p.tr(ps[:, q * 128:(q + 1) * 128], h[:, kk * 128:(kk + 1) * 128], k.ident[:], [h, k.ident], [ps])
            for q in range(4):
                if need32:
                    p.cp(h32[:, half * 4 + q, :], ps[:, q * 128:(q + 1) * 128], [ps], [h32], eng="act")
                    p.cp(hT[:, half * 4 + q, tt * 128:(tt + 1) * 128], h32[:, half * 4 + q, :], [h32], [hT])
                else:
                    p.cp(hT[:, half * 4 + q, tt * 128:(tt + 1) * 128], ps[:, q * 128:(q + 1) * 128], [ps], [hT], eng="act")
        return h32

    def s3(tt, h32):
        if p32 is not None:
            ps = p.next_ps()
            n = p32["n"]
            for kk in range(8):
                p.mm(ps[:, 0:n], h32[:, kk, :], p32["w"][:, kk, :], kk == 0, kk == 7, [h32, p32["w"]], [ps])
            p32["cb"](tt, ps)
        if router is not None:
            G = router["G"]
            ps = p.next_ps()
            for kk in range(8):
                p.mm(ps[:, 0:NE], h32[:, kk, :], wr[:, kk, :], kk == 0, False, [h32, wr], [ps])
            bcast_rows(p, k, ps[:, 0:NE], ps, brr[0:1, :], brr, False, True)
            lg = lgr.next()
            p.cp(lg[:], ps[:, 0:NE], [ps], [lg])
            m8 = m8r.next()
            p.op("dve", lambda e, m8=m8, lg=lg: e.max(out=m8[:], in_=lg[:]), [lg], [m8])
            nm = smr.next()
            p.ts(nm[:], m8[:, 0:1], -1.0, None, ALU.mult, None, [m8], [nm])
            eg = er.next()
            p.act(eg[:], lg[:], AF.Exp, [lg, nm], [eg], bias=nm[:, 0:1], scale=1.0)
            sm = smr.next()
            p.stt(eg[:], lg[:], m8[:, 3:4], eg[:], ALU.is_ge, ALU.mult, [lg, m8, eg], [eg])
            p.op("dve", lambda e, sm=sm, eg=eg: e.reduce_sum(out=sm[:], in_=eg[:], axis=AX.X), [eg], [sm])
            p.recip(sm[:], sm[:], [sm], [sm])
            p.ts(G[:, tt, :], eg[:], sm[:, 0:1], None, ALU.mult, None, [eg, sm], [G])

    q1, q2 = [], []
    for tt in tiles:
        q1.append((tt, s1(tt)))
        if len(q1) > 1:
            t_, h_ = q1.pop(0)
            q2.append((t_, s2(t_, h_)))
        if need32 and len(q2) > 1:
            s3(*q2.pop(0))
    while q1:
        t_, h_ = q1.pop(0)
        q2.append((t_, s2(t_, h_)))
    if need32:
        while q2:
            s3(*q2.pop(0))
    p.release()


def stage_moe(p, k, l, tiles, hT, G):
    p.mark()
    inp = k.inp
    t0 = tiles[0]
    ntile = len(tiles)
    acc = p.sb([128, ntile, 1024], F32, "acc")
    bupT = p.sb([128, NE * 16], F32, "bupT")
    bsrc = inp["moe_b_up"][l].rearrange("e (m q) -> (e m) q", q=128)
    for i in range(4):
        load_T(p, k, bupT[:, i * 128:(i + 1) * 128], bupT, bsrc[i * 128:(i + 1) * 128, :], 128)
    p.mark()
    bd = p.sb([NE, 1024], F32, "bd")
    p.dma("sp", bd[:], inp["moe_b_down"][l], writes=[bd])
    gtr = p.ring(2, [NE, 128], F32, "GT")
    for ti, tt in enumerate(tiles):
        ps = p.next_ps()
        p.tr(ps[0:NE, 0:128], G[:, tt, :], k.ident[:], [G, k.ident], [ps])
        gt = gtr.next()
        p.cp(gt[:], ps[0:NE, 0:128], [ps], [gt])
        for nh in range(2):
            ps2 = p.next_ps()
            p.mm(ps2[:, :], gt[:, :], bd[:, nh * 512:(nh + 1) * 512], True, True, [gt, bd], [ps2])
            p.cp(acc[:, ti, nh * 512:(nh + 1) * 512], ps2[:, :], [ps2], [acc], eng="act")
    p.release()
    p.mark()
    blocks = []
    i = 0
    while i < ntile:
        n = min(4, ntile - i)
        blocks.append((i, n))
        i += n
    wur = p.ring(2, [128, 8, 2, 512], BF16, "wu")
    wdr = p.ring(2, [128, 4, 1024], BF16, "wd")
    aTr = [p.ring(3, [128, 512], BF16, f"aT{m}") for m in range(4)]
    t1r = p.ring(3, [128, 512], F32, "t1")
    sgr = p.ring(2, [128, 512], F32, "sg")
    t2r = p.ring(3, [128, 512], F32, "t2")
    accd = [[Dep(f"acc{ti}_{nh}") for nh in range(2)] for ti in range(ntile)]
    for ti in range(ntile):
        for nh in range(2):
            accd[ti][nh].w = acc.dep.w
    pending = []

    def make_down(aT, wd, e, b0, bn):
        def emit():
            for j in range(bn):
                ti = b0 + j
                for nh in range(2):
                    po = p.next_ps()
                    for kk in range(4):
                        p.mm(po[:, :], aT[kk][:, j * 128:(j + 1) * 128], wd[:, kk, nh * 512:(nh + 1) * 512], kk == 0, kk == 3, [aT[kk], wd], [po])
                    av = acc[:, ti, nh * 512:(nh + 1) * 512]
                    p.stt(av, po[:, :], G[:, t0 + ti, e:e + 1], av, ALU.mult, ALU.add, [po, G, accd[ti][nh]], [accd[ti][nh]])
        return emit

    for e in range(NE):
        for half in range(2):
            wu = wur.next()
            wd = wdr.next()
            src = inp["moe_w_up"][l, e].rearrange("r (g h c) -> r g h c", g=2, h=2)
            for kk in range(8):
                p.dma("pool", wu[:, kk, :, :], src[kk * 128:(kk + 1) * 128, :, half, :], writes=[wu])
            for kk in range(4):
                r0 = half * 512 + kk * 128
                p.dma("pool", wd[:, kk, :], inp["moe_w_down"][l, e, r0:r0 + 128, :], writes=[wd])
            for (b0, bn) in blocks:
                tok0 = (t0 + b0) * 128
                ntok = bn * 128
                aT = []
                for m in range(4):
                    pa = p.next_ps()
                    for kk in range(8):
                        p.mm(pa[:, 0:ntok], wu[:, kk, 0, m * 128:(m + 1) * 128], hT[:, kk, tok0:tok0 + ntok], kk == 0, kk == 7, [wu, hT], [pa])
                    pb = p.next_ps()
                    for kk in range(8):
                        p.mm(pb[:, 0:ntok], wu[:, kk, 1, m * 128:(m + 1) * 128], hT[:, kk, tok0:tok0 + ntok], kk == 0, kk == 7, [wu, hT], [pb])
                    ca = e * 16 + half * 4 + m
                    cb = ca + 8
                    t1 = t1r.next()
                    p.ts(t1[:, 0:ntok], pa[:, 0:ntok], bupT[:, ca:ca + 1], 7.0, ALU.add, ALU.min, [pa, bupT], [t1])
                    sg = sgr.next()
                    p.act(sg[:, 0:ntok], t1[:, 0:ntok], AF.Sigmoid, [t1], [sg], scale=1.702)
                    t2 = t2r.next()
                    p.act(t2[:, 0:ntok], pb[:, 0:ntok], AF.Identity, [pb, bupT], [t2], bias=bupT[:, cb:cb + 1], scale=1.0)
                    p.ts(t2[:, 0:ntok], t2[:, 0:ntok], -7.0, 7.0, ALU.max, ALU.min, [t2], [t2])
                    p.tt(t1[:, 0:ntok], t1[:, 0:ntok], sg[:, 0:ntok], ALU.mult, [t1, sg], [t1])
                    a = aTr[m].next()
                    p.stt(a[:, 0:ntok], t2[:, 0:ntok], 1.0, t1[:, 0:ntok], ALU.add, ALU.mult, [t2, t1], [a])
                    aT.append(a)
                if pending:
                    pending.pop(0)()
                pending.append(make_down(aT, wd, e, b0, bn))
    while pending:
        pending.pop(0)()
    p.release()
    gts = {}
    for s in (0, 1):
        g = p.sb([128, 1024], F32, "gt2")
        p.dma("sp", g[:], k.MODS[l, s, 5], reads=[k.MODSd], writes=[g])
        gts[s] = g
    xr = p.ring(2, [128, 1024], F32, "xres")
    for ti, tt in enumerate(tiles):
        s = 1 if tt < 2 else 0
        x = xr.next()
        p.dma("sp", x[:], k.X[tt * 128:(tt + 1) * 128, :], reads=[k.Xd], writes=[x])
        p.tt(acc[:, ti, :], acc[:, ti, :], gts[s][:], ALU.mult, [accd[ti][0], accd[ti][1], gts[s]], [accd[ti][0], accd[ti][1]])
        p.tt(x[:], x[:], acc[:, ti, :], ALU.add, [x, accd[ti][0], accd[ti][1]], [x])
        p.dma("sp", k.X[tt * 128:(tt + 1) * 128, :], x[:], reads=[x], writes=[k.Xd])
    p.release()


def stage_attn(p, k, l, kind, j, need_ctx):
    inp = k.inp
    wname = "win" if kind == 0 else "glb"
    wqkv = inp[wname + "_w_qkv"][j]
    p.mark()
    qT = p.sb([128, 16, NTOK], BF16, "qT")
    qTd = [[Dep(f"qT{t}_{g}") for g in range(4)] for t in range(NT)]
    kT = p.sb([128, 4, NTOK], BF16, "kT")
    V = p.sb([128, NT, 4, 128], BF16, "V")
    p.memset(qT[64:128, :, :], 0.0, [qT], eng="pool")
    p.memset(kT[64:128, :, :], 0.0, [kT], eng="pool")
    p.memset(V[:, :, :, :], 1.0, [V], eng="pool")
    ones_bf = p.sb([128, 64], BF16, "ones_bf")
    p.memset(ones_bf[:], 1.0, [ones_bf])
    p.mark()
    p.ps = Ring(k.pstiles)
    hT = p.sb([128, 8, NTOK], BF16, "hT")
    stage_norm(p, k, l, 0, list(range(NT)), hT)
    cosT = p.sb([64, NLAT], F32, "cosT")
    sinT = p.sb([64, NLAT], F32, "sinT")
    PT = p.sb([64, 64], F32, "PT")
    p.dma("sp", cosT[:], k.cin["cosT"], writes=[cosT])
    p.dma("sp", sinT[:], k.cin["sinT"], writes=[sinT])
    p.dma("sp", PT[:], k.cin["PT"], writes=[PT])
    wq = p.sb([128, 8, 1088], BF16, "wq")
    wk = p.sb([128, 8, 512], BF16, "wkv")
    for kk in range(8):
        p.dma("pool", wq[:, kk, :], wqkv[kk * 128:(kk + 1) * 128, 0:1088], writes=[wq])
        p.dma("pool", wk[:, kk, :], wqkv[kk * 128:(kk + 1) * 128, 1024:1536], writes=[wk])
    if kind == 0:
        bT = p.sb([64, 24], F32, "bT")
        load_T(p, k, bT[:, :], bT, inp["win_b_qkv"][j].rearrange("(h d) -> h d", d=64), 24, C=64)
        bvrow = p.sb([1, 256], F32, "bvrow")
        p.dma("sp", bvrow[0:1, :], inp["win_b_qkv"][j:j + 1, 1280:1536], writes=[bvrow])
    else:
        gqk = p.sb([64, 2], F32, "gqk")
        load_T(p, k, gqk[:, 0:1], gqk, inp["glb_g_q"][j:j + 1, :], 1, C=64)
        load_T(p, k, gqk[:, 1:2], gqk, inp["glb_g_k"][j:j + 1, :], 1, C=64)
        avg64 = p.sb([64, 64], F32, "avg64")
        p.memset(avg64[:], 1.0 / 64, [avg64])
    q32r = p.ring(5, [64, 512], F32, "q32")
    sqr = p.ring(5 if kind == 1 else 4, [64, 512], F32, "qsq")
    blocks = [(0, 256), (256, 512), (768, 512), (1280, 512), (1792, 512)]
    for hh in range(20):
        isq = hh < 16
        items = []
        for (t0, n) in blocks:
            ps = p.next_ps()
            for kk in range(8):
                lw = wq[:, kk, hh * 64:hh * 64 + 128] if isq else wk[:, kk, (hh - 16) * 64:(hh - 16) * 64 + 128]
                p.mm(ps[:, 0:n], lw, hT[:, kk, t0:t0 + n], kk == 0, kk == 7, [wq if isq else wk, hT], [ps])
            q32 = q32r.next()
            if kind == 0:
                p.act(q32[:, 0:n], ps[0:64, 0:n], AF.Identity, [ps, bT], [q32], bias=bT[:, hh:hh + 1], scale=1.0)
            else:
                p.cp(q32[:, 0:n], ps[0:64, 0:n], [ps], [q32], eng="act")
            items.append((t0, n, q32, sqr.next()))
        if kind == 1:
            gcol = gqk[:, 0:1] if isq else gqk[:, 1:2]
            for (t0, n, q32, sq) in items:
                p.act(sq[:, 0:n], q32[:, 0:n], AF.Square, [q32], [sq])
            psns = []
            for (t0, n, q32, sq) in items:
                psn = p.next_ps()
                p.mm(psn[0:64, 0:n], avg64[:, :], sq[:, 0:n], True, True, [avg64, sq], [psn])
                psns.append(psn)
            for (t0, n, q32, sq), psn in zip(items, psns):
                p.ts(sq[:, 0:n], psn[0:64, 0:n], 1e-6, None, ALU.add, None, [psn], [sq])
                p.recip(sq[:, 0:n], sq[:, 0:n], [sq], [sq])
            for (t0, n, q32, sq) in items:
                p.act(sq[:, 0:n], sq[:, 0:n], AF.Sqrt, [sq], [sq])
            for (t0, n, q32, sq) in items:
                p.stt(q32[:, 0:n], q32[:, 0:n], gcol, sq[:, 0:n], ALU.mult, ALU.mult, [q32, gqk, sq], [q32])
        ps2s = {}
        for (t0, n, q32, sq) in items[1:]:
            ps2 = p.next_ps()
            p.mm(ps2[0:64, 0:n], PT[:, :], q32[:, 0:n], True, True, [PT, q32], [ps2])
            ps2s[t0] = ps2
        for (t0, n, q32, sq) in items[1:]:
            l0 = t0 - NCTX
            p.tt(sq[:, 0:n], ps2s[t0][0:64, 0:n], sinT[:, l0:l0 + n], ALU.mult, [ps2s[t0], sinT], [sq])
        for (t0, n, q32, sq) in items[1:]:
            l0 = t0 - NCTX
            p.tt(q32[:, 0:n], q32[:, 0:n], cosT[:, l0:l0 + n], ALU.mult, [q32, cosT], [q32], eng="pool")
        for (t0, n, q32, sq) in items:
            dst = qT[0:64, hh, t0:t0 + n] if isq else kT[0:64, hh - 16, t0:t0 + n]
            dW = [qTd[t][hh // 4] for t in range(t0 // 128, (t0 + n) // 128)] if isq else [kT]
            if t0 == 0:
                p.cp(dst, q32[:, 0:n], [q32], dW)
            else:
                p.tt(dst, q32[:, 0:n], sq[:, 0:n], ALU.add, [q32, sq], dW)
    for tt in range(NT):
        ps = p.next_ps()
        for kk in range(8):
            p.mm(ps[:, 0:256], hT[:, kk, tt * 128:(tt + 1) * 128], wk[:, kk, 256:512], kk == 0, (kk == 7 and kind == 1), [hT, wk], [ps])
        if kind == 0:
            bcast_rows(p, k, ps[:, 0:256], ps, bvrow[0:1, :], bvrow, False, True)
        p.cp(V[:, tt, :, 0:64], ps[:, 0:256].rearrange("p (g d) -> p g d", g=4), [ps], [V])
    p.release()
    p.mark()
    p.ps = Ring(k.pstiles[0:4])
    if kind == 0:
        mlo = p.sb([128, 512], BF16, "mlo")
        mhi = p.sb([128, 512], BF16, "mhi")
        p.dma("pool", mlo[:], k.cin["mask_lo"], writes=[mlo])
        p.dma("pool", mhi[:], k.cin["mask_hi"], writes=[mhi])
        srow = p.sb([1, 16], F32, "srow")
        p.dma("sp", srow[0:1, :], inp["win_sink"][j:j + 1, :], writes=[srow])
        p.act(srow[0:1, :], srow[0:1, :], AF.Exp, [srow], [srow])
        sinkexp = p.sb([128, 16], F32, "sinkexp")
        ps = p.next_ps()
        p.mm(ps[:, 0:16], k.ones_row[0:1, :], srow[0:1, :], True, True, [k.ones_row, srow], [ps])
        p.cp(sinkexp[:], ps[:, 0:16], [ps], [sinkexp])
    wo = p.sb([128, 16, 1024], BF16, "wo")
    p.memset(wo[64:128, :, :], 0.0, [wo])
    wsrc = inp[wname + "_w_o"][j].rearrange("(h d) n -> d h n", d=64)
    for h in range(16):
        p.dma("pool", wo[0:64, h, :], wsrc[:, h, :], writes=[wo])
    ptr = p.ring(5, [128, 512], BF16, "Pt")
    rdr = p.ring(2, [128, 512], F32, "rd")
    qtiles = list(range(2, NT)) + ([0, 1] if need_ctx else [])
    its = []
    acc_i = 0
    for qt in qtiles:
        if qt < 2:
            keys = [0, 1]
        elif kind == 1:
            keys = list(range(NT))
        else:
            keys = [0, 1] + [kt for kt in (qt - 1, qt, qt + 1) if 2 <= kt < NT]
        for g in range(4):
            acc = k.psx[acc_i % 4]
            acc_i += 1
            for ki, kt in enumerate(keys):
                its.append((qt, g, ki, kt, len(keys), acc))

    def stage1(it):
        qt, g, ki, kt, nk, acc = it
        rhs = qT[:, 4 * g:4 * g + 4, qt * 128:(qt + 1) * 128]
        psS = p.next_ps()
        p.mm(psS[:, :].rearrange("p (a b) -> p a b", a=4), kT[:, g, kt * 128:(kt + 1) * 128], rhs, True, True, [kT, qTd[qt][g]], [psS])
        pt = ptr.next()
        p.act(pt[:], psS[:, :], AF.Exp, [psS], [pt], scale=0.125)
        if kind == 0 and qt >= 2 and kt >= 2 and kt == qt - 1:
            p.tt(pt[:], pt[:], mlo[:], ALU.mult, [pt, mlo], [pt])
        elif kind == 0 and qt >= 2 and kt >= 2 and kt == qt + 1:
            p.tt(pt[:], pt[:], mhi[:], ALU.mult, [pt, mhi], [pt])
        return pt

    def stage2(it, pt):
        qt, g, ki, kt, nk, psO = it
        first, last = ki == 0, ki == nk - 1
        p.mm(psO[:, :], V[:, kt, g, :], pt[:], first, last, [V, pt], [psO])
        if not last:
            return
        rd = rdr.next()
        if kind == 0:
            for a_ in range(4):
                h = 4 * g + a_
                p.ts(rd[64:128, a_ * 128:(a_ + 1) * 128], psO[64:128, a_ * 128:(a_ + 1) * 128], sinkexp[64:128, h:h + 1], None, ALU.add, None, [psO, sinkexp], [rd])
            p.recip(rd[64:128, :], rd[64:128, :], [rd], [rd])
        else:
            p.recip(rd[64:128, :], psO[64:128, :], [psO], [rd])
        dst = qT[0:64, 4 * g:4 * g + 4, qt * 128:(qt + 1) * 128]
        p.tt(dst, psO[0:64, :].rearrange("p (a b) -> p a b", a=4), rd[64:128, :].rearrange("p (a b) -> p a b", a=4), ALU.mult, [psO, rd], [qTd[qt][g]])

    queue = []
    for it in its:
        queue.append((it, stage1(it)))
        if len(queue) > 2:
            stage2(*queue.pop(0))
    while queue:
        stage2(*queue.pop(0))
    gts = {}
    for s_ in ((0, 1) if need_ctx else (0,)):
        g_ = p.sb([128, 1024], F32, "gt1")
        p.dma("sp", g_[:], k.MODS[l, s_, 2], reads=[k.MODSd], writes=[g_])
        gts[s_] = g_
    if kind == 0:
        borow = p.sb([1, 1024], F32, "borow")
        p.dma("sp", borow[0:1, :], inp["win_b_o"][j:j + 1, :], writes=[borow])
    xr = p.ring(2, [128, 1024], F32, "xo")
    yr = p.ring(2, [128, 512], F32, "yo")
    for qt in qtiles:
        s_ = 1 if qt < 2 else 0
        x = xr.next()
        p.dma("sp", x[:], k.X[qt * 128:(qt + 1) * 128, :], reads=[k.Xd], writes=[x])
        for nh in range(2):
            ps = p.next_ps()
            for h in range(16):
                p.mm(ps[:, :], qT[:, h, qt * 128:(qt + 1) * 128], wo[:, h, nh * 512:(nh + 1) * 512], h == 0, (h == 15 and kind == 1), [qTd[qt][h // 4], wo], [ps])
            if kind == 0:
                bcast_rows(p, k, ps[:, :], ps, borow[0:1, nh * 512:(nh + 1) * 512], borow, False, True)
            y = yr.next()
            p.tt(y[:], ps[:, :], gts[s_][:, nh * 512:(nh + 1) * 512], ALU.mult, [ps, gts[s_]], [y])
            p.tt(x[:, nh * 512:(nh + 1) * 512], x[:, nh * 512:(nh + 1) * 512], y[:], ALU.add, [x, y], [x])
        p.dma("sp", k.X[qt * 128:(qt + 1) * 128, :], x[:], reads=[x], writes=[k.Xd])
    p.release()
    p.release()


def stage_gdn(p, k, l, j, need_ctx):
    inp = k.inp
    nc = k.nc
    w_in = inp["gdn_w_in"][j]
    if not hasattr(k, "gd"):
        k.gd = {n: (nc.dram_tensor("gd_" + n, shp, F32).ap(), Dep("gd_" + n)) for n, shp in (
            ("QT", [8, 128, NTOK]), ("KT", [8, 128, NTOK]), ("Kt", [NTOK, 8, 128]), ("Vt", [NTOK, 8, 128]),
            ("Z", [NTOK, 1024]), ("O0", [NTOK, 1024]), ("O1", [NTOK, 1024]))}
    gd = k.gd
    p.mark()
    BG = p.sb([128, NT, 32], F32, "BG")
    p.mark()
    hT = p.sb([128, 8, NTOK], BF16, "hT")
    wbg = p.sb([128, 8, 32], F32, "wbg")
    for kk in range(8):
        p.dma("sp", wbg[:, kk, :], w_in[kk * 128:(kk + 1) * 128, 4096:4128], writes=[wbg])
    r2 = p.sb([1, 32], F32, "r2")
    p.dma("sp", r2[0:1, 0:16], inp["gdn_dt_bias"][j].rearrange("a b -> (a b)").rearrange("(o n) -> o n", o=1), writes=[r2])
    p.dma("sp", r2[0:1, 16:32], inp["gdn_a_log"][j].rearrange("a b -> (a b)").rearrange("(o n) -> o n", o=1), writes=[r2])
    p.act(r2[0:1, 16:32], r2[0:1, 16:32], AF.Exp, [r2], [r2])
    p.ts(r2[0:1, 16:32], r2[0:1, 16:32], -1.0, None, ALU.mult, None, [r2], [r2])
    dtb = p.sb([128, 32], F32, "dtb")
    ps = p.next_ps()
    bcast_rows(p, k, ps[:, 0:32], ps, r2[0:1, :], r2, True, True)
    p.cp(dtb[:], ps[:, 0:32], [ps], [dtb])
    tmpr = p.ring(2, [128, 32], F32, "bgtmp")

    def cb(tt, ps):
        t = tmpr.next()
        p.cp(t[:], ps[:, 0:32], [ps], [t], eng="act")
        p.act(BG[:, tt, 0:16], t[:, 0:16], AF.Sigmoid, [t], [BG])
        p.tt(t[:, 16:32], t[:, 16:32], dtb[:, 0:16], ALU.add, [t, dtb], [t])
        p.act(t[:, 16:32], t[:, 16:32], AF.Exp, [t], [t])
        p.act(t[:, 16:32], t[:, 16:32], AF.Ln, [t], [t], bias=1.0, scale=1.0)
        p.tt(BG[:, tt, 16:32], t[:, 16:32], dtb[:, 16:32], ALU.mult, [t, dtb], [BG])

    stage_norm(p, k, l, 0, list(range(NT)), hT, p32={"w": wbg, "n": 32, "cb": cb})
    cw = p.sb([128, 24 * 5], F32, "cw")
    for c in range(24):
        load_T(p, k, cw[:, c * 5:(c + 1) * 5], cw, inp["gdn_conv_w"][j][:, c * 128:(c + 1) * 128], 5)
    ones128 = p.sb([128, 128], F32, "ones128")
    p.memset(ones128[:], 1.0, [ones128])
    pcr = p.ring(2, [128, 260], F32, "pc")
    plr = p.ring(2, [128, 2052], F32, "pl")
    for t_ in pcr.tiles + plr.tiles:
        p.memset(t_[:], 0.0, [t_])
    ycr = p.ring(2, [128, 256], F32, "yc")
    ylr = p.ring(2, [128, 2048], F32, "yl")
    wcr = p.ring(2, [128, 8, 128], BF16, "wc")
    sqr = p.ring(6, [128, 512], F32, "gsq")
    tkr = p.ring(3, [128, 128], F32, "tok")
    segs = ((0, 256), (256, 2048))

    def proj(c):
        wc = wcr.next()
        for kk in range(8):
            p.dma("pool", wc[:, kk, :], w_in[kk * 128:(kk + 1) * 128, c * 128:(c + 1) * 128], writes=[wc])
        bufs = (pcr.next(), plr.next())
        for (t0, n), buf in zip(segs, bufs):
            for blk in range(0, n, 512):
                nb = min(512, n - blk)
                ps = p.next_ps()
                for kk in range(8):
                    p.mm(ps[:, 0:nb], wc[:, kk, :], hT[:, kk, t0 + blk:t0 + blk + nb], kk == 0, kk == 7, [wc, hT], [ps])
                p.cp(buf[:, 2 + blk:2 + blk + nb], ps[:, 0:nb], [ps], [buf], eng="act")
        return bufs

    def post(c, bufs):
        for (t0, n), buf, y in zip(segs, bufs, (ycr.next(), ylr.next())):
            p.ts(y[:, 0:n], buf[:, 0:n], cw[:, c * 5:c * 5 + 1], None, ALU.mult, None, [buf, cw], [y])
            for jj in range(1, 5):
                p.stt(y[:, 0:n], buf[:, jj:jj + n], cw[:, c * 5 + jj:c * 5 + jj + 1], y[:, 0:n], ALU.mult, ALU.add, [buf, cw, y], [y])
            p.act(y[:, 0:n], y[:, 0:n], AF.Silu, [y], [y])
            if c < 16:
                scale = (128.0 ** -0.5) if c < 8 else 1.0
                bl = [(blk, min(512, n - blk)) for blk in range(0, n, 512)]
                sqs = [sqr.next() for _ in bl]
                for (blk, nb), sq in zip(bl, sqs):
                    p.act(sq[:, 0:nb], y[:, blk:blk + nb], AF.Square, [y], [sq])
                pss = []
                for (blk, nb), sq in zip(bl, sqs):
                    ps = p.next_ps()
                    p.mm(ps[:, 0:nb], ones128[:, :], sq[:, 0:nb], True, True, [ones128, sq], [ps])
                    pss.append(ps)
                for (blk, nb), sq, ps in zip(bl, sqs, pss):
                    p.ts(sq[:, 0:nb], ps[:, 0:nb], 1e-6, None, ALU.add, None, [ps], [sq])
                    p.recip(sq[:, 0:nb], sq[:, 0:nb], [sq], [sq])
                for (blk, nb), sq in zip(bl, sqs):
                    p.act(sq[:, 0:nb], sq[:, 0:nb], AF.Sqrt, [sq], [sq])
                for (blk, nb), sq in zip(bl, sqs):
                    p.stt(y[:, blk:blk + nb], y[:, blk:blk + nb], scale, sq[:, 0:nb], ALU.mult, ALU.mult, [y, sq], [y])
                dst = gd["QT"] if c < 8 else gd["KT"]
                p.dma("sp", dst[0][c % 8, :, t0:t0 + n], y[:, 0:n], reads=[y], writes=[dst[1]])
            if c >= 8:
                dst = gd["Kt"] if c < 16 else gd["Vt"]
                for ti in range(n // 128):
                    ps = p.next_ps()
                    p.tr(ps[:, 0:128], y[:, ti * 128:(ti + 1) * 128], k.ident[:], [y, k.ident], [ps])
                    tk = tkr.next()
                    p.cp(tk[:], ps[:, 0:128], [ps], [tk], eng="act")
                    r0 = t0 + ti * 128
                    p.dma("sp", dst[0][r0:r0 + 128, c % 8, :], tk[:], reads=[tk], writes=[dst[1]])

    nxt = proj(0)
    for c in range(24):
        cur = nxt
        if c + 1 < 24:
            nxt = proj(c + 1)
        post(c, cur)
    wz = p.sb([128, 8, 1024], BF16, "wz")
    for kk in range(8):
        p.dma("pool", wz[:, kk, :], w_in[kk * 128:(kk + 1) * 128, 3072:4096], writes=[wz])
    ztr = p.ring(2, [128, 1024], F32, "zt")
    for tt in range(NT):
        zt = ztr.next()
        for nh in range(2):
            ps = p.next_ps()
            for kk in range(8):
                p.mm(ps[:, :], hT[:, kk, tt * 128:(tt + 1) * 128], wz[:, kk, nh * 512:(nh + 1) * 512], kk == 0, kk == 7, [hT, wz], [ps])
            p.act(zt[:, nh * 512:(nh + 1) * 512], ps[:, :], AF.Silu, [ps], [zt])
        p.dma("sp", gd["Z"][0][tt * 128:(tt + 1) * 128, :], zt[:], reads=[zt], writes=[gd["Z"][1]])
    p.release()
    p.mark()
    cn = {}
    for n_ in ("L0", "L1", "SelC0", "SelC1", "SelA0", "SelA1", "SelB0", "SelB1", "Mb0", "Mb1", "Mn0", "Mn1"):
        t_ = p.sb([128, 128], F32, "c" + n_)
        p.dma("sp", t_[:], k.cin["gd_" + n_], writes=[t_])
        cn[n_] = t_
    rowm = p.sb([128, 2], F32, "rowm")
    p.dma("sp", rowm[:], k.cin["gd_rowm"], writes=[rowm])
    ones128 = p.sb([128, 128], F32, "ones128b")
    p.memset(ones128[:], 1.0, [ones128])
    ident = k.ident
    S = [[p.sb([128, 128], F32, f"S{d}{h}") for h in range(8)] for d in range(2)]
    for d in range(2):
        for h in range(8):
            p.memset(S[d][h][:], 0.0, [S[d][h]], eng="pool")
    H = range(8)
    VH = [(d, h) for h in range(8) for d in range(2)]

    def mk(name):
        return {vh: p.sb([128, 128], F32, f"{name}{vh[0]}{vh[1]}") for vh in VH}
    kTh, qTh, kh, vh_ = mk("kTh"), mk("qTh"), mk("kh"), mk("vh")
    dg, E1, E2, Bm, iT = mk("dg"), mk("E1"), mk("E2"), mk("Bm"), mk("iT")
    Pa, Pb, PTa, PTb, RT = mk("Pa"), mk("Pb"), mk("PTa"), mk("PTb"), mk("RT")
    qs1, qs2, osb = mk("qs1"), mk("qs2"), mk("osb")
    vb, kbg, kdF, kdS, u, wT, v1, v2 = Bm, Pb, PTa, PTb, dg, E1, E2, Pa
    scr = p.ring(4, [128, 64], F32, "gsc")
    order = [list(range(NT)), [1, 0] + list(range(NT - 1, 1, -1))]
    rows = [slice(0, 64), slice(64, 128)]
    for s_ in range(NT):
        tts = [order[0][s_], order[1][s_]]
        fis = [0, 1]
        ses = [1, 0]
        scs = [scr.next(), scr.next()]
        for d in range(2):
            tt, sc, fi, se = tts[d], scs[d], fis[d], ses[d]
            ps = p.next_ps()
            p.mm(ps[:, 0:8], cn[f"L{d}"][:, :], BG[:, tt, 16 + d * 8:24 + d * 8], True, True, [cn[f"L{d}"], BG], [ps])
            p.cp(sc[:, 0:8], ps[:, 0:8], [ps], [sc])
            ps = p.next_ps()
            for ci, nm_ in enumerate(("SelC", "SelA", "SelB")):
                p.mm(ps[:, ci * 8:(ci + 1) * 8], cn[f"{nm_}{d}"][:, :], sc[:, 0:8], True, True, [cn[f"{nm_}{d}"], sc], [ps])
            p.cp(sc[:, 8:32], ps[:, 0:24], [ps], [sc])
            p.act(sc[:, 32:40], sc[:, 0:8], AF.Exp, [sc], [sc])
            p.tt(sc[:, 40:48], sc[:, 8:16], sc[:, 0:8], ALU.subtract, [sc], [sc])
            p.act(sc[:, 40:48], sc[:, 40:48], AF.Exp, [sc], [sc])
            p.act(sc[:, 16:32], sc[:, 16:32], AF.Exp, [sc], [sc])
            p.ts(sc[:, 48:56], sc[:, 40:48], rowm[:, se:se + 1], None, ALU.mult, None, [sc, rowm], [sc])
            p.ts(sc[:, 40:48], sc[:, 40:48], rowm[:, fi:fi + 1], None, ALU.mult, None, [sc, rowm], [sc])
            p.ts(sc[:, 56:64], BG[:, tt, d * 8:d * 8 + 8], -1.0, None, ALU.mult, None, [BG], [sc])
            p.tt(sc[:, 8:16], BG[:, tt, d * 8:d * 8 + 8], sc[:, 32:40], ALU.mult, [BG, sc], [sc])
        for (d, h) in VH:
            vh = (d, h)
            r0 = tts[d] * 128
            p.dma("sp", kTh[vh][:], gd["KT"][0][h, :, r0:r0 + 128], reads=[gd["KT"][1]], writes=[kTh[vh]])
            p.dma("sp", qTh[vh][:], gd["QT"][0][h, :, r0:r0 + 128], reads=[gd["QT"][1]], writes=[qTh[vh]])
            p.dma("sp", kh[vh][:], gd["Kt"][0][r0:r0 + 128, h, :], reads=[gd["Kt"][1]], writes=[kh[vh]])
            p.dma("sp", vh_[vh][:], gd["Vt"][0][r0:r0 + 128, h, :], reads=[gd["Vt"][1]], writes=[vh_[vh]])
        for (d, h) in VH:
            vh = (d, h)
            sc = scs[d]
            gcs = sc[:, h:h + 1]
            p.ts(dg[vh][:], ident[:], gcs, 0.0, ALU.mult, ALU.add, [ident, sc], [dg[vh]], eng="pool")
            psKK = p.next_ps()
            p.mm(psKK[:, 0:128], kTh[vh][:, :], kTh[vh][:, :], True, True, [kTh[vh]], [psKK])
            psKQ = p.next_ps()
            p.mm(psKQ[:, 0:128], kTh[vh][:, :], qTh[vh][:, :], True, True, [kTh[vh], qTh[vh]], [psKQ])
            psBc = p.next_ps()
            p.mm(psBc[:, 0:128], ones128[:, :], dg[vh][:, :], True, True, [ones128, dg[vh]], [psBc])
            p.stt(E1[vh][:], psBc[:, 0:128], gcs, cn[f"Mb{d}"][:], ALU.subtract, ALU.max, [psBc, sc, cn[f"Mb{d}"]], [E1[vh]])
            p.stt(E2[vh][:], psBc[:, 0:128], gcs, cn[f"Mn{d}"][:], ALU.subtract, ALU.min, [psBc, sc, cn[f"Mn{d}"]], [E2[vh]])
            p.act(E1[vh][:], E1[vh][:], AF.Exp, [E1[vh]], [E1[vh]], scale=-1.0)
            p.act(E2[vh][:], E2[vh][:], AF.Exp, [E2[vh]], [E2[vh]])
            p.stt(Bm[vh][:], psKK[:, 0:128], sc[:, 56 + h:57 + h], E1[vh][:], ALU.mult, ALU.mult, [psKK, sc, E1[vh]], [Bm[vh]])
            p.tt(iT[vh][:], psKQ[:, 0:128], E2[vh][:], ALU.mult, [psKQ, E2[vh]], [iT[vh]])
        for vh in VH:
            ps = p.next_ps()
            p.tr(ps[:, 0:128], Bm[vh][:, :], ident[:], [Bm[vh], ident], [ps])
            p.cp(PTa[vh][:], ps[:, 0:128], [ps], [PTa[vh]], eng="act")
            p.tt(RT[vh][:], PTa[vh][:], ident[:], ALU.add, [PTa[vh], ident], [RT[vh]])
        Pc, PTc, Pn, PTn = Bm, PTa, Pa, PTb
        for step in range(5):
            for vh in VH:
                ps1 = p.next_ps()
                p.mm(ps1[:, 0:128], PTc[vh][:, :], Pc[vh][:, :], True, True, [PTc[vh], Pc[vh]], [ps1])
                p.cp(Pn[vh][:], ps1[:, 0:128], [ps1], [Pn[vh]], eng="act")
                if step < 4:
                    ps2 = p.next_ps()
                    p.mm(ps2[:, 0:128], Pc[vh][:, :], PTc[vh][:, :], True, True, [Pc[vh], PTc[vh]], [ps2])
                    p.cp(PTn[vh][:], ps2[:, 0:128], [ps2], [PTn[vh]])
            for vh in VH:
                ps3 = p.next_ps()
                p.mm(ps3[:, 0:128], Pn[vh][:, :], RT[vh][:, :], True, True, [Pn[vh], RT[vh]], [ps3])
                p.tt(RT[vh][:], RT[vh][:], ps3[:, 0:128], ALU.add, [RT[vh], ps3], [RT[vh]])
            Pc, PTc = Pn, PTn
            Pn = Pb if Pn is Pa else Pa
            PTn = PTa if PTn is PTb else PTb
        for (d, h) in VH:
            vh = (d, h)
            sc, tt = scs[d], tts[d]
            p.act(vb[vh][:], vh_[vh][:], AF.Copy, [vh_[vh], BG], [vb[vh]], scale=BG[:, tt, d * 8 + h:d * 8 + h + 1])
            p.ts(kbg[vh][:], kh[vh][:], sc[:, 8 + h:9 + h], None, ALU.mult, None, [kh[vh], sc], [kbg[vh]])
            p.ts(kdF[vh][:], kh[vh][:], sc[:, 40 + h:41 + h], 0.0, ALU.mult, ALU.add, [kh[vh], sc], [kdF[vh]], eng="pool")
            p.act(kdS[vh][:], kh[vh][:], AF.Copy, [kh[vh], sc], [kdS[vh]], scale=sc[:, 48 + h:49 + h])
            psu = p.next_ps()
            p.mm(psu[:, 0:128], RT[vh][:, :], vb[vh][:, :], True, True, [RT[vh], vb[vh]], [psu])
            p.cp(u[vh][:], psu[:, 0:128], [psu], [u[vh]], eng="act")
            psw = p.next_ps()
            p.mm(psw[:, 0:128], kbg[vh][:, :], RT[vh][:, :], True, True, [kbg[vh], RT[vh]], [psw])
            p.cp(wT[vh][:], psw[:, 0:128], [psw], [wT[vh]])
        for (d, h) in VH:
            vh = (d, h)
            Sd = S[d][h]
            ps = p.next_ps()
            p.mm(ps[:, 0:128], wT[vh][:, :], Sd[:, :], True, True, [wT[vh], Sd], [ps])
            p.tt(v1[vh][:], u[vh][:], ps[:, 0:128], ALU.subtract, [u[vh], ps], [v1[vh]])
            ps = p.next_ps()
            p.mm(ps[:, 0:128], qTh[vh][:, :], Sd[:, :], True, True, [qTh[vh], Sd], [ps])
            p.cp(qs1[vh][:], ps[:, 0:128], [ps], [qs1[vh]], eng="act")
        for (d, h) in VH:
            vh = (d, h)
            Sd, sc, fi = S[d][h], scs[d], fis[d]
            ps = p.next_ps()
            p.mm(ps[:, 0:128], kdF[vh][:, :], v1[vh][:, :], True, True, [kdF[vh], v1[vh]], [ps])
            p.stt(Sd[:], Sd[:], sc[:, 16 + fi * 8 + h:17 + fi * 8 + h], ps[:, 0:128], ALU.mult, ALU.add, [Sd, sc, ps], [Sd])
        for (d, h) in VH:
            vh = (d, h)
            Sd = S[d][h]
            ps = p.next_ps()
            p.mm(ps[:, 0:128], wT[vh][:, :], Sd[:, :], True, True, [wT[vh], Sd], [ps])
            p.tt(v2[vh][:], u[vh][:], ps[:, 0:128], ALU.subtract, [u[vh], ps], [v2[vh]])
            ps = p.next_ps()
            p.mm(ps[:, 0:128], qTh[vh][:, :], Sd[:, :], True, True, [qTh[vh], Sd], [ps])
            p.cp(qs2[vh][:], ps[:, 0:128], [ps], [qs2[vh]], eng="act")
        for (d, h) in VH:
            vh = (d, h)
            Sd, sc, se = S[d][h], scs[d], ses[d]
            p.cp(v1[vh][rows[se], :], v2[vh][rows[se], :], [v2[vh]], [v1[vh]], eng="act")
            ps = p.next_ps()
            p.mm(ps[:, 0:128], kdS[vh][:, :], v2[vh][:, :], True, True, [kdS[vh], v2[vh]], [ps])
            p.stt(Sd[:], Sd[:], sc[:, 16 + se * 8 + h:17 + se * 8 + h], ps[:, 0:128], ALU.mult, ALU.add, [Sd, sc, ps], [Sd])
        for (d, h) in VH:
            vh = (d, h)
            sc, fi, se = scs[d], fis[d], ses[d]
            r0 = tts[d] * 128
            ps = p.next_ps()
            p.mm(ps[:, 0:128], iT[vh][:, :], v1[vh][:, :], True, True, [iT[vh], v1[vh]], [ps])
            p.cp(osb[vh][:], ps[:, 0:128], [ps], [osb[vh]], eng="act")
            for (rr, qs) in ((rows[fi], qs1), (rows[se], qs2)):
                p.stt(osb[vh][rr, :], qs[vh][rr, :], sc[rr, 32 + h:33 + h], osb[vh][rr, :], ALU.mult, ALU.add, [qs[vh], sc, osb[vh]], [osb[vh]])
            od = gd[f"O{d}"]
            p.dma("sp", od[0][r0:r0 + 128, h * 128:(h + 1) * 128], osb[vh][:], reads=[osb[vh]], writes=[od[1]])
    p.release()
    p.mark()
    grow = p.sb([1, 1024], F32, "gorow")
    for h in H:
        p.dma("sp", grow[0:1, h * 128:(h + 1) * 128], inp["gdn_g_out"][j:j + 1, :], writes=[grow])
    gob = p.sb([128, 1024], F32, "gob")
    for nh in range(2):
        ps = p.next_ps()
        bcast_rows(p, k, ps[:, :], ps, grow[0:1, nh * 512:(nh + 1) * 512], grow, True, True)
        p.cp(gob[:, nh * 512:(nh + 1) * 512], ps[:, :], [ps], [gob], eng="act")
    wo = p.sb([128, 8, 1024], BF16, "gwo")
    for kk in range(8):
        p.dma("pool", wo[:, kk, :], inp["gdn_w_o"][j, kk * 128:(kk + 1) * 128, :], writes=[wo])
    gts = {}
    for s_ in ((0, 1) if need_ctx else (0,)):
        g_ = p.sb([128, 1024], F32, "ggt1")
        p.dma("sp", g_[:], k.MODS[l, s_, 2], reads=[k.MODSd], writes=[g_])
        gts[s_] = g_
    o0r = p.ring(3, [128, 1024], F32, "o0")
    o1r = p.ring(2, [128, 1024], F32, "o1")
    zr = p.ring(2, [128, 1024], F32, "zz")
    xr = p.ring(3, [128, 1024], F32, "gx")
    sqr_ = p.ring(2, [128, 1024], F32, "gsq2")
    ssr = p.ring(3, [128, 8], F32, "gss")
    oTr = p.ring(2, [128, 8, 128], BF16, "goT")
    yr = p.ring(2, [128, 512], F32, "gy")
    tiles = list(range(NT)) if need_ctx else list(range(2, NT))

    def c1(tt):
        r0 = tt * 128
        o0, o1, zz, x = o0r.next(), o1r.next(), zr.next(), xr.next()
        p.dma("sp", o0[:], gd["O0"][0][r0:r0 + 128, :], reads=[gd["O0"][1]], writes=[o0])
        p.dma("sp", o1[:], gd["O1"][0][r0:r0 + 128, :], reads=[gd["O1"][1]], writes=[o1])
        p.dma("sp", zz[:], gd["Z"][0][r0:r0 + 128, :], reads=[gd["Z"][1]], writes=[zz])
        p.dma("sp", x[:], k.X[r0:r0 + 128, :], reads=[k.Xd], writes=[x])
        p.tt(o0[:], o0[:], o1[:], ALU.add, [o0, o1], [o0])
        sq = sqr_.next()
        p.act(sq[:], o0[:], AF.Square, [o0], [sq])
        ss = ssr.next()
        p.op("dve", lambda e, ss=ss, sq=sq: e.reduce_sum(out=ss[:], in_=sq[:, :].rearrange("p (h d) -> p h d", h=8), axis=AX.X), [sq], [ss])
        rstd_of(p, k, ss[:], 128, 1e-6, [ss], [ss])
        p.tt(zz[:], zz[:], gob[:], ALU.mult, [zz, gob], [zz])
        for h in H:
            p.stt(o0[:, h * 128:(h + 1) * 128], o0[:, h * 128:(h + 1) * 128], ss[:, h:h + 1], zz[:, h * 128:(h + 1) * 128], ALU.mult, ALU.mult, [o0, ss, zz], [o0])
        return o0, x

    def c2(tt, o0, x):
        s_ = 1 if tt < 2 else 0
        r0 = tt * 128
        oT = oTr.next()
        for half in range(2):
            ps = p.next_ps()
            for q in range(4):
                kk = half * 4 + q
                p.tr(ps[:, q * 128:(q + 1) * 128], o0[:, kk * 128:(kk + 1) * 128], k.ident[:], [o0, k.ident], [ps])
            for q in range(4):
                p.cp(oT[:, half * 4 + q, :], ps[:, q * 128:(q + 1) * 128], [ps], [oT], eng="act")
        for nh in range(2):
            ps = p.next_ps()
            for kk in range(8):
                p.mm(ps[:, :], oT[:, kk, :], wo[:, kk, nh * 512:(nh + 1) * 512], kk == 0, kk == 7, [oT, wo], [ps])
            y = yr.next()
            p.tt(y[:], ps[:, :], gts[s_][:, nh * 512:(nh + 1) * 512], ALU.mult, [ps, gts[s_]], [y])
            p.tt(x[:, nh * 512:(nh + 1) * 512], x[:, nh * 512:(nh + 1) * 512], y[:], ALU.add, [x, y], [x])
        p.dma("sp", k.X[r0:r0 + 128, :], x[:], reads=[x], writes=[k.Xd])

    prev = None
    for tt in tiles:
        cur = (tt,) + c1(tt)
        if prev is not None:
            c2(*prev)
        prev = cur
    c2(*prev)
    p.release()
    p.release()


def stage_final(p, k):
    p.mark()
    gr = p.sb([1, 1024], F32, "gfrow")
    p.dma("sp", gr[0:1, :], k.inp["g_final"].rearrange("(o n) -> o n", o=1), writes=[gr])
    gb = p.sb([128, 1024], F32, "gfb")
    for nh in range(2):
        ps = p.next_ps()
        bcast_rows(p, k, ps[:, :], ps, gr[0:1, nh * 512:(nh + 1) * 512], gr, True, True)
        p.cp(gb[:, nh * 512:(nh + 1) * 512], ps[:, :], [ps], [gb], eng="act")
    xr = p.ring(2, [128, 1024], F32, "xf")
    sq = p.sb([128, 1024], F32, "sqf")
    ssr = p.ring(2, [128, 1], F32, "ssf")
    for tt in range(2, NT):
        x = xr.next()
        p.dma("sp", x[:], k.X[tt * 128:(tt + 1) * 128, :], reads=[k.Xd], writes=[x])
        ss = ssr.next()
        p.act(sq[:], x[:], AF.Square, [x], [sq, ss], accum_out=ss[:])
        rstd_of(p, k, ss[:], D, 1e-6, [ss], [ss])
        p.stt(x[:], x[:], ss[:, 0:1], gb[:], ALU.mult, ALU.mult, [x, ss, gb], [x])
        p.dma("sp", k.out[(tt - 2) * 128:(tt - 1) * 128, :], x[:], reads=[x], writes=[k.outd])
    p.release()


def build(cfg=None):
    cfg = cfg or {"stages": "all"}
    nc = bass.Bass("TRN2", target_bir_lowering=False)
    k = K()
    k.nc = nc
    k.inp = LazyInputs(nc)
    consts = host_consts()
    k.cin = {n: nc.dram_tensor("k_" + n, list(v.shape), F32, kind="ExternalInput").ap() for n, v in consts.items()}
    k.out = nc.dram_tensor("out", [NLAT, D], F32, kind="ExternalOutput").ap()
    k.outd = Dep("out")
    k.X = nc.dram_tensor("Xres", [NTOK, D], F32).ap()
    k.Xd = Dep("X")
    k.MODS = nc.dram_tensor("MODS", [DEPTH, 2, 6, 128, 1024], F32).ap()
    k.MODSd = Dep("MODS")
    dbg = cfg.get("debug_out", {})
    k.dbg = {n: nc.dram_tensor("dbg_" + n, s, F32, kind="ExternalOutput").ap() for n, s in dbg.items()}
    k.dbgd = Dep("dbg")
    p = Prog(nc)
    k.pstiles = [p.psum([128, 512], F32, f"psb{i}") for i in range(8)]
    p.ps = Ring(k.pstiles)
    k.psx = k.pstiles[4:8]
    k.ident = p.sb([128, 128], F32, "ident")
    p.dma("sp", k.ident[:], k.cin["ident"], writes=[k.ident])
    k.ones_row = p.sb([1, 128], F32, "ones_row")
    p.memset(k.ones_row[:], 1.0, [k.ones_row])
    k.ltmp = p.ring(2, [128, 128], F32, "ltmp")
    stages = cfg["stages"]
    if stages == "all":
        stages = [("init",), ("mod", list(range(DEPTH)))]
        for l in range(DEPTH):
            stages += [("mixer", l), ("ffn", l)]
        stages += [("final",)]
    for st in stages:
        if st[0] == "init":
            stage_init(p, k)
        elif st[0] == "mod":
            stage_mod(p, k, st[1])
        elif st[0] == "ffn":
            l = st[1]
            tiles = list(range(NT)) if l < DEPTH - 1 else list(range(2, NT))
            p.mark()
            hT = p.sb([128, 8, NTOK], BF16, "hT")
            G = p.sb([128, NT, NE], F32, "G")
            stage_norm(p, k, l, 1, tiles, hT, router=({"G": G} if not cfg.get("no_router") else None))
            if "G" in k.dbg:
                for tt in tiles:
                    p.dma("sp", k.dbg["G"][tt * 128:(tt + 1) * 128, :], G[:, tt, :], reads=[G], writes=[k.dbgd])
            if not cfg.get("skip_moe"):
                stage_moe(p, k, l, tiles, hT, G)
            p.release()
        elif st[0] == "final":
            stage_final(p, k)
        elif st[0] == "dumpX":
            p.dma("sp", k.dbg["X"], k.X, reads=[k.Xd], writes=[k.dbgd])
        elif st[0] == "mixer":
            l = st[1]
            kind, j = l % 3, l // 3
            need_ctx = l < DEPTH - 1
            if kind in (0, 1):
                p.barrier()
                p.ps = Ring(k.pstiles[0:4])
                stage_attn(p, k, l, kind, j, need_ctx)
                p.ps = Ring(k.pstiles)
            else:
                stage_gdn(p, k, l, j, need_ctx)
    p.emit()
    k_used = list(k.inp.keys())
    return nc, consts, k_used


def from_mixers(p, k, l):
    raise NotImplementedError


_CACHE = {}


def kernel(**inputs):
    if "nc" not in _CACHE:
        _CACHE["nc"] = build()
    nc, consts, used = _CACHE["nc"]
    n = 8
    in_maps = []
    for b in range(n):
        m = {}
        for name in used:
            a = np.asarray(inputs[name], dtype=np.float32)
            if name in ("x", "c", "ctx"):
                a = a[b]
            m[name] = np.ascontiguousarray(a)
        for cn, cv in consts.items():
            m["k_" + cn] = cv
        in_maps.append(m)
    res = run_bass_kernel_spmd(nc, in_maps, core_ids=list(range(n)))
    return np.stack([r["out"] for r in res.results], axis=0).astype(np.float32)
```
